# Optimizing a Trainium2 kernel written in Bass

```python
import math
import jax, jax.numpy as jnp
from jax import lax
import numpy as np

D_MODEL = 1024
BATCH = 4
SEQ = 8192
DEPTH = 2

N_HEADS = 16
HEAD_DIM = D_MODEL // N_HEADS
CONV_WIDTH = 31
D_FF = ((8 * D_MODEL // 3) + 127) // 128 * 128
N_EXPERTS = 8
TOP_K = 2
Q_BLOCK = 128
EPS = 1e-6
N_A_LAYERS = (DEPTH + 1) // 2
N_B_LAYERS = DEPTH - N_A_LAYERS
N_DENSE = (DEPTH + 1) // 2
N_MOE = DEPTH // 2
FORGET_BIAS_MEAN = 2.0

kernel_name = "yoco_conformer_fox_moe_block"


def rms_norm(x, g):
    xf = x.astype(jnp.float32)
    y = xf * lax.rsqrt(jnp.mean(xf * xf, axis=-1, keepdims=True) + EPS)
    return (y * g.astype(jnp.float32)).astype(x.dtype)


def layer_norm(x, g, b):
    xf = x.astype(jnp.float32)
    mu = jnp.mean(xf, axis=-1, keepdims=True)
    var = jnp.mean(jnp.square(xf - mu), axis=-1, keepdims=True)
    y = (xf - mu) * lax.rsqrt(var + EPS)
    return (y * g.astype(jnp.float32) + b.astype(jnp.float32)).astype(x.dtype)


def conformer_conv(h, w_in, b_in, w_dw, b_dw, ln_g, ln_b, w_out, b_out):
    u = h @ w_in + b_in
    a, gate = jnp.split(u, 2, axis=-1)
    u = a * jax.nn.sigmoid(gate)
    u = lax.conv_general_dilated(
        u, w_dw[:, None, :].astype(u.dtype),
        window_strides=(1,), padding=[(CONV_WIDTH - 1, 0)],
        dimension_numbers=("NWC", "WIO", "NWC"),
        feature_group_count=D_MODEL) + b_dw
    u = layer_norm(u, ln_g, ln_b)
    u = jax.nn.silu(u)
    return u @ w_out + b_out


def swiglu(h, w_gate, w_up, w_down):
    return (jax.nn.silu(h @ w_gate) * (h @ w_up)) @ w_down


def moe_swiglu(h, router_w, router_b, w_gate, w_up, w_down):
    logits = (h @ router_w + router_b).astype(jnp.float32)
    top_vals, top_idx = lax.top_k(logits, TOP_K)
    top_p = jax.nn.softmax(top_vals, axis=-1)
    gates = jnp.sum(jax.nn.one_hot(top_idx, N_EXPERTS, dtype=jnp.float32) * top_p[..., None], axis=-2)
    gates = gates.astype(h.dtype)
    out = jnp.zeros_like(h)
    for e in range(N_EXPERTS):
        out = out + gates[..., e:e + 1] * swiglu(h, w_gate[e], w_up[e], w_down[e])
    return out


def shared_kv(x, kv_norm, w_kvf, b_f):
    bsz, seq, _ = x.shape
    hn = rms_norm(x, kv_norm)
    proj = hn @ w_kvf
    k = proj[..., :D_MODEL].reshape(bsz, seq, N_HEADS, HEAD_DIM)
    v = proj[..., D_MODEL:2 * D_MODEL].reshape(bsz, seq, N_HEADS, HEAD_DIM)
    f_logit = proj[..., 2 * D_MODEL:].astype(jnp.float32) + b_f.astype(jnp.float32)
    logf_cum = jnp.cumsum(jax.nn.log_sigmoid(f_logit), axis=1)
    return k, v, jnp.transpose(logf_cum, (0, 2, 1))


def forgetting_attention(q, k, v, logf_cum):
    seq = q.shape[1]
    scale = HEAD_DIM ** -0.5
    qf = q.astype(jnp.float32)
    kf = k.astype(jnp.float32)
    outs = []
    for blk in range(seq // Q_BLOCK):
        q0 = blk * Q_BLOCK
        q1 = q0 + Q_BLOCK
        s = jnp.einsum("bqhd,bkhd->bhqk", qf[:, q0:q1], kf[:, :q1]) * scale
        s = s + logf_cum[:, :, q0:q1, None] - logf_cum[:, :, None, :q1]
        mask = (q0 + jnp.arange(Q_BLOCK))[:, None] >= jnp.arange(q1)[None, :]
        s = jnp.where(mask, s, -jnp.inf)
        p = jax.nn.softmax(s, axis=-1).astype(v.dtype)
        outs.append(jnp.einsum("bhqk,bkhd->bqhd", p, v[:, :q1]))
    return jnp.concatenate(outs, axis=1)


def setup_inputs(seed: int = 0) -> dict:
    key = jax.random.key(seed)
    ks = jax.random.split(key, 32)
    f32 = jnp.float32

    def w(k, shape, fan_in):
        return jax.random.normal(k, shape, f32) * (fan_in ** -0.5)

    def gain(k, shape):
        return 1.0 + 0.02 * jax.random.normal(k, shape, f32)

    def bias(k, shape, scale=0.02):
        return scale * jax.random.normal(k, shape, f32)

    D = D_MODEL
    return {
        "x": jax.random.normal(ks[0], (BATCH, SEQ, D), f32),
        "mix_norm": gain(ks[1], (DEPTH, D)),
        "ffn_norm": gain(ks[2], (DEPTH, D)),
        "conv_w_in": w(ks[3], (N_A_LAYERS, D, 2 * D), D),
        "conv_b_in": bias(ks[4], (N_A_LAYERS, 2 * D)),
        "conv_w_dw": w(ks[5], (N_A_LAYERS, CONV_WIDTH, D), CONV_WIDTH),
        "conv_b_dw": bias(ks[6], (N_A_LAYERS, D)),
        "conv_ln_g": gain(ks[7], (N_A_LAYERS, D)),
        "conv_ln_b": bias(ks[8], (N_A_LAYERS, D)),
        "conv_w_out": w(ks[9], (N_A_LAYERS, D, D), D),
        "conv_b_out": bias(ks[10], (N_A_LAYERS, D)),
        "kv_norm": gain(ks[11], (D,)),
        "w_kvf": w(ks[12], (D, 2 * D + N_HEADS), D),
        "b_f": FORGET_BIAS_MEAN + 0.5 * jax.random.normal(ks[13], (N_HEADS,), f32),
        "w_q": w(ks[14], (N_B_LAYERS, D, D), D),
        "w_o": w(ks[15], (N_B_LAYERS, D, D), D),
        "ffn_w_gate": w(ks[16], (N_DENSE, D, D_FF), D),
        "ffn_w_up": w(ks[17], (N_DENSE, D, D_FF), D),
        "ffn_w_down": w(ks[18], (N_DENSE, D_FF, D), D_FF),
        "router_w": w(ks[19], (N_MOE, D, N_EXPERTS), D),
        "router_b": bias(ks[20], (N_MOE, N_EXPERTS), 0.01),
        "moe_w_gate": w(ks[21], (N_MOE, N_EXPERTS, D, D_FF), D),
        "moe_w_up": w(ks[22], (N_MOE, N_EXPERTS, D, D_FF), D),
        "moe_w_down": w(ks[23], (N_MOE, N_EXPERTS, D_FF, D), D_FF),
        "final_norm": gain(ks[24], (D,)),
    }


def reference(x, mix_norm, ffn_norm, conv_w_in, conv_b_in, conv_w_dw, conv_b_dw, conv_ln_g, conv_ln_b,
              conv_w_out, conv_b_out, kv_norm, w_kvf, b_f, w_q, w_o, ffn_w_gate, ffn_w_up, ffn_w_down,
              router_w, router_b, moe_w_gate, moe_w_up, moe_w_down, final_norm):
    bsz, seq, _ = x.shape
    a_i = b_i = d_i = m_i = 0
    k = v = logf_cum = None
    for layer in range(DEPTH):
        h = rms_norm(x, mix_norm[layer])
        if layer < N_A_LAYERS:
            x = x + conformer_conv(h, conv_w_in[a_i], conv_b_in[a_i], conv_w_dw[a_i], conv_b_dw[a_i],
                                   conv_ln_g[a_i], conv_ln_b[a_i], conv_w_out[a_i], conv_b_out[a_i])
            a_i += 1
        else:
            if k is None:
                k, v, logf_cum = shared_kv(x, kv_norm, w_kvf, b_f)
            q = (h @ w_q[b_i]).reshape(bsz, seq, N_HEADS, HEAD_DIM)
            o = forgetting_attention(q, k, v, logf_cum).reshape(bsz, seq, D_MODEL)
            x = x + o @ w_o[b_i]
            b_i += 1
        h = rms_norm(x, ffn_norm[layer])
        if layer % 2 == 0:
            x = x + swiglu(h, ffn_w_gate[d_i], ffn_w_up[d_i], ffn_w_down[d_i])
            d_i += 1
        else:
            x = x + moe_swiglu(h, router_w[m_i], router_b[m_i], moe_w_gate[m_i], moe_w_up[m_i], moe_w_down[m_i])
            m_i += 1
    return rms_norm(x, final_norm)
```

```python
import numpy as np
import ml_dtypes
import concourse.bass as bass
import concourse.mybir as mybir
from concourse.bass_utils import run_bass_kernel_spmd


F32 = mybir.dt.float32
BF16 = mybir.dt.bfloat16
AF = mybir.ActivationFunctionType
ALU = mybir.AluOpType

ENGS = ("pe", "act", "dve", "pool", "sp")


class Buf:
    __slots__ = ("name", "last_w", "readers", "dsem")

    def __init__(self, name):
        self.name = name
        self.last_w = None
        self.readers = []
        self.dsem = None


class Op:
    __slots__ = ("eng", "emit", "deps", "is_dma", "needs_inc", "semval", "dsem", "dval", "npieces", "idx", "name", "inc", "hoist")

    def __init__(self, eng, emit, is_dma=False, name=""):
        self.eng = eng
        self.emit = emit
        self.deps = []
        self.is_dma = is_dma
        self.needs_inc = False
        self.semval = None
        self.dsem = None
        self.dval = None
        self.npieces = 0
        self.name = name
        self.hoist = False


class DSem:
    __slots__ = ("sem", "total", "last_op")

    def __init__(self, sem):
        self.sem = sem
        self.total = 0
        self.last_op = None


class Prog:
    def __init__(self, nc):
        self.nc = nc
        self.ops = {e: [] for e in ENGS}
        self.all_ops = []
        self.esem = {}
        self.dsems = []
        self.pending_barrier = {e: [] for e in ENGS}
        self.sb_off = 16512
        self.sb_end = 229344
        self._n = 0

    def sb(self, name, shape, dtype, off=None):
        nbytes = int(np.prod(shape[1:])) * (2 if dtype == BF16 else 4)
        if off is None:
            off = (self.sb_off + 63) // 64 * 64
            self.sb_off = off + nbytes
            assert self.sb_off <= self.sb_end, f"SBUF overflow at {name}: {self.sb_off}"
        self._n += 1
        return self.nc.alloc_sbuf_tensor_at(f"{name}_{self._n}", list(shape), dtype, offset=off)

    def new_dsem(self, name):
        d = DSem(self.nc.alloc_semaphore(f"d_{name}_{len(self.dsems)}"))
        self.dsems.append(d)
        return d

    def _add(self, op, reads, writes):
        deps = []
        for b in reads:
            if b.last_w is not None:
                deps.append(b.last_w)
        for b in writes:
            if b.last_w is not None:
                deps.append(b.last_w)
            deps.extend(b.readers)
        deps.extend(self.pending_barrier[op.eng])
        self.pending_barrier[op.eng] = []
        seen = set()
        for d in deps:
            if d is op or id(d) in seen:
                continue
            seen.add(id(d))
            op.deps.append(d)
            if not d.is_dma:
                d.needs_inc = True
        for b in reads:
            b.readers.append(op)
        for b in writes:
            b.last_w = op
            b.readers = []
        op.idx = len(self.all_ops)
        self.ops[op.eng].append(op)
        self.all_ops.append(op)
        return op

    def op(self, eng, emit, reads=(), writes=(), name=""):
        return self._add(Op(eng, emit, False, name), list(reads), list(writes))

    def dma(self, q, emit, dsem, npieces, reads=(), writes=(), name="", inc=16):
        o = Op(q, emit, True, name)
        o.dsem = dsem
        o.npieces = npieces
        o.inc = inc
        if dsem.last_op is not None:
            o.deps.append(dsem.last_op)
        dsem.total += inc * npieces
        o.dval = dsem.total
        dsem.last_op = o
        return self._add(o, list(reads), list(writes))

    def barrier(self):
        lasts = []
        for e in ENGS:
            if self.ops[e]:
                for o in reversed(self.ops[e]):
                    if not o.is_dma:
                        lasts.append(o)
                        break
        for d in self.dsems:
            if d.last_op is not None:
                lasts.append(d.last_op)
        for e in ENGS:
            self.pending_barrier[e] = list(lasts)

    def emit_all(self, final_waits_eng="sp"):
        nc = self.nc
        for e in ("pe", "act", "dve", "pool"):
            self.esem[e] = nc.alloc_semaphore(f"e_{e}")
        for e in ENGS:
            cnt = 0
            for o in self.ops[e]:
                if o.is_dma:
                    continue
                if o.needs_inc:
                    cnt += 1
                    o.semval = cnt
        peidx = {id(o): i for i, o in enumerate(self.ops["pe"])}
        maxpe = {}
        last = {e: -1 for e in ENGS}
        for o in self.all_ops:
            m = last[o.eng]
            if o.eng == "pe":
                m = max(m, peidx[id(o)])
            for d in o.deps:
                m = max(m, maxpe[id(d)])
            maxpe[id(o)] = m
            last[o.eng] = m
        stats = {}
        with nc.Block() as block:
            def wait_list(e):
                waited = {}
                out = []
                for o in self.ops[e]:
                    ws = []
                    for d in o.deps:
                        if d.is_dma:
                            key, sem, val = id(d.dsem), d.dsem.sem, d.dval
                        else:
                            if d.eng == e and e == "pe":
                                continue
                            key, sem, val = d.eng, self.esem[d.eng], d.semval
                        if waited.get(key, 0) >= val:
                            continue
                        waited[key] = val
                        ws.append((sem, val, maxpe[id(d)]))
                    out.append(ws)
                return out, waited

            def run(e):
                def body(eng):
                    W, waited = wait_list(e)
                    ops = self.ops[e]
                    if e == "pe":
                        for j in range(1, len(ops)):
                            if not ops[j].hoist:
                                continue
                            keep = []
                            for w in W[j]:
                                if w[2] <= j - 2:
                                    W[j - 1].append(w)
                                else:
                                    keep.append(w)
                            W[j] = keep
                    nw = 0
                    for o, ws in zip(ops, W):
                        for (sem, val, _) in ws:
                            eng.wait_ge(sem, val)
                            nw += 1
                        r = o.emit(eng)
                        if o.is_dma:
                            assert len(r) == o.npieces, (o.name, len(r), o.npieces)
                            for ins in r:
                                ins.then_inc(o.dsem.sem, o.inc)
                        elif o.needs_inc:
                            r.then_inc(self.esem[e], 1)
                    if e == final_waits_eng:
                        for d in self.dsems:
                            if d.total and waited.get(id(d), 0) < d.total:
                                eng.wait_ge(d.sem, d.total)
                    stats[e] = (len(ops), nw)
                return body
            block.tensor(run("pe"))
            block.scalar(run("act"))
            block.vector(run("dve"))
            block.gpsimd(run("pool"))
            block.sync(run("sp"))
        return stats


D = 1024
KC = 8
T = 512
TOK = 4096
NT = TOK // T
FF = 2816
FC = 22
H = 16
CW = 31
NE = 8
EPS = 1e-6

V_MIX0, V_FFN0, V_BA, V_BG, V_BDW, V_LNG, V_LNB, V_BOUT, V_KVN, V_MIX1, V_FFN1, V_FIN = [8 * i for i in range(12)]
NV = 96


class Ctx:
    pass


def declare_io(nc, c, debug, ne_decl=NE):
    c.ne_decl = ne_decl
    def din(name, shape, dt=F32):
        return nc.dram_tensor(name, list(shape), dt, kind="ExternalInput")

    c.x = din("x", [TOK, D])
    c.xhalo = din("xhalo", [32, D])
    c.vecs = din("vecs", [128, NV])
    c.wdw = din("wdw", [128, KC, CW])
    c.bf = din("bf", [16, 1])
    c.hmask = din("hmask", [128, 1])
    c.flagrow = din("flagrow", [1, TOK], BF16)
    c.rw = din("rw", [128, KC, NE])
    c.rb4 = din("rb4", [128, 4, NE])
    c.sel = din("sel", [128, NE, 128])
    c.ident = din("ident", [128, 128])
    c.cmask = din("cmask", [128, 128], BF16)
    c.w_in = din("conv_w_in", [D, 2 * D])
    c.w_out = din("conv_w_out", [D, D])
    c.ffn_g = din("ffn_w_gate", [D, FF])
    c.ffn_u = din("ffn_w_up", [D, FF])
    c.ffn_d = din("ffn_w_down", [FF, D])
    c.w_kvf = din("w_kvf", [D, 2 * D + H])
    c.w_q = din("w_q", [D, D])
    c.w_o = din("w_o", [D, D])
    c.moe_g = din("moe_w_gate", [ne_decl, D, FF])
    c.moe_u = din("moe_w_up", [ne_decl, D, FF])
    c.moe_d = din("moe_w_down", [ne_decl, FF, D])

    def scr(name, shape, dt=BF16):
        return nc.dram_tensor(name, list(shape), dt)

    c.w_in_b = scr("w_in_b", [D, 2 * D])
    c.w_out_b = scr("w_out_b", [D, D])
    c.ffn_g_b = scr("ffn_g_b", [D, FF])
    c.ffn_u_b = scr("ffn_u_b", [D, FF])
    c.ffn_d_b = scr("ffn_d_b", [FF, D])
    c.w_kvf_b = scr("w_kvf_b", [D, 2 * D + H])
    c.w_q_b = scr("w_q_b", [D, D])
    c.w_o_b = scr("w_o_b", [D, D])
    c.moe_g_b = scr("moe_g_b", [ne_decl, D, FF])
    c.moe_u_b = scr("moe_u_b", [ne_decl, D, FF])
    c.moe_d_b = scr("moe_d_b", [ne_decl, FF, D])
    c.x1T = scr("x1T", [128, KC, TOK], F32)
    c.gk = [scr(f"gk{t}", [D, T]) for t in range(NT)]
    c.gko = [scr(f"gko{t}", [2 * D, T]) for t in range(NT)]
    c.gv = [scr(f"gv{t}", [T, D]) for t in range(NT)]
    c.gvo = [scr(f"gvo{t}", [2 * T, D]) for t in range(NT)]
    c.ga = scr("ga", [112, TOK])
    c.gao = scr("gao", [224, TOK])
    c.qT = scr("qT", [D, TOK])
    c.kaug_own = scr("kaug_own", [H, 7, TOK])
    c.qaug = scr("qaug", [H, 7, TOK])
    c.oT = scr("oT", [D, TOK])
    c.flog = scr("flog_d", [16, TOK], F32)
    c.rsum = scr("rsum", [H, TOK], F32)
    c.out = nc.dram_tensor("out", [TOK, D], F32, kind="ExternalOutput")
    c.debug = debug
    if debug in ("x2", "x3", "g"):
        c.dbg_x2T = nc.dram_tensor("dbg_x2T", [128, KC, TOK], F32, kind="ExternalOutput")


def setup_common(P, c):
    nc = P.nc
    c.ps = nc.alloc_psum_tensor("ps", [128, 8, 512], F32)
    c.psb = [Buf(f"psb{i}") for i in range(8)]
    c.ps_rr = 0
    c.ident_f = P.sb("ident_f", [128, 128], F32)
    c.ident_b = P.sb("ident_b", [128, 128], BF16)
    c.ones_b = P.sb("ones_b", [128, 128], BF16)
    c.vec = P.sb("vec", [128, NV], F32)
    c.vech = P.sb("vech", [128, 16], F32)
    c.wdw_s = P.sb("wdw_s", [128, KC, CW], F32)
    c.bf_s = P.sb("bf_s", [16, 1], F32)
    c.hmask_s = P.sb("hmask_s", [128, 1], F32)
    c.eps_t = P.sb("eps_t", [128, 1], F32)
    c.B_const = Buf("const")
    ds = P.new_dsem("const")
    c.ds_const = ds

    def ld(eng):
        r = []
        r.append(eng.dma_start(out=c.ident_f[:, :], in_=c.ident[:, :]))
        r.append(eng.dma_start(out=c.vec[:, :], in_=c.vecs[:, :]))
        r.append(eng.dma_start(out=c.wdw_s[:, :, :], in_=c.wdw[:, :, :]))
        r.append(eng.dma_start(out=c.bf_s[:, :], in_=c.bf[:, :]))
        r.append(eng.dma_start(out=c.hmask_s[:, :], in_=c.hmask[:, :]))
        return r
    P.dma("sp", ld, ds, 5, writes=[c.B_const], name="const_ld")
    c.B_const2 = Buf("const2")
    c.sb_phase_base = None

    def mk(eng):
        eng.tensor_copy(out=c.ident_b[:, :], in_=c.ident_f[:, :])
        eng.memset(c.ones_b[:, :], 1.0 / 1024.0)
        eng.memset(c.eps_t[:, :], EPS)
        return eng.tensor_scalar(out=c.vech[:, :], in0=c.vec[:, V_BA:V_BA + 16], scalar1=0.5, scalar2=None, op0=ALU.mult)
    P.op("dve", mk, reads=[c.B_const], writes=[c.B_const2], name="const_mk")


def psbank(c):
    i = c.ps_rr
    c.ps_rr = (c.ps_rr + 1) % 8
    return i


def convert_weights(P, c):
    c.B_w = {}

    def grp(name, pairs):
        ds = P.new_dsem("cv_" + name)
        b = Buf("wb_" + name)
        c.B_w[name] = b

        def em(eng, pairs=pairs):
            return [eng.dma_start(out=o, in_=i) for (o, i) in pairs]
        P.dma("pool", em, ds, len(pairs), writes=[b], name="cv_" + name)

    grp("in", [(c.w_in_b[:, :], c.w_in[:, :]), (c.w_out_b[:, :], c.w_out[:, :])])
    grp("ffn", [(c.ffn_g_b[:, :], c.ffn_g[:, :]), (c.ffn_u_b[:, :], c.ffn_u[:, :]),
                (c.ffn_d_b[0:1024, :], c.ffn_d[0:1024, :]), (c.ffn_d_b[1024:2048, :], c.ffn_d[1024:2048, :]),
                (c.ffn_d_b[2048:FF, :], c.ffn_d[2048:FF, :])])
    grp("att", [(c.w_kvf_b[:, :], c.w_kvf[:, :]), (c.w_q_b[:, :], c.w_q[:, :]), (c.w_o_b[:, :], c.w_o[:, :])])


def convert_expert(P, c, e):
    ds = P.new_dsem(f"cv_e{e}")
    b = Buf(f"wb_e{e}")
    c.B_w[f"e{e}"] = b
    pairs = [(c.moe_g_b[e, :, :], c.moe_g[e, :, :]), (c.moe_u_b[e, :, :], c.moe_u[e, :, :]),
             (c.moe_d_b[e, 0:1024, :], c.moe_d[e, 0:1024, :]), (c.moe_d_b[e, 1024:2048, :], c.moe_d[e, 1024:2048, :]),
             (c.moe_d_b[e, 2048:FF, :], c.moe_d[e, 2048:FF, :])]

    def em(eng, pairs=pairs):
        return [eng.dma_start(out=o, in_=i) for (o, i) in pairs]
    P.dma("pool", em, ds, len(pairs), writes=[b], name=f"cv_e{e}")


class Ring:
    def __init__(self, P, n, name="ring"):
        self.P = P
        self.n = n
        self.off = []
        self.bufs = [Buf(f"{name}{i}") for i in range(n)]
        self.ds = [P.new_dsem(f"{name}{i}") for i in range(n)]
        self.v8 = []
        self.vgu = []
        for i in range(n):
            t = P.sb(f"{name}{i}", [128, 8, 1024], BF16)
            off = P.sb_off - 16384
            self.v8.append(t)
            self.vgu.append(P.sb(f"{name}gu{i}", [128, 2, 8, 512], BF16, off=off))
        self.rr = 0

    def load(self, pieces_fn, wbuf, name="wl"):
        i = self.rr
        self.rr = (self.rr + 1) % self.n
        pieces = pieces_fn(i)

        def em(eng, pieces=pieces):
            return [eng.dma_start(out=o, in_=s) for (o, s) in pieces]
        self.P.dma("sp", em, self.ds[i], len(pieces), reads=[wbuf], writes=[self.bufs[i]], name=name)
        return i


def w8(src, c0, ncols):
    return src[:, c0:c0 + ncols].rearrange("(kc p) c -> p kc c", p=128)


def alloc_phase_a(P, c):
    c.ring = Ring(P, 4)
    c.x_tok = P.sb("x_tok", [128, 4, D], F32)
    off = P.sb_off - 16384
    c.v = P.sb("v", [128, KC, T], F32, off=off)
    c.B_r1 = Buf("r1")
    c.B_v = [Buf(f"v{k}") for k in range(KC)]
    c.xT = P.sb("xT", [128, KC, T], F32)
    c.B_xT = [Buf(f"xT{k}") for k in range(KC)]
    c.sq = P.sb("sq", [128, KC, T], BF16)
    c.B_sq = Buf("sq")
    c.hT = P.sb("hT", [128, KC, T], BF16)
    c.B_hT = Buf("hT")
    c.ap_ = P.sb("aprime", [128, KC, T], BF16)
    c.B_ap = [Buf(f"ap{k}") for k in range(KC)]
    c.uT = P.sb("uT", [128, KC, 32 + T], BF16)
    c.B_uT = [Buf(f"uT{k}") for k in range(KC)]
    c.sT = P.sb("sT", [128, KC, T], BF16)
    c.B_sT = Buf("sT")
    c.aT = P.sb("aT", [128, FC, T], BF16)
    c.B_aT = [Buf(f"aT{k}") for k in range(FC)]
    offa = P.sb_off - FC * T * 2
    c.kst = P.sb("kst", [128, KC, T], BF16, off=offa)
    c.vst = P.sb("vst", [128, 4, D], BF16, off=offa + 8192)
    c.diag = P.sb("diag", [128, 2, CW, 128], BF16)
    c.B_diag = [Buf("diag0"), Buf("diag1")]
    c.th = [P.sb(f"th{i}", [128, T], F32) for i in range(3)]
    c.B_th = [Buf(f"th{i}") for i in range(3)]
    c.th_rr = 0
    c.st = [P.sb(f"st{i}", [128, T], F32) for i in range(6)]
    c.B_st = [Buf(f"st{i}") for i in range(6)]
    c.B_flog = Buf("flog")
    c.wf = P.sb("wf", [128, KC, 16], BF16)
    c.B_wf = Buf("wf")
    c.ds_x = P.new_dsem("x")
    c.ds_st = [P.new_dsem(f"store{i}") for i in range(4)]
    c.B_x1T = [Buf(f"x1T_{t}") for t in range(NT)]
    c.B_gk = [Buf(f"gk{t}") for t in range(NT)]
    c.B_gv = [Buf(f"gv{t}") for t in range(NT)]
    c.B_gko = [Buf(f"gko{t}") for t in range(NT)]
    c.B_gvo = [Buf(f"gvo{t}") for t in range(NT)]
    c.B_ga = Buf("ga")
    c.B_gao = Buf("gao")
    c.B_qT = Buf("qTd")


def mm_group(P, c, bank, pairs, reads, ncols, nrows=128, name="mm", col0=0):
    ps = c.ps
    n = len(pairs)

    def em(pe, pairs=pairs):
        r = None
        for i, (l, rr) in enumerate(pairs):
            r = pe.matmul(ps[0:nrows, bank, col0:col0 + ncols], l, rr, start=(i == 0), stop=(i == n - 1))
        return r
    o = P.op("pe", em, reads=reads, writes=[c.psb[bank]], name=name)
    o.hoist = True
    return o


def rms_stats(P, c, ncols, src_bufs):
    xT, sq = c.xT, c.sq
    P.op("act", lambda a: a.activation(out=sq[:, :, 0:ncols], in_=xT[:, :, 0:ncols], func=AF.Square),
         reads=src_bufs, writes=[c.B_sq], name="sq")
    b = psbank(c)
    mm_group(P, c, b, [(c.ones_b[:, :], sq[:, k, 0:ncols]) for k in range(KC)], [c.B_sq, c.B_const2], ncols, name="ms")
    i0, i1 = 0, 1
    t0, t1 = c.st[i0], c.st[i1]
    P.op("act", lambda a: a.activation(out=t0[:, 0:ncols], in_=c.ps[:, b, 0:ncols], func=AF.Ln, bias=c.eps_t[:, 0:1], scale=1.0),
         reads=[c.psb[b], c.B_const2], writes=[c.B_st[i0]], name="ms_ln")
    P.op("act", lambda a: a.activation(out=t1[:, 0:ncols], in_=t0[:, 0:ncols], func=AF.Exp, scale=-0.5),
         reads=[c.B_st[i0]], writes=[c.B_st[i1]], name="rstd")
    return i1


def apply_norm(P, c, ncols, rstd_i, gcol, dst, dstbuf, name="hn"):
    xT = c.xT
    rs = c.st[rstd_i]
    for k in range(KC):
        P.op("dve", lambda v, k=k: v.scalar_tensor_tensor(out=dst[:, k, 0:ncols], in0=xT[:, k, 0:ncols], scalar=c.vec[:, gcol + k:gcol + k + 1],
                                                         in1=rs[:, 0:ncols], op0=ALU.mult, op1=ALU.mult),
             reads=[c.B_xT[k], c.B_st[rstd_i], c.B_const], writes=[dstbuf], name=name)


def load_x_and_transpose(P, c, src_ap, ntok):
    nch = (ntok + 127) // 128
    if ntok >= 128:
        def em(eng):
            return [eng.dma_start(out=c.x_tok[:, 0:nch, :], in_=src_ap.rearrange("(c p) d -> p c d", p=128))]
    else:
        def em(eng):
            return [eng.dma_start(out=c.x_tok[0:ntok, 0, :], in_=src_ap)]
    P.dma("sp", em, c.ds_x, 1, writes=[c.B_r1] + c.B_v, name="x_ld")
    for k in range(KC):
        b = psbank(c)

        def emt(pe, k=k, b=b):
            r = None
            for tc in range(nch):
                n = min(128, ntok - tc * 128)
                r = pe.transpose(out=c.ps[:, b, tc * 128:tc * 128 + n], in_=c.x_tok[0:n, tc, k * 128:(k + 1) * 128], identity=c.ident_f[0:n, 0:n])
            return r
        P.op("pe", emt, reads=[c.B_r1, c.B_const], writes=[c.psb[b]], name="xpose")
        if k % 2 == 0:
            P.op("act", lambda a, k=k, b=b: a.copy(out=c.xT[:, k, 0:ntok], in_=c.ps[:, b, 0:ntok]), reads=[c.psb[b]], writes=[c.B_xT[k]], name="xT_ev")
        else:
            P.op("dve", lambda v, k=k, b=b: v.tensor_copy(out=c.xT[:, k, 0:ntok], in_=c.ps[:, b, 0:ntok]), reads=[c.psb[b]], writes=[c.B_xT[k]], name="xT_ev")


def glu_stage(P, c, ncols, ucol0, mask_halo=False):
    ring = c.ring
    sa = ring.load(lambda i: [(ring.v8[i][:, :, :], w8(c.w_in_b, 0, 1024))], c.B_w["in"], name="w_in_a")
    sg = ring.load(lambda i: [(ring.v8[i][:, :, :], w8(c.w_in_b, 1024, 1024))], c.B_w["in"], name="w_in_g")
    for oc in range(KC):
        b = psbank(c)
        mm_group(P, c, b, [(ring.v8[sa][:, k, oc * 128:(oc + 1) * 128], c.hT[:, k, 0:ncols]) for k in range(KC)],
                 [ring.bufs[sa], c.B_hT], ncols, name="w_in_a")
        P.op("act", lambda a, oc=oc, b=b: a.activation(out=c.ap_[:, oc, 0:ncols], in_=c.ps[:, b, 0:ncols], func=AF.Identity,
                                                       bias=c.vech[:, oc:oc + 1], scale=0.5),
             reads=[c.psb[b], c.B_const2], writes=[c.B_ap[oc]], name="aprime")
    for oc in range(KC):
        b = psbank(c)
        mm_group(P, c, b, [(ring.v8[sg][:, k, oc * 128:(oc + 1) * 128], c.hT[:, k, 0:ncols]) for k in range(KC)],
                 [ring.bufs[sg], c.B_hT], ncols, name="w_in_g")
        ti = c.th_rr
        c.th_rr = (c.th_rr + 1) % 3
        th = c.th[ti]
        P.op("act", lambda a, oc=oc, b=b, th=th: a.activation(out=th[:, 0:ncols], in_=c.ps[:, b, 0:ncols], func=AF.Tanh,
                                                              bias=c.vech[:, 8 + oc:9 + oc], scale=0.5),
             reads=[c.psb[b], c.B_const2], writes=[c.B_th[ti]], name="tanh")
        if not mask_halo:
            P.op("dve", lambda v, oc=oc, th=th: v.scalar_tensor_tensor(out=c.uT[:, oc, ucol0:ucol0 + ncols], in0=th[:, 0:ncols], scalar=1.0,
                                                                      in1=c.ap_[:, oc, 0:ncols], op0=ALU.add, op1=ALU.mult),
                 reads=[c.B_th[ti], c.B_ap[oc]], writes=[c.B_uT[oc]], name="glu")
        else:
            P.op("dve", lambda v, oc=oc, th=th: v.scalar_tensor_tensor(out=th[:, 0:ncols], in0=th[:, 0:ncols], scalar=1.0, in1=c.ap_[:, oc, 0:ncols], op0=ALU.add, op1=ALU.mult),
                 reads=[c.B_th[ti], c.B_ap[oc]], writes=[c.B_th[ti]], name="glu_h1")
            P.op("dve", lambda v, oc=oc, th=th: v.tensor_scalar(out=c.uT[:, oc, ucol0:ucol0 + ncols], in0=th[:, 0:ncols], scalar1=c.hmask_s[:, 0:1], scalar2=None, op0=ALU.mult),
                 reads=[c.B_th[ti], c.B_const], writes=[c.B_uT[oc]], name="glu_h2")


def conv_ln_stage(P, c):
    for k in range(KC):
        di = k % 2
        dg = c.diag

        def emd(g, k=k, di=di):
            r = None
            for j in range(CW):
                r = g.tensor_scalar(out=dg[:, di, j, :], in0=c.ident_b[:, :], scalar1=c.wdw_s[:, k, j:j + 1], scalar2=1.0, op0=ALU.mult, op1=ALU.mult)
            return r
        P.op("dve", emd, reads=[c.B_const, c.B_const2], writes=[c.B_diag[di]], name="diag")
        b = psbank(c)
        mm_group(P, c, b, [(dg[:, di, j, :], c.uT[:, k, 2 + j:2 + j + T]) for j in range(CW)], [c.B_diag[di], c.B_uT[k]], T, name="conv")
        P.op("act", lambda a, k=k, b=b: a.activation(out=c.v[:, k, :], in_=c.ps[:, b, :], func=AF.Identity, bias=c.vec[:, V_BDW + k:V_BDW + k + 1], scale=1.0),
             reads=[c.psb[b], c.B_const], writes=[c.B_v[k]], name="v_ev")
        P.op("act", lambda a, k=k, b=b: a.activation(out=c.sq[:, k, :], in_=c.ps[:, b, :], func=AF.Square, bias=c.vec[:, V_BDW + k:V_BDW + k + 1], scale=1.0),
             reads=[c.psb[b], c.B_const], writes=[c.B_sq], name="v_sq")
        P.op("pool", lambda g, k=k: g.tensor_copy(out=c.ap_[:, k, :], in_=c.v[:, k, :]), reads=[c.B_v[k]], writes=[c.B_ap[k]], name="vb")
    bm = psbank(c)
    mm_group(P, c, bm, [(c.ones_b[:, :], c.ap_[:, k, :]) for k in range(KC)], c.B_ap + [c.B_const2], T, name="ln_mean")
    be = psbank(c)
    mm_group(P, c, be, [(c.ones_b[:, :], c.sq[:, k, :]) for k in range(KC)], [c.B_sq, c.B_const2], T, name="ln_ex2")
    mean, m2, var, rstd, Bt = c.st[2], c.st[3], c.st[0], c.st[1], c.st[4]
    P.op("act", lambda a: a.copy(out=mean[:, :], in_=c.ps[:, bm, :]), reads=[c.psb[bm]], writes=[c.B_st[2]], name="mean")
    P.op("dve", lambda v: v.tensor_tensor(out=m2[:, :], in0=mean[:, :], in1=mean[:, :], op=ALU.mult), reads=[c.B_st[2]], writes=[c.B_st[3]], name="m2")
    P.op("dve", lambda v: v.scalar_tensor_tensor(out=var[:, :], in0=c.ps[:, be, :], scalar=EPS, in1=m2[:, :], op0=ALU.add, op1=ALU.subtract),
         reads=[c.psb[be], c.B_st[3]], writes=[c.B_st[0]], name="var")
    P.op("act", lambda a: a.activation(out=var[:, :], in_=var[:, :], func=AF.Ln), reads=[c.B_st[0]], writes=[c.B_st[0]], name="ln_ln")
    P.op("act", lambda a: a.activation(out=rstd[:, :], in_=var[:, :], func=AF.Exp, scale=-0.5), reads=[c.B_st[0]], writes=[c.B_st[1]], name="ln_rstd")
    P.op("dve", lambda v: v.scalar_tensor_tensor(out=Bt[:, :], in0=mean[:, :], scalar=-1.0, in1=rstd[:, :], op0=ALU.mult, op1=ALU.mult),
         reads=[c.B_st[2], c.B_st[1]], writes=[c.B_st[4]], name="ln_B")
    for k in range(KC):
        ti = c.th_rr
        c.th_rr = (c.th_rr + 1) % 3
        th = c.th[ti]
        P.op("pool", lambda g, k=k, th=th: g.tensor_tensor(out=th[:, :], in0=c.v[:, k, :], in1=rstd[:, :], op=ALU.mult),
             reads=[c.B_v[k], c.B_st[1]], writes=[c.B_th[ti]], name="ln_t1")
        P.op("dve", lambda v, th=th: v.tensor_tensor(out=th[:, :], in0=th[:, :], in1=Bt[:, :], op=ALU.add),
             reads=[c.B_th[ti], c.B_st[4]], writes=[c.B_th[ti]], name="ln_t2")
        P.op("act", lambda a, k=k, th=th: a.activation(out=c.sT[:, k, :], in_=th[:, :], func=AF.Silu, bias=c.vec[:, V_LNB + k:V_LNB + k + 1],
                                                       scale=c.vec[:, V_LNG + k:V_LNG + k + 1]),
             reads=[c.B_th[ti], c.B_const], writes=[c.B_sT], name="ln_silu")


def halo_shift(P, c):
    for k in range(KC):
        P.op("pool", lambda g, k=k: g.tensor_copy(out=c.uT[:, k, 2:32], in_=c.uT[:, k, 2 + T:32 + T]), reads=[c.B_uT[k]], writes=[c.B_uT[k]], name="halo")


def wout_stage(P, c):
    ring = c.ring
    s = ring.load(lambda i: [(ring.v8[i][:, :, :], w8(c.w_out_b, 0, 1024))], c.B_w["in"], name="w_out")
    for dc in range(KC):
        b = psbank(c)
        mm_group(P, c, b, [(ring.v8[s][:, k, dc * 128:(dc + 1) * 128], c.sT[:, k, :]) for k in range(KC)], [ring.bufs[s], c.B_sT], T, name="w_out")
        P.op("dve", lambda v, dc=dc, b=b: v.scalar_tensor_tensor(out=c.xT[:, dc, :], in0=c.ps[:, b, :], scalar=c.vec[:, V_BOUT + dc:V_BOUT + dc + 1],
                                                                in1=c.xT[:, dc, :], op0=ALU.add, op1=ALU.add),
             reads=[c.psb[b], c.B_xT[dc], c.B_const], writes=[c.B_xT[dc]], name="res1")


FGROUPS = [(0, 4), (4, 4), (8, 4), (12, 4), (16, 4), (20, 2)]
DHALF = [[(0, 8), (8, 4)], [(12, 8), (20, 2)]]


def ffn_stage(P, c, g_b, u_b, d_b, wbuf, hsrc, hbuf, gate=None):
    ring = c.ring
    for (f0, nf) in FGROUPS:
        s = ring.load(lambda i, f0=f0, nf=nf: [(ring.vgu[i][:, 0, :, 0:nf * 128], w8(g_b, f0 * 128, nf * 128)),
                                              (ring.vgu[i][:, 1, :, 0:nf * 128], w8(u_b, f0 * 128, nf * 128))], wbuf, name="w_gu")
        for j in range(nf):
            fc = f0 + j
            bg = psbank(c)
            mm_group(P, c, bg, [(ring.vgu[s][:, 0, k, j * 128:(j + 1) * 128], hsrc[:, k, :]) for k in range(KC)], [ring.bufs[s], hbuf], T, name="ffn_g")
            bu = psbank(c)
            mm_group(P, c, bu, [(ring.vgu[s][:, 1, k, j * 128:(j + 1) * 128], hsrc[:, k, :]) for k in range(KC)], [ring.bufs[s], hbuf], T, name="ffn_u")
            ti = c.th_rr
            c.th_rr = (c.th_rr + 1) % 3
            th = c.th[ti]
            P.op("act", lambda a, bg=bg, th=th: a.activation(out=th[:, :], in_=c.ps[:, bg, :], func=AF.Silu), reads=[c.psb[bg]], writes=[c.B_th[ti]], name="silu")
            P.op("dve", lambda v, bu=bu, th=th, fc=fc: v.tensor_tensor(out=c.aT[:, fc, :], in0=th[:, :], in1=c.ps[:, bu, :], op=ALU.mult),
                 reads=[c.B_th[ti], c.psb[bu]], writes=[c.B_aT[fc]], name="a_mul")
    for half in range(2):
        slots = []
        for (f0, nf) in DHALF[half]:
            s = ring.load(lambda i, f0=f0, nf=nf: [(ring.v8[i][:, 0:nf, :], d_b[f0 * 128:(f0 + nf) * 128, :].rearrange("(fc p) c -> p fc c", p=128))], wbuf, name="w_d")
            slots.append((s, f0, nf))
        for dc in range(KC):
            b = psbank(c)
            pairs = []
            reads = []
            for (s, f0, nf) in slots:
                reads.append(ring.bufs[s])
                for j in range(nf):
                    pairs.append((ring.v8[s][:, j, dc * 128:(dc + 1) * 128], c.aT[:, f0 + j, :]))
                    reads.append(c.B_aT[f0 + j])
            mm_group(P, c, b, pairs, reads, T, name="ffn_d")
            if gate is None:
                P.op("dve", lambda v, dc=dc, b=b: v.tensor_tensor(out=c.xT[:, dc, :], in0=c.ps[:, b, :], in1=c.xT[:, dc, :], op=ALU.add),
                     reads=[c.psb[b], c.B_xT[dc]], writes=[c.B_xT[dc]], name="res2")
            else:
                gt, gbuf = gate
                ti = c.th_rr
                c.th_rr = (c.th_rr + 1) % 3
                th = c.th[ti]
                P.op("dve", lambda v, b=b, th=th: v.tensor_tensor(out=th[:, :], in0=c.ps[:, b, :], in1=gt, op=ALU.mult),
                     reads=[c.psb[b], gbuf], writes=[c.B_th[ti]], name="gmul")
                P.op("pool", lambda g, dc=dc, th=th: g.tensor_tensor(out=c.xT[:, dc, :], in0=th[:, :], in1=c.xT[:, dc, :], op=ALU.add),
                     reads=[c.B_th[ti], c.B_xT[dc]], writes=[c.B_xT[dc]], name="res_moe")


def allgather(P, c, src, dst, bsrc, bdst):
    ds = P.new_dsem("cc")

    def em(g):
        return [g.collective_compute("AllGather", ALU.bypass, replica_groups=[[0, 1], [2, 3], [4, 5], [6, 7]],
                                     ins=[src.ap()], outs=[dst.ap()])]
    P.dma("pool", em, ds, 1, reads=[bsrc], writes=[bdst], name="allgather", inc=1)


def proj_stage(P, c, t):
    ring = c.ring
    t0 = t * T
    ri = rms_stats(P, c, T, c.B_xT)
    apply_norm(P, c, T, ri, V_KVN, c.hT, c.B_hT, name="hn_kv")
    apply_norm(P, c, T, ri, V_MIX1, c.sT, c.B_sT, name="hn_q")
    s = ring.load(lambda i: [(ring.v8[i][:, :, :], w8(c.w_kvf_b, 0, 1024))], c.B_w["att"], name="w_k")
    B_kst = Buf("kst")
    for cc in range(KC):
        b = psbank(c)
        mm_group(P, c, b, [(ring.v8[s][:, k, cc * 128:(cc + 1) * 128], c.hT[:, k, :]) for k in range(KC)], [ring.bufs[s], c.B_hT], T, name="k_mm")
        P.op("act", lambda a, cc=cc, b=b: a.copy(out=c.kst[:, cc, :], in_=c.ps[:, b, :]), reads=[c.psb[b]], writes=[c.B_aT[cc]], name="k_ev")
    P.dma("act", lambda e: [e.dma_start(out=c.gk[t][:, :].rearrange("(cc p) t -> p cc t", p=128), in_=c.kst[:, :, :])],
          c.ds_st[0], 1, reads=c.B_aT[0:8], writes=[c.B_gk[t]], name="k_st")
    allgather(P, c, c.gk[t], c.gko[t], c.B_gk[t], c.B_gko[t])
    s = ring.load(lambda i: [(ring.v8[i][:, :, :], w8(c.w_kvf_b, 1024, 1024))], c.B_w["att"], name="w_v")
    for tc in range(4):
        for hf in range(2):
            b = psbank(c)
            mm_group(P, c, b, [(c.hT[:, k, tc * 128:(tc + 1) * 128], ring.v8[s][:, k, hf * 512:(hf + 1) * 512]) for k in range(KC)],
                     [ring.bufs[s], c.B_hT], 512, name="v_mm")
            P.op("dve", lambda v, tc=tc, hf=hf, b=b: v.tensor_copy(out=c.vst[:, tc, hf * 512:(hf + 1) * 512], in_=c.ps[:, b, :]),
                 reads=[c.psb[b]], writes=[c.B_aT[8 + tc * 2 + hf]], name="v_ev")
    P.dma("act", lambda e: [e.dma_start(out=c.gv[t][:, :].rearrange("(tc p) d -> p tc d", p=128), in_=c.vst[:, :, :])],
          c.ds_st[1], 1, reads=c.B_aT[8:16], writes=[c.B_gv[t]], name="v_st")
    allgather(P, c, c.gv[t], c.gvo[t], c.B_gv[t], c.B_gvo[t])
    b = psbank(c)
    mm_group(P, c, b, [(c.wf[:, k, :], c.hT[:, k, :]) for k in range(KC)], [c.B_wf, c.B_hT], T, nrows=16, name="f_mm")
    P.op("act", lambda a, b=b: a.activation(out=c.st[5][0:16, :], in_=c.ps[0:16, b, :], func=AF.Identity, bias=c.bf_s[:, 0:1], scale=1.0),
         reads=[c.psb[b], c.B_const], writes=[c.B_st[5]], name="f_ev")
    P.dma("act", lambda e: [e.dma_start(out=c.flog[:, t0:t0 + T], in_=c.st[5][0:16, :])], c.ds_st[3], 1, reads=[c.B_st[5]], writes=[c.B_flog], name="f_st")
    s = ring.load(lambda i: [(ring.v8[i][:, :, :], w8(c.w_q_b, 0, 1024))], c.B_w["att"], name="w_q")
    for cc in range(KC):
        b = psbank(c)
        mm_group(P, c, b, [(ring.v8[s][:, k, cc * 128:(cc + 1) * 128], c.sT[:, k, :]) for k in range(KC)], [ring.bufs[s], c.B_sT], T, name="q_mm")
        P.op("act", lambda a, cc=cc, b=b: a.activation(out=c.kst[:, cc, :], in_=c.ps[:, b, :], func=AF.Copy, scale=0.125),
             reads=[c.psb[b]], writes=[c.B_aT[cc]], name="q_ev")
    P.dma("act", lambda e: [e.dma_start(out=c.qT[:, t0:t0 + T].rearrange("(cc p) t -> p cc t", p=128), in_=c.kst[:, :, :])],
          c.ds_st[2], 1, reads=c.B_aT[0:8], writes=[c.B_qT], name="q_st")


def phase_a(P, c, ntiles=NT, debug=None):
    P.dma("sp", lambda e: [e.dma_start(out=c.wf[:, :, :], in_=w8(c.w_kvf_b, 2048, 16))], P.new_dsem("wf"), 1, reads=[c.B_w["att"]], writes=[c.B_wf], name="wf_ld")
    load_x_and_transpose(P, c, c.xhalo[:, :], 32)
    ri = rms_stats(P, c, 32, c.B_xT)
    apply_norm(P, c, 32, ri, V_MIX0, c.hT, c.B_hT)
    glu_stage(P, c, 32, 0, mask_halo=True)
    for t in range(ntiles):
        t0 = t * T
        load_x_and_transpose(P, c, c.x[t0:t0 + T, :], T)
        ri = rms_stats(P, c, T, c.B_xT)
        apply_norm(P, c, T, ri, V_MIX0, c.hT, c.B_hT)
        glu_stage(P, c, T, 32)
        conv_ln_stage(P, c)
        halo_shift(P, c)
        wout_stage(P, c)
        if debug == "xa":
            P.dma("act", lambda e, t0=t0: [e.dma_start(out=c.x1T[:, :, t0:t0 + T], in_=c.xT[:, :, :])], c.ds_st[3], 1, reads=c.B_xT, writes=[c.B_x1T[t]], name="xa_st")
            continue
        ri = rms_stats(P, c, T, c.B_xT)
        apply_norm(P, c, T, ri, V_FFN0, c.hT, c.B_hT)
        ffn_stage(P, c, c.ffn_g_b, c.ffn_u_b, c.ffn_d_b, c.B_w["ffn"], c.hT, c.B_hT)
        P.dma("act", lambda e, t0=t0: [e.dma_start(out=c.x1T[:, :, t0:t0 + T], in_=c.xT[:, :, :])], c.ds_st[3], 1, reads=c.B_xT, writes=[c.B_x1T[t]], name="x1_st")
        proj_stage(P, c, t)
        if t < c.ne_decl:
            convert_expert(P, c, t)


NKB_HALF = 32
GRP = 3


def bcast_last(ap2d, n):
    a = [list(x) for x in ap2d.ap]
    return bass.AP(tensor=ap2d.tensor, offset=ap2d.offset, ap=a + [[0, n]])


def phase_b(P, c):
    base = c.sb_phase_base
    P.sb_off = base
    fl = P.sb("fl", [16, TOK], F32)
    ones = P.sb("ones16", [16, TOK], F32)
    C = P.sb("C", [16, TOK], F32)
    e1 = P.sb("e1", [16, TOK], F32)
    hs = [P.sb(f"h{i}", [16, TOK], BF16) for i in range(3)]
    rs_ = [P.sb(f"r{i}", [16, TOK], BF16) for i in range(3)]
    ns = [P.sb(f"n{i}", [16, TOK], BF16) for i in range(3)]
    oneb = P.sb("oneb", [16, TOK], BF16)
    zerob = P.sb("zerob", [16, TOK], BF16)
    B = {k: Buf("pb_" + k) for k in ("fl", "ones", "C", "e1", "h", "r", "n", "cb")}
    ds = P.new_dsem("pb")
    P.dma("sp", lambda e: [e.dma_start(out=fl[:, :], in_=c.flog[:, :])], ds, 1, reads=[c.B_flog], writes=[B["fl"]], name="fl_ld")

    def mk(v):
        v.memset(ones[:, :], 1.0)
        v.memset(oneb[:, :], 1.0)
        return v.memset(zerob[:, :], 0.0)
    P.op("dve", mk, writes=[B["ones"], B["cb"]], name="pb_const")
    P.op("act", lambda a: a.activation(out=fl[:, :], in_=fl[:, :], func=AF.Exp, scale=-1.0), reads=[B["fl"]], writes=[B["fl"]], name="exp_f")
    P.op("act", lambda a: a.activation(out=fl[:, :], in_=fl[:, :], func=AF.Ln, bias=1.0, scale=1.0), reads=[B["fl"]], writes=[B["fl"]], name="ln_f")
    P.op("dve", lambda v: v.tensor_tensor_scan(out=C[:, :], data0=ones[:, :], data1=fl[:, :], initial=0.0, op0=ALU.mult, op1=ALU.add),
         reads=[B["fl"], B["ones"]], writes=[B["C"]], name="scan")

    def split(src, outs, bsrc, bout, nm):
        bs = [Buf(nm + str(i)) for i in range(3)]
        P.op("dve", lambda v: v.tensor_copy(out=outs[0][:, :], in_=src[:, :]), reads=[bsrc], writes=[bs[0]], name=nm)
        P.op("dve", lambda v: v.tensor_tensor(out=e1[:, :], in0=src[:, :], in1=outs[0][:, :], op=ALU.subtract), reads=[bsrc, bs[0]], writes=[B["e1"]], name=nm)
        P.op("dve", lambda v: v.tensor_copy(out=outs[1][:, :], in_=e1[:, :]), reads=[B["e1"]], writes=[bs[1]], name=nm)
        P.op("dve", lambda v: v.tensor_tensor(out=e1[:, :], in0=e1[:, :], in1=outs[1][:, :], op=ALU.subtract), reads=[B["e1"], bs[1]], writes=[B["e1"]], name=nm)
        P.op("dve", lambda v: v.tensor_copy(out=outs[2][:, :], in_=e1[:, :]), reads=[B["e1"]] + bs[0:2], writes=[bout], name=nm)
    split(C, hs, B["C"], B["h"], "split_h")

    def neg(v):
        r = None
        for i in range(3):
            r = v.tensor_scalar(out=ns[i][:, :], in0=hs[i][:, :], scalar1=-1.0, scalar2=None, op0=ALU.mult)
        return r
    P.op("dve", neg, reads=[B["h"]], writes=[B["n"]], name="neg_h")
    P.op("dve", lambda v: v.tensor_scalar(out=fl[:, :], in0=C[:, :], scalar1=C[:, TOK - 1:TOK], scalar2=None, op0=ALU.subtract),
         reads=[B["C"], B["fl"]], writes=[B["fl"]], name="R")
    split(fl, rs_, B["fl"], B["r"], "split_r")
    c.B_kaug_own = Buf("kaug_own")
    c.B_qaug = Buf("qaug_d")
    gin_aug = c.ga[:, :].rearrange("(h r) t -> h r t", r=7)

    def st(e):
        r = []
        for i in range(3):
            r.append(e.dma_start(out=c.kaug_own[:, i, :], in_=oneb[:, :]))
            r.append(e.dma_start(out=c.kaug_own[:, 3 + i, :], in_=hs[i][:, :]))
            r.append(e.dma_start(out=gin_aug[:, i, :], in_=oneb[:, :]))
            r.append(e.dma_start(out=gin_aug[:, 3 + i, :], in_=rs_[i][:, :]))
            r.append(e.dma_start(out=c.qaug[:, i, :], in_=ns[i][:, :]))
            r.append(e.dma_start(out=c.qaug[:, 3 + i, :], in_=oneb[:, :]))
        r.append(e.dma_start(out=c.kaug_own[:, 6, :], in_=zerob[:, :]))
        r.append(e.dma_start(out=gin_aug[:, 6, :], in_=zerob[:, :]))
        r.append(e.dma_start(out=c.qaug[:, 6, :], in_=oneb[:, :]))
        return r
    P.dma("sp", st, ds, 21, reads=[B["h"], B["r"], B["n"], B["cb"]], writes=[c.B_kaug_own, c.B_ga, c.B_qaug], name="aug_st")


def phase_c(P, c):
    allgather(P, c, c.ga, c.gao, c.B_ga, c.B_gao)


def phase_d(P, c, nheads=H):
    nc = P.nc
    P.sb_off = c.sb_phase_base
    Kt = [P.sb(f"Kt{i}", [128, 2 * TOK], BF16) for i in range(2)]
    Vt = [P.sb(f"Vt{i}", [128, 2 * NKB_HALF, 65], BF16) for i in range(2)]
    Qt = [P.sb(f"Qt{i}", [128, TOK], BF16) for i in range(2)]
    PT = [P.sb(f"PT{i}", [128, GRP, 512], BF16) for i in range(3)]
    osb = [P.sb(f"osb{i}", [128, 512], BF16) for i in range(2)]
    rsb = [P.sb(f"rsb{i}", [128, 512], F32) for i in range(2)]
    cm = P.sb("cm", [128, 128], BF16)
    B_K = [Buf(f"Kt{i}") for i in range(2)]
    B_V = [Buf(f"Vt{i}") for i in range(2)]
    B_Q = [Buf(f"Qt{i}") for i in range(2)]
    B_PT = [Buf(f"PT{i}") for i in range(3)]
    B_osb = [Buf(f"osb{i}") for i in range(2)]
    B_cm = Buf("cm")
    ds_k = [P.new_dsem(f"k{i}") for i in range(2)]
    ds_o = [P.new_dsem(f"o{i}") for i in range(2)]
    ds_m = P.new_dsem("cm")
    c.B_oT = Buf("oT_d")
    c.B_rsum = Buf("rsum_d")
    P.dma("sp", lambda e: [e.dma_start(out=cm[:, :], in_=c.cmask[:, :])], ds_m, 1, writes=[B_cm], name="cm_ld")

    def ones_col(v):
        v.memset(Vt[0][:, :, 64:65], 1.0)
        return v.memset(Vt[1][:, :, 64:65], 1.0)
    P.op("dve", ones_col, writes=B_V, name="ones_col")

    pt_rr = 0
    o_rr = 0
    def load_head(h):
        bi = h % 2
        K, V, Q = Kt[bi], Vt[bi], Qt[bi]

        def ldk(e, h=h, K=K, V=V, Q=Q):
            r = []
            for t in range(NT):
                r.append(e.dma_start(out=K[0:64, t * T:(t + 1) * T], in_=c.gko[t][h * 64:(h + 1) * 64, :]))
                r.append(e.dma_start(out=K[0:64, TOK + t * T:TOK + (t + 1) * T], in_=c.gk[t][h * 64:(h + 1) * 64, :]))
                r.append(e.dma_start(out=V[:, 4 * t:4 * t + 4, 0:64], in_=c.gvo[t][0:T, h * 64:(h + 1) * 64].rearrange("(kb p) d -> p kb d", p=128)))
                r.append(e.dma_start(out=V[:, NKB_HALF + 4 * t:NKB_HALF + 4 * t + 4, 0:64], in_=c.gv[t][:, h * 64:(h + 1) * 64].rearrange("(kb p) d -> p kb d", p=128)))
            r.append(e.dma_start(out=K[64:70, 0:TOK], in_=c.gao[h * 7:h * 7 + 6, :]))
            r.append(e.dma_start(out=K[70:71, 0:TOK], in_=c.flagrow[:, :]))
            r.append(e.dma_start(out=K[64:71, TOK:2 * TOK], in_=c.kaug_own[h, :, :]))
            r.append(e.dma_start(out=Q[0:64, :], in_=c.qT[h * 64:(h + 1) * 64, :]))
            r.append(e.dma_start(out=Q[64:71, :], in_=c.qaug[h, :, :]))
            return r
        P.dma("sp", ldk, ds_k[bi], 4 * NT + 5, reads=c.B_gko + c.B_gk + c.B_gvo + c.B_gv + [c.B_gao, c.B_kaug_own, c.B_qT, c.B_qaug], writes=[B_K[bi], B_V[bi], B_Q[bi]], name="kvq_ld")

    load_head(0)
    for h in range(nheads):
        bi = h % 2
        K, V, Q = Kt[bi], Vt[bi], Qt[bi]
        if h + 1 < nheads:
            load_head(h + 1)

        for qb in range(NT):
            q0 = qb * 512
            blocks = [(kb, 0) for kb in range(NKB_HALF)] + [(NKB_HALF + j, 0) for j in range(4 * qb)]
            blocks += [(NKB_HALF + 4 * qb + i, 128 * i) for i in range(4)]
            groups = [blocks[i:i + GRP] for i in range(0, len(blocks), GRP)]
            ob = 6 + (o_rr % 2)
            oi = o_rr % 2
            o_rr += 1
            nblk = len(blocks)

            def emit_S(g, gi):
                sb = (gi % 2) * GRP

                def em(pe, g=g, sb=sb, K=K, Q=Q, q0=q0):
                    r = None
                    for j, (kb, c0) in enumerate(g):
                        r = pe.matmul(c.ps[:, sb + j, c0:512], K[0:71, kb * 128:(kb + 1) * 128], Q[0:71, q0 + c0:q0 + 512], start=True, stop=True)
                    return r
                P.op("pe", em, reads=[B_K[bi], B_Q[bi]], writes=[c.psb[sb + j] for j in range(len(g))], name="S")

            state = {"blk": 0}

            def emit_exp_pv(g, gi):
                nonlocal pt_rr
                sb = (gi % 2) * GRP
                pi = pt_rr % 3
                pt_rr += 1
                pt = PT[pi]
                ng = len(g)
                P.op("act", lambda a, sb=sb, ng=ng, pt=pt: a.activation(out=pt[:, 0:ng, :], in_=c.ps[:, sb:sb + ng, :], func=AF.Exp),
                     reads=[c.psb[sb + j] for j in range(ng)], writes=[B_PT[pi]], name="exp")
                for j, (kb, c0) in enumerate(g):
                    if kb >= NKB_HALF + 4 * qb:
                        P.op("pool", lambda gp, j=j, c0=c0, pt=pt: gp.tensor_tensor(out=pt[:, j, c0:c0 + 128], in0=pt[:, j, c0:c0 + 128], in1=cm[:, :], op=ALU.mult),
                             reads=[B_PT[pi], B_cm], writes=[B_PT[pi]], name="cmask")
                b0 = state["blk"]

                def em(pe, g=g, pt=pt, b0=b0, ob=ob, nblk=nblk, V=V):
                    r = None
                    for j, (kb, c0) in enumerate(g):
                        r = pe.matmul(c.ps[0:65, ob, c0:512], V[:, kb, 0:65], pt[:, j, c0:512], start=(b0 + j == 0), stop=(b0 + j == nblk - 1))
                    return r
                state["blk"] += ng
                P.op("pe", em, reads=[B_PT[pi], B_V[bi]], writes=[c.psb[ob]], name="PV")

            emit_S(groups[0], 0)
            for gi in range(len(groups)):
                if gi + 1 < len(groups):
                    emit_S(groups[gi + 1], gi + 1)
                emit_exp_pv(groups[gi], gi)
            def ev(v, ob=ob, oi=oi):
                v.tensor_copy(out=osb[oi][0:64, :], in_=c.ps[0:64, ob, :])
                return v.tensor_copy(out=rsb[oi][64:65, :], in_=c.ps[64:65, ob, :])
            P.op("dve", ev, reads=[c.psb[ob]], writes=[B_osb[oi]], name="o_ev")
            P.dma("pool", lambda e, h=h, q0=q0, oi=oi: [e.dma_start(out=c.oT[h * 64:(h + 1) * 64, q0:q0 + 512], in_=osb[oi][0:64, :]),
                                                      e.dma_start(out=c.rsum[h:h + 1, q0:q0 + 512], in_=rsb[oi][64:65, :])],
                  ds_o[oi], 2, reads=[B_osb[oi]], writes=[c.B_oT, c.B_rsum], name="o_st")


def phase_e(P, c, ntiles=NT, nexp=NE):
    nc = P.nc
    P.sb_off = c.sb_phase_base
    ring = Ring(P, 4, name="ringe")
    c.ring = ring
    c.xT = P.sb("xTe", [128, KC, T], F32)
    oTt = P.sb("oTt", [128, KC, T], BF16)
    rbc = P.sb("rbc", [128, KC, T], F32)
    ytok = P.sb("ytok", [128, 4, D], F32, off=P.sb_off - 16384)
    hf = P.sb("hf", [128, KC, T], F32)
    c.hT = P.sb("hTe", [128, KC, T], BF16)
    c.sq = P.sb("sqe", [128, KC, T], BF16)
    Gs = P.sb("Gs", [128, NE, T], F32)
    c.aT = P.sb("aTe", [128, FC, T], BF16)
    c.th = [P.sb(f"the{i}", [128, T], F32) for i in range(3)]
    c.st = [P.sb(f"ste{i}", [128, T], F32) for i in range(6)]
    rw_s = P.sb("rw_s", [128, KC, NE], F32)
    rb_s = P.sb("rb_s", [128, 4, NE], F32)
    sel_s = P.sb("sel_s", [128, NE, 128], F32)
    lg = P.sb("lg", [128, 4, NE], F32)
    lg2 = P.sb("lg2", [128, 4, NE], F32)
    eq1 = P.sb("eq1", [128, 4, NE], F32)
    eq2 = P.sb("eq2", [128, 4, NE], F32)
    gt = P.sb("gt", [128, 4, NE], F32)
    m1 = P.sb("m1", [128, 4], F32)
    m2 = P.sb("m2", [128, 4], F32)
    p1 = P.sb("p1", [128, 4], F32)
    p2 = P.sb("p2", [128, 4], F32)
    gT_s = P.sb("gT_s", [128, T], F32)
    gT_p = P.sb("gT_p", [8, T], F32)
    print("SBUF used phase E:", P.sb_off)
    c.B_xT = [Buf(f"xTe{k}") for k in range(KC)]
    c.B_hT, c.B_sq = Buf("hTe"), Buf("sqe")
    c.B_aT = [Buf(f"aTe{k}") for k in range(FC)]
    c.B_th = [Buf(f"the{i}") for i in range(3)]
    c.B_st = [Buf(f"ste{i}") for i in range(6)]
    B_oTt, B_rbc, B_rt = Buf("oTt"), Buf("rbc"), Buf("rt")
    B_hf = [Buf("hf")] * KC
    B_G = [Buf(f"G{e}") for e in range(NE)]
    B_gT, B_gTp = Buf("gT"), Buf("gTp")
    B_rc = Buf("rconst")
    ds_in = [P.new_dsem(f"ein{i}") for i in range(3)]
    ds_m = [P.new_dsem(f"em{i}") for i in range(3)]
    ds_h = [P.new_dsem(f"eh{i}") for i in range(3)]
    ds_out = P.new_dsem("eout")
    B_out = Buf("out_d")
    B_x2d = [Buf(f"x2d{t}") for t in range(ntiles)]
    B_hd = [Buf(f"hd{t}") for t in range(ntiles)]
    B_gd = [Buf(f"gd{t}") for t in range(ntiles)]
    x2T_d = nc.dram_tensor("x2T_d", [128, KC, TOK], F32)
    hT_d = nc.dram_tensor("hT_d", [128, KC, TOK], BF16)
    gT_d = nc.dram_tensor("gT_d", [8, TOK], F32)
    cp = Ctx()
    cp.__dict__.update(c.__dict__)
    cp.xT = hf
    cp.B_xT = B_hf
    P.dma("sp", lambda e: [e.dma_start(out=rw_s[:, :, :], in_=c.rw[:, :, :]), e.dma_start(out=rb_s[:, :, :], in_=c.rb4[:, :, :]),
                           e.dma_start(out=sel_s[:, :, :], in_=c.sel[:, :, :])], P.new_dsem("rconst"), 3, writes=[B_rc], name="rconst_ld")
    P.op("dve", lambda v: v.memset(gT_s[:, :], 0.0), writes=[B_gT], name="gT_zero")
    X = mybir.AxisListType.X

    def prologue(t):
        t0 = t * T
        P.dma("sp", lambda e: [e.dma_start(out=hf[:, :, :], in_=c.x1T[:, :, t0:t0 + T])], ds_in[0], 1, reads=c.B_x1T, writes=B_hf, name="x1_ld")
        P.dma("sp", lambda e: [e.dma_start(out=oTt[:, :, :], in_=c.oT[:, t0:t0 + T].rearrange("(cc p) t -> p cc t", p=128))], ds_in[1], 1,
              reads=[c.B_oT], writes=[B_oTt], name="oT_ld")

        def ldr(e):
            r = []
            for hh in range(2):
                base = c.rsum[hh:hh + 1, t0:t0 + T]
                src = bass.AP(tensor=base.tensor, offset=base.offset, ap=[[0, 64], [2 * TOK, 8], [1, T]])
                r.append(e.dma_start(out=rbc[hh * 64:(hh + 1) * 64, :, :], in_=src))
            return r
        P.dma("sp", ldr, ds_in[2], 2, reads=[c.B_rsum], writes=[B_rbc], name="rsum_ld")
        P.op("dve", lambda v: v.reciprocal(out=rbc[:, :, :], in_=rbc[:, :, :]), reads=[B_rbc], writes=[B_rbc], name="recip")
        P.op("pool", lambda g: g.tensor_tensor(out=oTt[:, :, :], in0=oTt[:, :, :], in1=rbc[:, :, :], op=ALU.mult), reads=[B_rbc, B_oTt], writes=[B_oTt], name="o_norm")
        yield
        s_ = ring.load(lambda i: [(ring.v8[i][:, :, :], w8(c.w_o_b, 0, 1024))], c.B_w["att"], name="w_o")
        for dc in range(KC):
            b = psbank(c)
            mm_group(P, c, b, [(ring.v8[s_][:, k, dc * 128:(dc + 1) * 128], oTt[:, k, :]) for k in range(KC)], [ring.bufs[s_], B_oTt], T, name="w_o")
            P.op("dve", lambda v, dc=dc, b=b: v.tensor_tensor(out=hf[:, dc, :], in0=c.ps[:, b, :], in1=hf[:, dc, :], op=ALU.add),
                 reads=[c.psb[b], B_hf[dc]], writes=[B_hf[dc]], name="res_att")
        if c.debug == "x2":
            P.dma("act", lambda e: [e.dma_start(out=c.dbg_x2T[:, :, t0:t0 + T], in_=hf[:, :, :])], ds_out, 1, reads=B_hf, writes=[B_out], name="x2_st")
        P.dma("act", lambda e: [e.dma_start(out=x2T_d[:, :, t0:t0 + T], in_=hf[:, :, :])], ds_h[0], 1, reads=B_hf, writes=[B_x2d[t]], name="x2d_st")
        yield
        ri = rms_stats(P, cp, T, B_hf)
        apply_norm(P, cp, T, ri, V_FFN1, rbc, B_rbc, name="hf")
        P.op("act", lambda a: a.copy(out=oTt[:, :, :], in_=rbc[:, :, :]), reads=[B_rbc], writes=[B_oTt], name="hT_cast")
        P.dma("act", lambda e: [e.dma_start(out=hT_d[:, :, t0:t0 + T], in_=oTt[:, :, :])], ds_h[1], 1, reads=[B_oTt], writes=[B_hd[t]], name="hd_st")
        yield
        b = psbank(c)

        def emr(pe, b=b):
            r = None
            for tc in range(4):
                for k in range(KC):
                    r = pe.matmul(c.ps[:, b, tc * 8:(tc + 1) * 8], rbc[:, k, tc * 128:(tc + 1) * 128], rw_s[:, k, :], start=(k == 0), stop=(k == KC - 1))
            return r
        P.op("pe", emr, reads=[B_rbc, B_rc], writes=[c.psb[b]], name="router_mm")
        B_s = {k: Buf("rt_" + k) for k in ("lg", "m1", "eq1", "lg2", "m2", "eq2", "p2", "p1", "gt")}
        P.op("dve", lambda v, b=b: v.tensor_tensor(out=lg[:, :, :], in0=c.ps[:, b, 0:32].rearrange("p (a e) -> p a e", e=NE), in1=rb_s[:, :, :], op=ALU.add),
             reads=[c.psb[b], B_rc, B_rt], writes=[B_s["lg"]], name="lg")
        P.op("dve", lambda v: v.tensor_reduce(out=m1[:, :], in_=lg[:, :, :], axis=X, op=ALU.max), reads=[B_s["lg"]], writes=[B_s["m1"]], name="m1")
        P.op("dve", lambda v: v.tensor_tensor(out=eq1[:, :, :], in0=lg[:, :, :], in1=bcast_last(m1[:, :], NE), op=ALU.is_equal),
             reads=[B_s["lg"], B_s["m1"]], writes=[B_s["eq1"]], name="eq1")
        P.op("dve", lambda v: v.scalar_tensor_tensor(out=lg2[:, :, :], in0=eq1[:, :, :], scalar=-1e30, in1=lg[:, :, :], op0=ALU.mult, op1=ALU.add),
             reads=[B_s["eq1"], B_s["lg"]], writes=[B_s["lg2"]], name="lg2")
        P.op("dve", lambda v: v.tensor_reduce(out=m2[:, :], in_=lg2[:, :, :], axis=X, op=ALU.max), reads=[B_s["lg2"]], writes=[B_s["m2"]], name="m2")
        P.op("dve", lambda v: v.tensor_tensor(out=eq2[:, :, :], in0=lg2[:, :, :], in1=bcast_last(m2[:, :], NE), op=ALU.is_equal),
             reads=[B_s["lg2"], B_s["m2"]], writes=[B_s["eq2"]], name="eq2")
        P.op("dve", lambda v: v.tensor_tensor(out=p2[:, :], in0=m2[:, :], in1=m1[:, :], op=ALU.subtract), reads=[B_s["m1"], B_s["m2"]], writes=[B_s["p2"]], name="d21")
        P.op("act", lambda a: a.activation(out=p2[:, :], in_=p2[:, :], func=AF.Tanh, scale=0.5), reads=[B_s["p2"]], writes=[B_s["p2"]], name="tanh_r")
        P.op("dve", lambda v: v.tensor_scalar(out=p2[:, :], in0=p2[:, :], scalar1=0.5, scalar2=0.5, op0=ALU.mult, op1=ALU.add), reads=[B_s["p2"]], writes=[B_s["p2"]], name="p2")
        P.op("dve", lambda v: v.tensor_scalar(out=p1[:, :], in0=p2[:, :], scalar1=-1.0, scalar2=1.0, op0=ALU.mult, op1=ALU.add), reads=[B_s["p2"]], writes=[B_s["p1"]], name="p1")
        P.op("dve", lambda v: v.tensor_tensor(out=gt[:, :, :], in0=eq1[:, :, :], in1=bcast_last(p1[:, :], NE), op=ALU.mult), reads=[B_s["eq1"], B_s["p1"]], writes=[B_s["gt"]], name="g1")
        P.op("dve", lambda v: v.tensor_tensor(out=eq2[:, :, :], in0=eq2[:, :, :], in1=bcast_last(p2[:, :], NE), op=ALU.mult), reads=[B_s["eq2"], B_s["p2"]], writes=[B_s["eq2"]], name="g2")
        P.op("dve", lambda v: v.tensor_tensor(out=gt[:, :, :], in0=gt[:, :, :], in1=eq2[:, :, :], op=ALU.add), reads=[B_s["gt"], B_s["eq2"]], writes=[B_s["gt"], B_rt], name="gates")
        yield
        b = psbank(c)

        def emgt(pe, b=b):
            r = None
            for tc in range(4):
                r = pe.transpose(out=c.ps[0:8, b, tc * 128:(tc + 1) * 128], in_=gt[:, tc, :], identity=c.ident_f[:, :])
            return r
        P.op("pe", emgt, reads=[B_rt, c.B_const], writes=[c.psb[b]], name="gT")
        P.op("act", lambda a, b=b: a.copy(out=gT_p[0:8, :], in_=c.ps[0:8, b, :]), reads=[c.psb[b]], writes=[B_gTp], name="gT_ev")
        P.dma("act", lambda e: [e.dma_start(out=gT_d[:, t0:t0 + T], in_=gT_p[0:8, :])], ds_h[2], 1, reads=[B_gTp], writes=[B_gd[t]], name="gd_st")
        yield

    def run_all(gen):
        for _ in gen:
            pass

    run_all(prologue(0))
    for t in range(ntiles):
        t0 = t * T
        nxt = prologue(t + 1) if t + 1 < ntiles else None
        P.dma("sp", lambda e, t0=t0: [e.dma_start(out=c.xT[:, :, :], in_=x2T_d[:, :, t0:t0 + T])], ds_m[0], 1, reads=[B_x2d[t]], writes=c.B_xT, name="x2_ld")
        P.dma("sp", lambda e, t0=t0: [e.dma_start(out=c.hT[:, :, :], in_=hT_d[:, :, t0:t0 + T])], ds_m[1], 1, reads=[B_hd[t]], writes=[c.B_hT], name="h_ld")
        P.dma("sp", lambda e, t0=t0: [e.dma_start(out=gT_s[0:8, :], in_=gT_d[:, t0:t0 + T])], ds_m[2], 1, reads=[B_gd[t]], writes=[B_gT], name="g_ld")
        for e in range(NE):
            b = psbank(c)
            P.op("pe", lambda pe, b=b, e=e: pe.matmul(c.ps[:, b, :], sel_s[:, e, :], gT_s[:, :], start=True, stop=True),
                 reads=[B_gT, B_rc], writes=[c.psb[b]], name="G_bc")
            if e % 2 == 0:
                P.op("act", lambda a, b=b, e=e: a.copy(out=Gs[:, e, :], in_=c.ps[:, b, :]), reads=[c.psb[b]], writes=[B_G[e]], name="G_ev")
            else:
                P.op("dve", lambda v, b=b, e=e: v.tensor_copy(out=Gs[:, e, :], in_=c.ps[:, b, :]), reads=[c.psb[b]], writes=[B_G[e]], name="G_ev")
        if c.debug == "x2":
            if nxt is not None:
                run_all(nxt)
            continue
        for e in range(nexp):
            ffn_stage(P, c, c.moe_g_b[e], c.moe_u_b[e], c.moe_d_b[e], c.B_w[f"e{e}"], c.hT, c.B_hT, gate=(Gs[:, e, :], B_G[e]))
            if nxt is not None and 1 <= e <= 5:
                next(nxt)
        if nxt is not None and nexp < 6:
            run_all(nxt)
        if c.debug == "x3":
            P.dma("act", lambda e, t0=t0: [e.dma_start(out=c.dbg_x2T[:, :, t0:t0 + T], in_=c.xT[:, :, :])], ds_out, 1, reads=c.B_xT, writes=[B_out], name="x3_st")
            continue
        ri = rms_stats(P, c, T, c.B_xT)
        B_y = B_hf[0]
        apply_norm(P, c, T, ri, V_FIN, hf, B_y, name="yT")
        for tc in range(4):
            for kh in range(2):
                b = psbank(c)

                def emt(pe, tc=tc, kh=kh, b=b):
                    r = None
                    for kk in range(4):
                        k = kh * 4 + kk
                        r = pe.transpose(out=c.ps[:, b, kk * 128:(kk + 1) * 128], in_=hf[:, k, tc * 128:(tc + 1) * 128], identity=c.ident_f[:, :])
                    return r
                P.op("pe", emt, reads=[B_y, c.B_const], writes=[c.psb[b]], name="y_xpose")
                if kh == 0:
                    P.op("act", lambda a, tc=tc, kh=kh, b=b: a.copy(out=ytok[:, tc, kh * 512:(kh + 1) * 512], in_=c.ps[:, b, :]), reads=[c.psb[b]], writes=[B_rbc], name="y_ev")
                else:
                    P.op("dve", lambda v, tc=tc, kh=kh, b=b: v.tensor_copy(out=ytok[:, tc, kh * 512:(kh + 1) * 512], in_=c.ps[:, b, :]), reads=[c.psb[b]], writes=[B_rbc], name="y_ev")
        P.dma("act", lambda e, t0=t0: [e.dma_start(out=c.out[t0:t0 + T, :].rearrange("(tc p) d -> p tc d", p=128), in_=ytok[:, :, :])], ds_out, 1,
              reads=[B_rbc], writes=[B_out], name="out_st")


def host_prep(inp):
    f32 = np.float32
    x = np.asarray(inp["x"], f32)

    def pv(v):
        return np.ascontiguousarray(np.asarray(v, f32).reshape(8, 128).T)
    b_in = np.asarray(inp["conv_b_in"], f32)[0]
    vecs = np.concatenate([
        pv(inp["mix_norm"][0]), pv(inp["ffn_norm"][0]), pv(b_in[:1024]), pv(b_in[1024:]),
        pv(inp["conv_b_dw"][0]), pv(inp["conv_ln_g"][0]), pv(inp["conv_ln_b"][0]), pv(inp["conv_b_out"][0]),
        pv(inp["kv_norm"]), pv(inp["mix_norm"][1]), pv(inp["ffn_norm"][1]), pv(inp["final_norm"])], axis=1)
    wdw = np.ascontiguousarray(np.asarray(inp["conv_w_dw"], f32)[0].reshape(31, 8, 128).transpose(2, 1, 0))
    bf = np.asarray(inp["b_f"], f32).reshape(16, 1)
    rw = np.ascontiguousarray(np.asarray(inp["router_w"], f32)[0].reshape(8, 128, 8).transpose(1, 0, 2))
    rb4 = np.ascontiguousarray(np.broadcast_to(np.asarray(inp["router_b"], f32)[0][None, None, :], (128, 4, 8)))
    sel = np.zeros((128, 8, 128), f32)
    for e in range(8):
        sel[e, e, :] = 1.0
    ident = np.eye(128, dtype=f32)
    cmask = (np.arange(128)[:, None] <= np.arange(128)[None, :]).astype(ml_dtypes.bfloat16)
    common = dict(vecs=vecs, wdw=wdw, bf=bf, rw=rw, rb4=rb4, sel=sel, ident=ident, cmask=cmask,
                  conv_w_in=np.asarray(inp["conv_w_in"], f32)[0], conv_w_out=np.asarray(inp["conv_w_out"], f32)[0],
                  ffn_w_gate=np.asarray(inp["ffn_w_gate"], f32)[0], ffn_w_up=np.asarray(inp["ffn_w_up"], f32)[0],
                  ffn_w_down=np.asarray(inp["ffn_w_down"], f32)[0], w_kvf=np.asarray(inp["w_kvf"], f32),
                  w_q=np.asarray(inp["w_q"], f32)[0], w_o=np.asarray(inp["w_o"], f32)[0],
                  moe_w_gate=np.asarray(inp["moe_w_gate"], f32)[0], moe_w_up=np.asarray(inp["moe_w_up"], f32)[0],
                  moe_w_down=np.asarray(inp["moe_w_down"], f32)[0])
    maps = []
    for core in range(8):
        b, hf = core // 2, core % 2
        m = dict(common)
        m["x"] = np.ascontiguousarray(x[b, hf * 4096:(hf + 1) * 4096])
        if hf == 0:
            m["xhalo"] = np.zeros((32, 1024), f32)
            m["hmask"] = np.zeros((128, 1), f32)
            m["flagrow"] = np.full((1, 4096), -30000.0, dtype=ml_dtypes.bfloat16)
        else:
            m["xhalo"] = np.ascontiguousarray(x[b, 4096 - 32:4096])
            m["hmask"] = np.ones((128, 1), f32)
            m["flagrow"] = np.zeros((1, 4096), dtype=ml_dtypes.bfloat16)
        maps.append(m)
    return maps


def build(debug=None, ntiles=NT, ne_decl=NE, stop_after=None):
    nc = bass.Bass("TRN2", target_bir_lowering=False)
    c = Ctx()
    declare_io(nc, c, debug, ne_decl)
    P = Prog(nc)
    setup_common(P, c)
    convert_weights(P, c)
    c.sb_phase_base = P.sb_off
    alloc_phase_a(P, c)
    print("SBUF used after phase A alloc:", P.sb_off)
    phase_a(P, c, ntiles=ntiles, debug=debug)
    if debug not in ("xa", "a"):
        P.barrier()
        phase_b(P, c)
        if stop_after != "b":
            phase_c(P, c)
        P.barrier()
        if stop_after in ("b", "c"):
            dk = nc.dram_tensor("dbg_kaug", [H, 7, TOK], BF16, kind="ExternalOutput")
            dq = nc.dram_tensor("dbg_qaug", [H, 7, TOK], BF16, kind="ExternalOutput")
            dgi = nc.dram_tensor("dbg_gin", [112, TOK], BF16, kind="ExternalOutput")
            ds = P.new_dsem("dbg")
            P.dma("sp", lambda e: [e.dma_start(out=dk[:, :, :], in_=c.kaug_own[:, :, :]),
                                   e.dma_start(out=dq[:, :, :], in_=c.qaug[:, :, :]), e.dma_start(out=dgi[:, :], in_=c.ga[:, :])], ds, 3, name="dbg_out")
        else:
            phase_d(P, c)
            P.barrier()
            if stop_after == "d":
                do = nc.dram_tensor("dbg_oT", [D, TOK], BF16, kind="ExternalOutput")
                dr = nc.dram_tensor("dbg_rsum", [H, TOK], F32, kind="ExternalOutput")
                ds = P.new_dsem("dbg")
                P.dma("sp", lambda e: [e.dma_start(out=do[:, :], in_=c.oT[:, :]), e.dma_start(out=dr[:, :], in_=c.rsum[:, :])], ds, 2, name="dbg_out")
            else:
                ce = Ctx()
                ce.__dict__.update(c.__dict__)
                phase_e(P, ce, nexp=ne_decl)
    if debug in ("xa", "a"):
        c.dbg_x1T = nc.dram_tensor("dbg_x1T", [128, KC, TOK], F32, kind="ExternalOutput")
        c.dbg_gin = nc.dram_tensor("dbg_gin", [2160, TOK], BF16, kind="ExternalOutput")
        c.dbg_qT = nc.dram_tensor("dbg_qT", [D, TOK], BF16, kind="ExternalOutput")
        c.dbg_flog = nc.dram_tensor("dbg_flog", [16, TOK], F32, kind="ExternalOutput")
        ds = P.new_dsem("dbg")
        P.dma("sp", lambda e: [e.dma_start(out=c.dbg_x1T[:, :, :], in_=c.x1T[:, :, :]),
                               e.dma_start(out=c.dbg_gin[:, :], in_=c.gin[:, :]),
                               e.dma_start(out=c.dbg_qT[:, :], in_=c.qT[:, :]),
                               e.dma_start(out=c.dbg_flog[:, :], in_=c.flog[:, :])], ds, 4,
              reads=c.B_x1T + [c.B_gin, c.B_qT, c.B_flog], name="dbg_out")
    stats = P.emit_all()
    print("ops per engine (n, waits):", stats)
    return nc


def kernel(**inputs):
    maps = host_prep(inputs)
    nc = build(debug=None)
    res = run_bass_kernel_spmd(nc, maps, core_ids=list(range(8)))
    out = np.empty((4, 8192, 1024), np.float32)
    for core in range(8):
        out[core // 2, (core % 2) * 4096:(core % 2 + 1) * 4096] = res.results[core]["out"]
    return out
```

```python
import numpy as np
import ml_dtypes
import concourse.bass as bass
import concourse.mybir as mybir
from concourse.bass_utils import run_bass_kernel_spmd


F32 = mybir.dt.float32
BF16 = mybir.dt.bfloat16
AF = mybir.ActivationFunctionType
ALU = mybir.AluOpType

ENGS = ("pe", "act", "dve", "pool", "sp")


class Buf:
    __slots__ = ("name", "last_w", "readers", "dsem")

    def __init__(self, name):
        self.name = name
        self.last_w = None
        self.readers = []
        self.dsem = None


class Op:
    __slots__ = ("eng", "emit", "deps", "is_dma", "needs_inc", "semval", "dsem", "dval", "npieces", "idx", "name", "inc", "hoist")

    def __init__(self, eng, emit, is_dma=False, name=""):
        self.eng = eng
        self.emit = emit
        self.deps = []
        self.is_dma = is_dma
        self.needs_inc = False
        self.semval = None
        self.dsem = None
        self.dval = None
        self.npieces = 0
        self.name = name
        self.hoist = False


class DSem:
    __slots__ = ("sem", "total", "last_op")

    def __init__(self, sem):
        self.sem = sem
        self.total = 0
        self.last_op = None


class Prog:
    def __init__(self, nc):
        self.nc = nc
        self.ops = {e: [] for e in ENGS}
        self.all_ops = []
        self.esem = {}
        self.dsems = []
        self.pending_barrier = {e: [] for e in ENGS}
        self.sb_off = 16512
        self.sb_end = 229344
        self._n = 0

    def sb(self, name, shape, dtype, off=None):
        nbytes = int(np.prod(shape[1:])) * (2 if dtype == BF16 else 4)
        if off is None:
            off = (self.sb_off + 63) // 64 * 64
            self.sb_off = off + nbytes
            assert self.sb_off <= self.sb_end, f"SBUF overflow at {name}: {self.sb_off}"
        self._n += 1
        return self.nc.alloc_sbuf_tensor_at(f"{name}_{self._n}", list(shape), dtype, offset=off)

    def new_dsem(self, name):
        d = DSem(self.nc.alloc_semaphore(f"d_{name}_{len(self.dsems)}"))
        self.dsems.append(d)
        return d

    def _add(self, op, reads, writes):
        deps = []
        for b in reads:
            if b.last_w is not None:
                deps.append(b.last_w)
        for b in writes:
            if b.last_w is not None:
                deps.append(b.last_w)
            deps.extend(b.readers)
        deps.extend(self.pending_barrier[op.eng])
        self.pending_barrier[op.eng] = []
        seen = set()
        for d in deps:
            if d is op or id(d) in seen:
                continue
            seen.add(id(d))
            op.deps.append(d)
            if not d.is_dma:
                d.needs_inc = True
        for b in reads:
            b.readers.append(op)
        for b in writes:
            b.last_w = op
            b.readers = []
        op.idx = len(self.all_ops)
        self.ops[op.eng].append(op)
        self.all_ops.append(op)
        return op

    def op(self, eng, emit, reads=(), writes=(), name=""):
        return self._add(Op(eng, emit, False, name), list(reads), list(writes))

    def dma(self, q, emit, dsem, npieces, reads=(), writes=(), name="", inc=16):
        o = Op(q, emit, True, name)
        o.dsem = dsem
        o.npieces = npieces
        o.inc = inc
        if dsem.last_op is not None:
            o.deps.append(dsem.last_op)
        dsem.total += inc * npieces
        o.dval = dsem.total
        dsem.last_op = o
        return self._add(o, list(reads), list(writes))

    def barrier(self):
        lasts = []
        for e in ENGS:
            if self.ops[e]:
                for o in reversed(self.ops[e]):
                    if not o.is_dma:
                        lasts.append(o)
                        break
        for d in self.dsems:
            if d.last_op is not None:
                lasts.append(d.last_op)
        for e in ENGS:
            self.pending_barrier[e] = list(lasts)

    def emit_all(self, final_waits_eng="sp"):
        nc = self.nc
        for e in ("pe", "act", "dve", "pool"):
            self.esem[e] = nc.alloc_semaphore(f"e_{e}")
        for e in ENGS:
            cnt = 0
            for o in self.ops[e]:
                if o.is_dma:
                    continue
                if o.needs_inc:
                    cnt += 1
                    o.semval = cnt
        peidx = {id(o): i for i, o in enumerate(self.ops["pe"])}
        maxpe = {}
        last = {e: -1 for e in ENGS}
        for o in self.all_ops:
            m = last[o.eng]
            if o.eng == "pe":
                m = max(m, peidx[id(o)])
            for d in o.deps:
                m = max(m, maxpe[id(d)])
            maxpe[id(o)] = m
            last[o.eng] = m
        stats = {}
        with nc.Block() as block:
            def wait_list(e):
                waited = {}
                out = []
                for o in self.ops[e]:
                    ws = []
                    for d in o.deps:
                        if d.is_dma:
                            key, sem, val = id(d.dsem), d.dsem.sem, d.dval
                        else:
                            if d.eng == e and e == "pe":
                                continue
                            key, sem, val = d.eng, self.esem[d.eng], d.semval
                        if waited.get(key, 0) >= val:
                            continue
                        waited[key] = val
                        ws.append((sem, val, maxpe[id(d)]))
                    out.append(ws)
                return out, waited

            def run(e):
                def body(eng):
                    W, waited = wait_list(e)
                    ops = self.ops[e]
                    if e == "pe":
                        for j in range(1, len(ops)):
                            if not ops[j].hoist:
                                continue
                            keep = []
                            for w in W[j]:
                                if w[2] <= j - 2:
                                    W[j - 1].append(w)
                                else:
                                    keep.append(w)
                            W[j] = keep
                    nw = 0
                    for o, ws in zip(ops, W):
                        for (sem, val, _) in ws:
                            eng.wait_ge(sem, val)
                            nw += 1
                        r = o.emit(eng)
                        if o.is_dma:
                            assert len(r) == o.npieces, (o.name, len(r), o.npieces)
                            for ins in r:
                                ins.then_inc(o.dsem.sem, o.inc)
                        elif o.needs_inc:
                            r.then_inc(self.esem[e], 1)
                    if e == final_waits_eng:
                        for d in self.dsems:
                            if d.total and waited.get(id(d), 0) < d.total:
                                eng.wait_ge(d.sem, d.total)
                    stats[e] = (len(ops), nw)
                return body
            block.tensor(run("pe"))
            block.scalar(run("act"))
            block.vector(run("dve"))
            block.gpsimd(run("pool"))
            block.sync(run("sp"))
        return stats


D = 1024
KC = 8
T = 512
TOK = 4096
NT = TOK // T
FF = 2816
FC = 22
H = 16
CW = 31
NE = 8
EPS = 1e-6

V_MIX0, V_FFN0, V_BA, V_BG, V_BDW, V_LNG, V_LNB, V_BOUT, V_KVN, V_MIX1, V_FFN1, V_FIN = [8 * i for i in range(12)]
NV = 96


class Ctx:
    pass


def declare_io(nc, c, debug, ne_decl=NE):
    c.ne_decl = ne_decl
    def din(name, shape, dt=F32):
        return nc.dram_tensor(name, list(shape), dt, kind="ExternalInput")

    c.x = din("x", [TOK, D])
    c.xhalo = din("xhalo", [32, D])
    c.vecs = din("vecs", [128, NV])
    c.wdw = din("wdw", [128, KC, CW])
    c.bf = din("bf", [16, 1])
    c.hmask = din("hmask", [128, 1])
    c.flagrow = din("flagrow", [1, TOK], BF16)
    c.rw = din("rw", [128, KC, NE])
    c.rb4 = din("rb4", [128, 4, NE])
    c.sel = din("sel", [128, NE, 128])
    c.ident = din("ident", [128, 128])
    c.cmask = din("cmask", [128, 128], BF16)
    c.w_in = din("conv_w_in", [D, 2 * D])
    c.w_out = din("conv_w_out", [D, D])
    c.ffn_g = din("ffn_w_gate", [D, FF])
    c.ffn_u = din("ffn_w_up", [D, FF])
    c.ffn_d = din("ffn_w_down", [FF, D])
    c.w_kvf = din("w_kvf", [D, 2 * D + H])
    c.w_q = din("w_q", [D, D])
    c.w_o = din("w_o", [D, D])
    c.moe_g = din("moe_w_gate", [ne_decl, D, FF])
    c.moe_u = din("moe_w_up", [ne_decl, D, FF])
    c.moe_d = din("moe_w_down", [ne_decl, FF, D])

    def scr(name, shape, dt=BF16):
        return nc.dram_tensor(name, list(shape), dt)

    c.w_in_b = scr("w_in_b", [D, 2 * D])
    c.w_out_b = scr("w_out_b", [D, D])
    c.ffn_g_b = scr("ffn_g_b", [D, FF])
    c.ffn_u_b = scr("ffn_u_b", [D, FF])
    c.ffn_d_b = scr("ffn_d_b", [FF, D])
    c.w_kvf_b = scr("w_kvf_b", [D, 2 * D + H])
    c.w_q_b = scr("w_q_b", [D, D])
    c.w_o_b = scr("w_o_b", [D, D])
    c.moe_g_b = scr("moe_g_b", [ne_decl, D, FF])
    c.moe_u_b = scr("moe_u_b", [ne_decl, D, FF])
    c.moe_d_b = scr("moe_d_b", [ne_decl, FF, D])
    c.x1T = scr("x1T", [128, KC, TOK], F32)
    c.gk = [scr(f"gk{t}", [D, T]) for t in range(NT)]
    c.gko = [scr(f"gko{t}", [2 * D, T]) for t in range(NT)]
    c.gv = [scr(f"gv{t}", [T, D]) for t in range(NT)]
    c.gvo = [scr(f"gvo{t}", [2 * T, D]) for t in range(NT)]
    c.ga = scr("ga", [112, TOK])
    c.gao = scr("gao", [224, TOK])
    c.qT = scr("qT", [D, TOK])
    c.kaug_own = scr("kaug_own", [H, 7, TOK])
    c.qaug = scr("qaug", [H, 7, TOK])
    c.oT = scr("oT", [D, TOK])
    c.flog = scr("flog_d", [16, TOK], F32)
    c.rsum = scr("rsum", [H, TOK], F32)
    c.out = nc.dram_tensor("out", [TOK, D], F32, kind="ExternalOutput")
    c.debug = debug
    if debug in ("x2", "x3", "g"):
        c.dbg_x2T = nc.dram_tensor("dbg_x2T", [128, KC, TOK], F32, kind="ExternalOutput")


def setup_common(P, c):
    nc = P.nc
    c.ps = nc.alloc_psum_tensor("ps", [128, 8, 512], F32)
    c.psb = [Buf(f"psb{i}") for i in range(8)]
    c.ps_rr = 0
    c.ident_f = P.sb("ident_f", [128, 128], F32)
    c.ident_b = P.sb("ident_b", [128, 128], BF16)
    c.ones_b = P.sb("ones_b", [128, 128], BF16)
    c.vec = P.sb("vec", [128, NV], F32)
    c.vech = P.sb("vech", [128, 16], F32)
    c.wdw_s = P.sb("wdw_s", [128, KC, CW], F32)
    c.bf_s = P.sb("bf_s", [16, 1], F32)
    c.hmask_s = P.sb("hmask_s", [128, 1], F32)
    c.eps_t = P.sb("eps_t", [128, 1], F32)
    c.B_const = Buf("const")
    ds = P.new_dsem("const")
    c.ds_const = ds

    def ld(eng):
        r = []
        r.append(eng.dma_start(out=c.ident_f[:, :], in_=c.ident[:, :]))
        r.append(eng.dma_start(out=c.vec[:, :], in_=c.vecs[:, :]))
        r.append(eng.dma_start(out=c.wdw_s[:, :, :], in_=c.wdw[:, :, :]))
        r.append(eng.dma_start(out=c.bf_s[:, :], in_=c.bf[:, :]))
        r.append(eng.dma_start(out=c.hmask_s[:, :], in_=c.hmask[:, :]))
        return r
    P.dma("sp", ld, ds, 5, writes=[c.B_const], name="const_ld")
    c.B_const2 = Buf("const2")
    c.sb_phase_base = None

    def mk(eng):
        eng.tensor_copy(out=c.ident_b[:, :], in_=c.ident_f[:, :])
        eng.memset(c.ones_b[:, :], 1.0 / 1024.0)
        eng.memset(c.eps_t[:, :], EPS)
        return eng.tensor_scalar(out=c.vech[:, :], in0=c.vec[:, V_BA:V_BA + 16], scalar1=0.5, scalar2=None, op0=ALU.mult)
    P.op("dve", mk, reads=[c.B_const], writes=[c.B_const2], name="const_mk")


def psbank(c):
    i = c.ps_rr
    c.ps_rr = (c.ps_rr + 1) % 8
    return i


def convert_weights(P, c):
    c.B_w = {}

    def grp(name, pairs):
        ds = P.new_dsem("cv_" + name)
        b = Buf("wb_" + name)
        c.B_w[name] = b

        def em(eng, pairs=pairs):
            return [eng.dma_start(out=o, in_=i) for (o, i) in pairs]
        P.dma("pool", em, ds, len(pairs), writes=[b], name="cv_" + name)

    grp("in", [(c.w_in_b[:, :], c.w_in[:, :]), (c.w_out_b[:, :], c.w_out[:, :])])
    grp("ffn", [(c.ffn_g_b[:, :], c.ffn_g[:, :]), (c.ffn_u_b[:, :], c.ffn_u[:, :]),
                (c.ffn_d_b[0:1024, :], c.ffn_d[0:1024, :]), (c.ffn_d_b[1024:2048, :], c.ffn_d[1024:2048, :]),
                (c.ffn_d_b[2048:FF, :], c.ffn_d[2048:FF, :])])
    grp("att", [(c.w_kvf_b[:, :], c.w_kvf[:, :]), (c.w_q_b[:, :], c.w_q[:, :]), (c.w_o_b[:, :], c.w_o[:, :])])


def convert_expert(P, c, e):
    ds = P.new_dsem(f"cv_e{e}")
    b = Buf(f"wb_e{e}")
    c.B_w[f"e{e}"] = b
    pairs = [(c.moe_g_b[e, :, :], c.moe_g[e, :, :]), (c.moe_u_b[e, :, :], c.moe_u[e, :, :]),
             (c.moe_d_b[e, 0:1024, :], c.moe_d[e, 0:1024, :]), (c.moe_d_b[e, 1024:2048, :], c.moe_d[e, 1024:2048, :]),
             (c.moe_d_b[e, 2048:FF, :], c.moe_d[e, 2048:FF, :])]

    def em(eng, pairs=pairs):
        return [eng.dma_start(out=o, in_=i) for (o, i) in pairs]
    P.dma("pool", em, ds, len(pairs), writes=[b], name=f"cv_e{e}")


class Ring:
    def __init__(self, P, n, name="ring"):
        self.P = P
        self.n = n
        self.off = []
        self.bufs = [Buf(f"{name}{i}") for i in range(n)]
        self.ds = [P.new_dsem(f"{name}{i}") for i in range(n)]
        self.v8 = []
        self.vgu = []
        for i in range(n):
            t = P.sb(f"{name}{i}", [128, 8, 1024], BF16)
            off = P.sb_off - 16384
            self.v8.append(t)
            self.vgu.append(P.sb(f"{name}gu{i}", [128, 2, 8, 512], BF16, off=off))
        self.rr = 0

    def load(self, pieces_fn, wbuf, name="wl"):
        i = self.rr
        self.rr = (self.rr + 1) % self.n
        pieces = pieces_fn(i)

        def em(eng, pieces=pieces):
            return [eng.dma_start(out=o, in_=s) for (o, s) in pieces]
        self.P.dma("sp", em, self.ds[i], len(pieces), reads=[wbuf], writes=[self.bufs[i]], name=name)
        return i


def w8(src, c0, ncols):
    return src[:, c0:c0 + ncols].rearrange("(kc p) c -> p kc c", p=128)


def alloc_phase_a(P, c):
    c.ring = Ring(P, 4)
    c.x_tok = P.sb("x_tok", [128, 4, D], F32)
    off = P.sb_off - 16384
    c.v = P.sb("v", [128, KC, T], F32, off=off)
    c.B_r1 = Buf("r1")
    c.B_v = [Buf(f"v{k}") for k in range(KC)]
    c.xT = P.sb("xT", [128, KC, T], F32)
    c.B_xT = [Buf(f"xT{k}") for k in range(KC)]
    c.sq = P.sb("sq", [128, KC, T], BF16)
    c.B_sq = Buf("sq")
    c.hT = P.sb("hT", [128, KC, T], BF16)
    c.B_hT = Buf("hT")
    c.ap_ = P.sb("aprime", [128, KC, T], BF16)
    c.B_ap = [Buf(f"ap{k}") for k in range(KC)]
    c.uT = P.sb("uT", [128, KC, 32 + T], BF16)
    c.B_uT = [Buf(f"uT{k}") for k in range(KC)]
    c.sT = P.sb("sT", [128, KC, T], BF16)
    c.B_sT = Buf("sT")
    c.aT = P.sb("aT", [128, FC, T], BF16)
    c.B_aT = [Buf(f"aT{k}") for k in range(FC)]
    offa = P.sb_off - FC * T * 2
    c.kst = P.sb("kst", [128, KC, T], BF16, off=offa)
    c.vst = P.sb("vst", [128, 4, D], BF16, off=offa + 8192)
    c.diag = P.sb("diag", [128, 2, CW, 128], BF16)
    c.B_diag = [Buf("diag0"), Buf("diag1")]
    c.th = [P.sb(f"th{i}", [128, T], F32) for i in range(3)]
    c.B_th = [Buf(f"th{i}") for i in range(3)]
    c.th_rr = 0
    c.st = [P.sb(f"st{i}", [128, T], F32) for i in range(6)]
    c.B_st = [Buf(f"st{i}") for i in range(6)]
    c.B_flog = Buf("flog")
    c.wf = P.sb("wf", [128, KC, 16], BF16)
    c.B_wf = Buf("wf")
    c.ds_x = P.new_dsem("x")
    c.ds_st = [P.new_dsem(f"store{i}") for i in range(4)]
    c.B_x1T = [Buf(f"x1T_{t}") for t in range(NT)]
    c.B_gk = [Buf(f"gk{t}") for t in range(NT)]
    c.B_gv = [Buf(f"gv{t}") for t in range(NT)]
    c.B_gko = [Buf(f"gko{t}") for t in range(NT)]
    c.B_gvo = [Buf(f"gvo{t}") for t in range(NT)]
    c.B_ga = Buf("ga")
    c.B_gao = Buf("gao")
    c.B_qT = Buf("qTd")


def mm_group(P, c, bank, pairs, reads, ncols, nrows=128, name="mm", col0=0):
    ps = c.ps
    n = len(pairs)

    def em(pe, pairs=pairs):
        r = None
        for i, (l, rr) in enumerate(pairs):
            r = pe.matmul(ps[0:nrows, bank, col0:col0 + ncols], l, rr, start=(i == 0), stop=(i == n - 1))
        return r
    o = P.op("pe", em, reads=reads, writes=[c.psb[bank]], name=name)
    o.hoist = True
    return o


def rms_stats(P, c, ncols, src_bufs):
    xT, sq = c.xT, c.sq
    P.op("act", lambda a: a.activation(out=sq[:, :, 0:ncols], in_=xT[:, :, 0:ncols], func=AF.Square),
         reads=src_bufs, writes=[c.B_sq], name="sq")
    b = psbank(c)
    mm_group(P, c, b, [(c.ones_b[:, :], sq[:, k, 0:ncols]) for k in range(KC)], [c.B_sq, c.B_const2], ncols, name="ms")
    i0, i1 = 0, 1
    t0, t1 = c.st[i0], c.st[i1]
    P.op("act", lambda a: a.activation(out=t0[:, 0:ncols], in_=c.ps[:, b, 0:ncols], func=AF.Ln, bias=c.eps_t[:, 0:1], scale=1.0),
         reads=[c.psb[b], c.B_const2], writes=[c.B_st[i0]], name="ms_ln")
    P.op("act", lambda a: a.activation(out=t1[:, 0:ncols], in_=t0[:, 0:ncols], func=AF.Exp, scale=-0.5),
         reads=[c.B_st[i0]], writes=[c.B_st[i1]], name="rstd")
    return i1


def apply_norm(P, c, ncols, rstd_i, gcol, dst, dstbuf, name="hn"):
    xT = c.xT
    rs = c.st[rstd_i]
    for k in range(KC):
        P.op("dve", lambda v, k=k: v.scalar_tensor_tensor(out=dst[:, k, 0:ncols], in0=xT[:, k, 0:ncols], scalar=c.vec[:, gcol + k:gcol + k + 1],
                                                         in1=rs[:, 0:ncols], op0=ALU.mult, op1=ALU.mult),
             reads=[c.B_xT[k], c.B_st[rstd_i], c.B_const], writes=[dstbuf], name=name)


def load_x_and_transpose(P, c, src_ap, ntok):
    nch = (ntok + 127) // 128
    if ntok >= 128:
        def em(eng):
            return [eng.dma_start(out=c.x_tok[:, 0:nch, :], in_=src_ap.rearrange("(c p) d -> p c d", p=128))]
    else:
        def em(eng):
            return [eng.dma_start(out=c.x_tok[0:ntok, 0, :], in_=src_ap)]
    P.dma("sp", em, c.ds_x, 1, writes=[c.B_r1] + c.B_v, name="x_ld")
    for k in range(KC):
        b = psbank(c)

        def emt(pe, k=k, b=b):
            r = None
            for tc in range(nch):
                n = min(128, ntok - tc * 128)
                r = pe.transpose(out=c.ps[:, b, tc * 128:tc * 128 + n], in_=c.x_tok[0:n, tc, k * 128:(k + 1) * 128], identity=c.ident_f[0:n, 0:n])
            return r
        P.op("pe", emt, reads=[c.B_r1, c.B_const], writes=[c.psb[b]], name="xpose")
        if k % 2 == 0:
            P.op("act", lambda a, k=k, b=b: a.copy(out=c.xT[:, k, 0:ntok], in_=c.ps[:, b, 0:ntok]), reads=[c.psb[b]], writes=[c.B_xT[k]], name="xT_ev")
        else:
            P.op("dve", lambda v, k=k, b=b: v.tensor_copy(out=c.xT[:, k, 0:ntok], in_=c.ps[:, b, 0:ntok]), reads=[c.psb[b]], writes=[c.B_xT[k]], name="xT_ev")


def glu_stage(P, c, ncols, ucol0, mask_halo=False):
    ring = c.ring
    sa = ring.load(lambda i: [(ring.v8[i][:, :, :], w8(c.w_in_b, 0, 1024))], c.B_w["in"], name="w_in_a")
    sg = ring.load(lambda i: [(ring.v8[i][:, :, :], w8(c.w_in_b, 1024, 1024))], c.B_w["in"], name="w_in_g")
    for oc in range(KC):
        b = psbank(c)
        mm_group(P, c, b, [(ring.v8[sa][:, k, oc * 128:(oc + 1) * 128], c.hT[:, k, 0:ncols]) for k in range(KC)],
                 [ring.bufs[sa], c.B_hT], ncols, name="w_in_a")
        P.op("act", lambda a, oc=oc, b=b: a.activation(out=c.ap_[:, oc, 0:ncols], in_=c.ps[:, b, 0:ncols], func=AF.Identity,
                                                       bias=c.vech[:, oc:oc + 1], scale=0.5),
             reads=[c.psb[b], c.B_const2], writes=[c.B_ap[oc]], name="aprime")
    for oc in range(KC):
        b = psbank(c)
        mm_group(P, c, b, [(ring.v8[sg][:, k, oc * 128:(oc + 1) * 128], c.hT[:, k, 0:ncols]) for k in range(KC)],
                 [ring.bufs[sg], c.B_hT], ncols, name="w_in_g")
        ti = c.th_rr
        c.th_rr = (c.th_rr + 1) % 3
        th = c.th[ti]
        P.op("act", lambda a, oc=oc, b=b, th=th: a.activation(out=th[:, 0:ncols], in_=c.ps[:, b, 0:ncols], func=AF.Tanh,
                                                              bias=c.vech[:, 8 + oc:9 + oc], scale=0.5),
             reads=[c.psb[b], c.B_const2], writes=[c.B_th[ti]], name="tanh")
        if not mask_halo:
            P.op("dve", lambda v, oc=oc, th=th: v.scalar_tensor_tensor(out=c.uT[:, oc, ucol0:ucol0 + ncols], in0=th[:, 0:ncols], scalar=1.0,
                                                                      in1=c.ap_[:, oc, 0:ncols], op0=ALU.add, op1=ALU.mult),
                 reads=[c.B_th[ti], c.B_ap[oc]], writes=[c.B_uT[oc]], name="glu")
        else:
            P.op("dve", lambda v, oc=oc, th=th: v.scalar_tensor_tensor(out=th[:, 0:ncols], in0=th[:, 0:ncols], scalar=1.0, in1=c.ap_[:, oc, 0:ncols], op0=ALU.add, op1=ALU.mult),
                 reads=[c.B_th[ti], c.B_ap[oc]], writes=[c.B_th[ti]], name="glu_h1")
            P.op("dve", lambda v, oc=oc, th=th: v.tensor_scalar(out=c.uT[:, oc, ucol0:ucol0 + ncols], in0=th[:, 0:ncols], scalar1=c.hmask_s[:, 0:1], scalar2=None, op0=ALU.mult),
                 reads=[c.B_th[ti], c.B_const], writes=[c.B_uT[oc]], name="glu_h2")


def conv_ln_stage(P, c):
    for k in range(KC):
        di = k % 2
        dg = c.diag

        def emd(g, k=k, di=di):
            r = None
            for j in range(CW):
                r = g.tensor_scalar(out=dg[:, di, j, :], in0=c.ident_b[:, :], scalar1=c.wdw_s[:, k, j:j + 1], scalar2=1.0, op0=ALU.mult, op1=ALU.mult)
            return r
        P.op("pool" if k % 2 == 0 else "dve", emd, reads=[c.B_const, c.B_const2], writes=[c.B_diag[di]], name="diag")
        b = psbank(c)
        mm_group(P, c, b, [(dg[:, di, j, :], c.uT[:, k, 2 + j:2 + j + T]) for j in range(CW)], [c.B_diag[di], c.B_uT[k]], T, name="conv")
        P.op("act", lambda a, k=k, b=b: a.activation(out=c.v[:, k, :], in_=c.ps[:, b, :], func=AF.Identity, bias=c.vec[:, V_BDW + k:V_BDW + k + 1], scale=1.0),
             reads=[c.psb[b], c.B_const], writes=[c.B_v[k]], name="v_ev")
        P.op("act", lambda a, k=k, b=b: a.activation(out=c.sq[:, k, :], in_=c.ps[:, b, :], func=AF.Square, bias=c.vec[:, V_BDW + k:V_BDW + k + 1], scale=1.0),
             reads=[c.psb[b], c.B_const], writes=[c.B_sq], name="v_sq")
        P.op("pool", lambda g, k=k: g.tensor_copy(out=c.ap_[:, k, :], in_=c.v[:, k, :]), reads=[c.B_v[k]], writes=[c.B_ap[k]], name="vb")
    bm = psbank(c)
    mm_group(P, c, bm, [(c.ones_b[:, :], c.ap_[:, k, :]) for k in range(KC)], c.B_ap + [c.B_const2], T, name="ln_mean")
    be = psbank(c)
    mm_group(P, c, be, [(c.ones_b[:, :], c.sq[:, k, :]) for k in range(KC)], [c.B_sq, c.B_const2], T, name="ln_ex2")
    mean, m2, var, rstd, Bt = c.st[2], c.st[3], c.st[0], c.st[1], c.st[4]
    P.op("act", lambda a: a.copy(out=mean[:, :], in_=c.ps[:, bm, :]), reads=[c.psb[bm]], writes=[c.B_st[2]], name="mean")
    P.op("dve", lambda v: v.tensor_tensor(out=m2[:, :], in0=mean[:, :], in1=mean[:, :], op=ALU.mult), reads=[c.B_st[2]], writes=[c.B_st[3]], name="m2")
    P.op("dve", lambda v: v.scalar_tensor_tensor(out=var[:, :], in0=c.ps[:, be, :], scalar=EPS, in1=m2[:, :], op0=ALU.add, op1=ALU.subtract),
         reads=[c.psb[be], c.B_st[3]], writes=[c.B_st[0]], name="var")
    P.op("act", lambda a: a.activation(out=var[:, :], in_=var[:, :], func=AF.Ln), reads=[c.B_st[0]], writes=[c.B_st[0]], name="ln_ln")
    P.op("act", lambda a: a.activation(out=rstd[:, :], in_=var[:, :], func=AF.Exp, scale=-0.5), reads=[c.B_st[0]], writes=[c.B_st[1]], name="ln_rstd")
    P.op("dve", lambda v: v.scalar_tensor_tensor(out=Bt[:, :], in0=mean[:, :], scalar=-1.0, in1=rstd[:, :], op0=ALU.mult, op1=ALU.mult),
         reads=[c.B_st[2], c.B_st[1]], writes=[c.B_st[4]], name="ln_B")
    for k in range(KC):
        ti = c.th_rr
        c.th_rr = (c.th_rr + 1) % 3
        th = c.th[ti]
        P.op("pool", lambda g, k=k, th=th: g.tensor_tensor(out=th[:, :], in0=c.v[:, k, :], in1=rstd[:, :], op=ALU.mult),
             reads=[c.B_v[k], c.B_st[1]], writes=[c.B_th[ti]], name="ln_t1")
        P.op("dve", lambda v, th=th: v.tensor_tensor(out=th[:, :], in0=th[:, :], in1=Bt[:, :], op=ALU.add),
             reads=[c.B_th[ti], c.B_st[4]], writes=[c.B_th[ti]], name="ln_t2")
        P.op("act", lambda a, k=k, th=th: a.activation(out=c.sT[:, k, :], in_=th[:, :], func=AF.Silu, bias=c.vec[:, V_LNB + k:V_LNB + k + 1],
                                                       scale=c.vec[:, V_LNG + k:V_LNG + k + 1]),
             reads=[c.B_th[ti], c.B_const], writes=[c.B_sT], name="ln_silu")


def halo_shift(P, c):
    for k in range(KC):
        P.op("pool", lambda g, k=k: g.tensor_copy(out=c.uT[:, k, 2:32], in_=c.uT[:, k, 2 + T:32 + T]), reads=[c.B_uT[k]], writes=[c.B_uT[k]], name="halo")


def wout_stage(P, c):
    ring = c.ring
    s = ring.load(lambda i: [(ring.v8[i][:, :, :], w8(c.w_out_b, 0, 1024))], c.B_w["in"], name="w_out")
    for dc in range(KC):
        b = psbank(c)
        mm_group(P, c, b, [(ring.v8[s][:, k, dc * 128:(dc + 1) * 128], c.sT[:, k, :]) for k in range(KC)], [ring.bufs[s], c.B_sT], T, name="w_out")
        P.op("dve", lambda v, dc=dc, b=b: v.scalar_tensor_tensor(out=c.xT[:, dc, :], in0=c.ps[:, b, :], scalar=c.vec[:, V_BOUT + dc:V_BOUT + dc + 1],
                                                                in1=c.xT[:, dc, :], op0=ALU.add, op1=ALU.add),
             reads=[c.psb[b], c.B_xT[dc], c.B_const], writes=[c.B_xT[dc]], name="res1")


FGROUPS = [(0, 4), (4, 4), (8, 4), (12, 4), (16, 4), (20, 2)]
DHALF = [[(0, 8), (8, 4)], [(12, 8), (20, 2)]]


def ffn_stage(P, c, g_b, u_b, d_b, wbuf, hsrc, hbuf, gate=None):
    ring = c.ring
    for (f0, nf) in FGROUPS:
        s = ring.load(lambda i, f0=f0, nf=nf: [(ring.vgu[i][:, 0, :, 0:nf * 128], w8(g_b, f0 * 128, nf * 128)),
                                              (ring.vgu[i][:, 1, :, 0:nf * 128], w8(u_b, f0 * 128, nf * 128))], wbuf, name="w_gu")
        for j in range(nf):
            fc = f0 + j
            bg = psbank(c)
            mm_group(P, c, bg, [(ring.vgu[s][:, 0, k, j * 128:(j + 1) * 128], hsrc[:, k, :]) for k in range(KC)], [ring.bufs[s], hbuf], T, name="ffn_g")
            bu = psbank(c)
            mm_group(P, c, bu, [(ring.vgu[s][:, 1, k, j * 128:(j + 1) * 128], hsrc[:, k, :]) for k in range(KC)], [ring.bufs[s], hbuf], T, name="ffn_u")
            ti = c.th_rr
            c.th_rr = (c.th_rr + 1) % 3
            th = c.th[ti]
            P.op("act", lambda a, bg=bg, th=th: a.activation(out=th[:, :], in_=c.ps[:, bg, :], func=AF.Silu), reads=[c.psb[bg]], writes=[c.B_th[ti]], name="silu")
            P.op("dve", lambda v, bu=bu, th=th, fc=fc: v.tensor_tensor(out=c.aT[:, fc, :], in0=th[:, :], in1=c.ps[:, bu, :], op=ALU.mult),
                 reads=[c.B_th[ti], c.psb[bu]], writes=[c.B_aT[fc]], name="a_mul")
    for half in range(2):
        slots = []
        for (f0, nf) in DHALF[half]:
            s = ring.load(lambda i, f0=f0, nf=nf: [(ring.v8[i][:, 0:nf, :], d_b[f0 * 128:(f0 + nf) * 128, :].rearrange("(fc p) c -> p fc c", p=128))], wbuf, name="w_d")
            slots.append((s, f0, nf))
        for dc in range(KC):
            b = psbank(c)
            pairs = []
            reads = []
            for (s, f0, nf) in slots:
                reads.append(ring.bufs[s])
                for j in range(nf):
                    pairs.append((ring.v8[s][:, j, dc * 128:(dc + 1) * 128], c.aT[:, f0 + j, :]))
                    reads.append(c.B_aT[f0 + j])
            mm_group(P, c, b, pairs, reads, T, name="ffn_d")
            if gate is None:
                P.op("dve", lambda v, dc=dc, b=b: v.tensor_tensor(out=c.xT[:, dc, :], in0=c.ps[:, b, :], in1=c.xT[:, dc, :], op=ALU.add),
                     reads=[c.psb[b], c.B_xT[dc]], writes=[c.B_xT[dc]], name="res2")
            else:
                gt, gbuf = gate
                ti = c.th_rr
                c.th_rr = (c.th_rr + 1) % 3
                th = c.th[ti]
                P.op("dve", lambda v, b=b, th=th: v.tensor_tensor(out=th[:, :], in0=c.ps[:, b, :], in1=gt, op=ALU.mult),
                     reads=[c.psb[b], gbuf], writes=[c.B_th[ti]], name="gmul")
                P.op("pool", lambda g, dc=dc, th=th: g.tensor_tensor(out=c.xT[:, dc, :], in0=th[:, :], in1=c.xT[:, dc, :], op=ALU.add),
                     reads=[c.B_th[ti], c.B_xT[dc]], writes=[c.B_xT[dc]], name="res_moe")


def allgather(P, c, src, dst, bsrc, bdst):
    ds = P.new_dsem("cc")

    def em(g):
        return [g.collective_compute("AllGather", ALU.bypass, replica_groups=[[0, 1], [2, 3], [4, 5], [6, 7]],
                                     ins=[src.ap()], outs=[dst.ap()])]
    P.dma("pool", em, ds, 1, reads=[bsrc], writes=[bdst], name="allgather", inc=1)


def proj_stage(P, c, t):
    ring = c.ring
    t0 = t * T
    ri = rms_stats(P, c, T, c.B_xT)
    apply_norm(P, c, T, ri, V_KVN, c.hT, c.B_hT, name="hn_kv")
    apply_norm(P, c, T, ri, V_MIX1, c.sT, c.B_sT, name="hn_q")
    s = ring.load(lambda i: [(ring.v8[i][:, :, :], w8(c.w_kvf_b, 0, 1024))], c.B_w["att"], name="w_k")
    B_kst = Buf("kst")
    for cc in range(KC):
        b = psbank(c)
        mm_group(P, c, b, [(ring.v8[s][:, k, cc * 128:(cc + 1) * 128], c.hT[:, k, :]) for k in range(KC)], [ring.bufs[s], c.B_hT], T, name="k_mm")
        P.op("act", lambda a, cc=cc, b=b: a.copy(out=c.kst[:, cc, :], in_=c.ps[:, b, :]), reads=[c.psb[b]], writes=[c.B_aT[cc]], name="k_ev")
    P.dma("act", lambda e: [e.dma_start(out=c.gk[t][:, :].rearrange("(cc p) t -> p cc t", p=128), in_=c.kst[:, :, :])],
          c.ds_st[0], 1, reads=c.B_aT[0:8], writes=[c.B_gk[t]], name="k_st")
    allgather(P, c, c.gk[t], c.gko[t], c.B_gk[t], c.B_gko[t])
    s = ring.load(lambda i: [(ring.v8[i][:, :, :], w8(c.w_kvf_b, 1024, 1024))], c.B_w["att"], name="w_v")
    for tc in range(4):
        for hf in range(2):
            b = psbank(c)
            mm_group(P, c, b, [(c.hT[:, k, tc * 128:(tc + 1) * 128], ring.v8[s][:, k, hf * 512:(hf + 1) * 512]) for k in range(KC)],
                     [ring.bufs[s], c.B_hT], 512, name="v_mm")
            P.op("dve", lambda v, tc=tc, hf=hf, b=b: v.tensor_copy(out=c.vst[:, tc, hf * 512:(hf + 1) * 512], in_=c.ps[:, b, :]),
                 reads=[c.psb[b]], writes=[c.B_aT[8 + tc * 2 + hf]], name="v_ev")
    P.dma("act", lambda e: [e.dma_start(out=c.gv[t][:, :].rearrange("(tc p) d -> p tc d", p=128), in_=c.vst[:, :, :])],
          c.ds_st[1], 1, reads=c.B_aT[8:16], writes=[c.B_gv[t]], name="v_st")
    allgather(P, c, c.gv[t], c.gvo[t], c.B_gv[t], c.B_gvo[t])
    b = psbank(c)
    mm_group(P, c, b, [(c.wf[:, k, :], c.hT[:, k, :]) for k in range(KC)], [c.B_wf, c.B_hT], T, nrows=16, name="f_mm")
    P.op("act", lambda a, b=b: a.activation(out=c.st[5][0:16, :], in_=c.ps[0:16, b, :], func=AF.Identity, bias=c.bf_s[:, 0:1], scale=1.0),
         reads=[c.psb[b], c.B_const], writes=[c.B_st[5]], name="f_ev")
    P.dma("act", lambda e: [e.dma_start(out=c.flog[:, t0:t0 + T], in_=c.st[5][0:16, :])], c.ds_st[3], 1, reads=[c.B_st[5]], writes=[c.B_flog], name="f_st")
    s = ring.load(lambda i: [(ring.v8[i][:, :, :], w8(c.w_q_b, 0, 1024))], c.B_w["att"], name="w_q")
    for cc in range(KC):
        b = psbank(c)
        mm_group(P, c, b, [(ring.v8[s][:, k, cc * 128:(cc + 1) * 128], c.sT[:, k, :]) for k in range(KC)], [ring.bufs[s], c.B_sT], T, name="q_mm")
        P.op("act", lambda a, cc=cc, b=b: a.activation(out=c.kst[:, cc, :], in_=c.ps[:, b, :], func=AF.Copy, scale=0.125),
             reads=[c.psb[b]], writes=[c.B_aT[cc]], name="q_ev")
    P.dma("act", lambda e: [e.dma_start(out=c.qT[:, t0:t0 + T].rearrange("(cc p) t -> p cc t", p=128), in_=c.kst[:, :, :])],
          c.ds_st[2], 1, reads=c.B_aT[0:8], writes=[c.B_qT], name="q_st")


def phase_a(P, c, ntiles=NT, debug=None):
    load_x_and_transpose(P, c, c.xhalo[:, :], 32)
    ri = rms_stats(P, c, 32, c.B_xT)
    apply_norm(P, c, 32, ri, V_MIX0, c.hT, c.B_hT)
    glu_stage(P, c, 32, 0, mask_halo=True)
    for t in range(ntiles):
        t0 = t * T
        load_x_and_transpose(P, c, c.x[t0:t0 + T, :], T)
        ri = rms_stats(P, c, T, c.B_xT)
        apply_norm(P, c, T, ri, V_MIX0, c.hT, c.B_hT)
        glu_stage(P, c, T, 32)
        conv_ln_stage(P, c)
        halo_shift(P, c)
        wout_stage(P, c)
        if debug == "xa":
            P.dma("act", lambda e, t0=t0: [e.dma_start(out=c.x1T[:, :, t0:t0 + T], in_=c.xT[:, :, :])], c.ds_st[3], 1, reads=c.B_xT, writes=[c.B_x1T[t]], name="xa_st")
            continue
        ri = rms_stats(P, c, T, c.B_xT)
        apply_norm(P, c, T, ri, V_FFN0, c.hT, c.B_hT)
        ffn_stage(P, c, c.ffn_g_b, c.ffn_u_b, c.ffn_d_b, c.B_w["ffn"], c.hT, c.B_hT)
        P.dma("act", lambda e, t0=t0: [e.dma_start(out=c.x1T[:, :, t0:t0 + T], in_=c.xT[:, :, :])], c.ds_st[3], 1, reads=c.B_xT, writes=[c.B_x1T[t]], name="x1_st")
        if t == 0:
            P.dma("sp", lambda e: [e.dma_start(out=c.wf[:, :, :], in_=w8(c.w_kvf_b, 2048, 16))], P.new_dsem("wf"), 1, reads=[c.B_w["att"]], writes=[c.B_wf], name="wf_ld")
        proj_stage(P, c, t)
        if t < c.ne_decl:
            convert_expert(P, c, t)


NKB_HALF = 32
GRP = 3


def bcast_last(ap2d, n):
    a = [list(x) for x in ap2d.ap]
    return bass.AP(tensor=ap2d.tensor, offset=ap2d.offset, ap=a + [[0, n]])


def phase_b(P, c):
    base = c.sb_phase_base
    P.sb_off = base
    fl = P.sb("fl", [16, TOK], F32)
    ones = P.sb("ones16", [16, TOK], F32)
    C = P.sb("C", [16, TOK], F32)
    e1 = P.sb("e1", [16, TOK], F32)
    hs = [P.sb(f"h{i}", [16, TOK], BF16) for i in range(3)]
    rs_ = [P.sb(f"r{i}", [16, TOK], BF16) for i in range(3)]
    ns = [P.sb(f"n{i}", [16, TOK], BF16) for i in range(3)]
    oneb = P.sb("oneb", [16, TOK], BF16)
    zerob = P.sb("zerob", [16, TOK], BF16)
    B = {k: Buf("pb_" + k) for k in ("fl", "ones", "C", "e1", "h", "r", "n", "cb")}
    ds = P.new_dsem("pb")
    P.dma("sp", lambda e: [e.dma_start(out=fl[:, :], in_=c.flog[:, :])], ds, 1, reads=[c.B_flog], writes=[B["fl"]], name="fl_ld")

    def mk(v):
        v.memset(ones[:, :], 1.0)
        v.memset(oneb[:, :], 1.0)
        return v.memset(zerob[:, :], 0.0)
    P.op("dve", mk, writes=[B["ones"], B["cb"]], name="pb_const")
    P.op("act", lambda a: a.activation(out=fl[:, :], in_=fl[:, :], func=AF.Exp, scale=-1.0), reads=[B["fl"]], writes=[B["fl"]], name="exp_f")
    P.op("act", lambda a: a.activation(out=fl[:, :], in_=fl[:, :], func=AF.Ln, bias=1.0, scale=1.0), reads=[B["fl"]], writes=[B["fl"]], name="ln_f")
    P.op("dve", lambda v: v.tensor_tensor_scan(out=C[:, :], data0=ones[:, :], data1=fl[:, :], initial=0.0, op0=ALU.mult, op1=ALU.add),
         reads=[B["fl"], B["ones"]], writes=[B["C"]], name="scan")

    def split(src, outs, bsrc, bout, nm):
        bs = [Buf(nm + str(i)) for i in range(3)]
        P.op("dve", lambda v: v.tensor_copy(out=outs[0][:, :], in_=src[:, :]), reads=[bsrc], writes=[bs[0]], name=nm)
        P.op("dve", lambda v: v.tensor_tensor(out=e1[:, :], in0=src[:, :], in1=outs[0][:, :], op=ALU.subtract), reads=[bsrc, bs[0]], writes=[B["e1"]], name=nm)
        P.op("dve", lambda v: v.tensor_copy(out=outs[1][:, :], in_=e1[:, :]), reads=[B["e1"]], writes=[bs[1]], name=nm)
        P.op("dve", lambda v: v.tensor_tensor(out=e1[:, :], in0=e1[:, :], in1=outs[1][:, :], op=ALU.subtract), reads=[B["e1"], bs[1]], writes=[B["e1"]], name=nm)
        P.op("dve", lambda v: v.tensor_copy(out=outs[2][:, :], in_=e1[:, :]), reads=[B["e1"]] + bs[0:2], writes=[bout], name=nm)
    split(C, hs, B["C"], B["h"], "split_h")

    def neg(v):
        r = None
        for i in range(3):
            r = v.tensor_scalar(out=ns[i][:, :], in0=hs[i][:, :], scalar1=-1.0, scalar2=None, op0=ALU.mult)
        return r
    P.op("dve", neg, reads=[B["h"]], writes=[B["n"]], name="neg_h")
    P.op("dve", lambda v: v.tensor_scalar(out=fl[:, :], in0=C[:, :], scalar1=C[:, TOK - 1:TOK], scalar2=None, op0=ALU.subtract),
         reads=[B["C"], B["fl"]], writes=[B["fl"]], name="R")
    split(fl, rs_, B["fl"], B["r"], "split_r")
    c.B_kaug_own = Buf("kaug_own")
    c.B_qaug = Buf("qaug_d")
    gin_aug = c.ga[:, :].rearrange("(h r) t -> h r t", r=7)

    def st(e):
        r = []
        for i in range(3):
            r.append(e.dma_start(out=c.kaug_own[:, i, :], in_=oneb[:, :]))
            r.append(e.dma_start(out=c.kaug_own[:, 3 + i, :], in_=hs[i][:, :]))
            r.append(e.dma_start(out=gin_aug[:, i, :], in_=oneb[:, :]))
            r.append(e.dma_start(out=gin_aug[:, 3 + i, :], in_=rs_[i][:, :]))
            r.append(e.dma_start(out=c.qaug[:, i, :], in_=ns[i][:, :]))
            r.append(e.dma_start(out=c.qaug[:, 3 + i, :], in_=oneb[:, :]))
        r.append(e.dma_start(out=c.kaug_own[:, 6, :], in_=zerob[:, :]))
        r.append(e.dma_start(out=gin_aug[:, 6, :], in_=zerob[:, :]))
        r.append(e.dma_start(out=c.qaug[:, 6, :], in_=oneb[:, :]))
        return r
    P.dma("sp", st, ds, 21, reads=[B["h"], B["r"], B["n"], B["cb"]], writes=[c.B_kaug_own, c.B_ga, c.B_qaug], name="aug_st")


def phase_c(P, c):
    allgather(P, c, c.ga, c.gao, c.B_ga, c.B_gao)


def phase_d(P, c, nheads=H):
    nc = P.nc
    P.sb_off = c.sb_phase_base
    Kt = [P.sb(f"Kt{i}", [128, 2 * TOK], BF16) for i in range(2)]
    Vt = [P.sb(f"Vt{i}", [128, 2 * NKB_HALF, 65], BF16) for i in range(2)]
    Qt = [P.sb(f"Qt{i}", [128, TOK], BF16) for i in range(2)]
    PT = [P.sb(f"PT{i}", [128, GRP, 512], BF16) for i in range(3)]
    osb = [P.sb(f"osb{i}", [128, 512], BF16) for i in range(2)]
    rsb = [P.sb(f"rsb{i}", [128, 512], F32) for i in range(2)]
    cm = P.sb("cm", [128, 128], BF16)
    B_K = [Buf(f"Kt{i}") for i in range(2)]
    B_V = [Buf(f"Vt{i}") for i in range(2)]
    B_Q = [Buf(f"Qt{i}") for i in range(2)]
    B_PT = [Buf(f"PT{i}") for i in range(3)]
    B_osb = [Buf(f"osb{i}") for i in range(2)]
    B_cm = Buf("cm")
    ds_k = [P.new_dsem(f"k{i}") for i in range(2)]
    ds_o = [P.new_dsem(f"o{i}") for i in range(2)]
    ds_m = P.new_dsem("cm")
    c.B_oT = Buf("oT_d")
    c.B_rsum = Buf("rsum_d")
    P.dma("sp", lambda e: [e.dma_start(out=cm[:, :], in_=c.cmask[:, :])], ds_m, 1, writes=[B_cm], name="cm_ld")

    def ones_col(v):
        v.memset(Vt[0][:, :, 64:65], 1.0)
        return v.memset(Vt[1][:, :, 64:65], 1.0)
    P.op("dve", ones_col, writes=B_V, name="ones_col")

    pt_rr = 0
    o_rr = 0
    def load_head(h):
        bi = h % 2
        K, V, Q = Kt[bi], Vt[bi], Qt[bi]

        def ldk(e, h=h, K=K, V=V, Q=Q):
            r = []
            for t in range(NT):
                r.append(e.dma_start(out=K[0:64, t * T:(t + 1) * T], in_=c.gko[t][h * 64:(h + 1) * 64, :]))
                r.append(e.dma_start(out=K[0:64, TOK + t * T:TOK + (t + 1) * T], in_=c.gk[t][h * 64:(h + 1) * 64, :]))
                r.append(e.dma_start(out=V[:, 4 * t:4 * t + 4, 0:64], in_=c.gvo[t][0:T, h * 64:(h + 1) * 64].rearrange("(kb p) d -> p kb d", p=128)))
                r.append(e.dma_start(out=V[:, NKB_HALF + 4 * t:NKB_HALF + 4 * t + 4, 0:64], in_=c.gv[t][:, h * 64:(h + 1) * 64].rearrange("(kb p) d -> p kb d", p=128)))
            r.append(e.dma_start(out=K[64:70, 0:TOK], in_=c.gao[h * 7:h * 7 + 6, :]))
            r.append(e.dma_start(out=K[70:71, 0:TOK], in_=c.flagrow[:, :]))
            r.append(e.dma_start(out=K[64:71, TOK:2 * TOK], in_=c.kaug_own[h, :, :]))
            r.append(e.dma_start(out=Q[0:64, :], in_=c.qT[h * 64:(h + 1) * 64, :]))
            r.append(e.dma_start(out=Q[64:71, :], in_=c.qaug[h, :, :]))
            return r
        P.dma("sp", ldk, ds_k[bi], 4 * NT + 5, reads=c.B_gko + c.B_gk + c.B_gvo + c.B_gv + [c.B_gao, c.B_kaug_own, c.B_qT, c.B_qaug], writes=[B_K[bi], B_V[bi], B_Q[bi]], name="kvq_ld")

    load_head(0)
    for h in range(nheads):
        bi = h % 2
        K, V, Q = Kt[bi], Vt[bi], Qt[bi]
        if h + 1 < nheads:
            load_head(h + 1)

        for qb in range(NT):
            q0 = qb * 512
            blocks = [(kb, 0) for kb in range(NKB_HALF)] + [(NKB_HALF + j, 0) for j in range(4 * qb)]
            blocks += [(NKB_HALF + 4 * qb + i, 128 * i) for i in range(4)]
            groups = [blocks[i:i + GRP] for i in range(0, len(blocks), GRP)]
            ob = 6 + (o_rr % 2)
            oi = o_rr % 2
            o_rr += 1
            nblk = len(blocks)

            def emit_S(g, gi):
                sb = (gi % 2) * GRP

                def em(pe, g=g, sb=sb, K=K, Q=Q, q0=q0):
                    r = None
                    for j, (kb, c0) in enumerate(g):
                        r = pe.matmul(c.ps[:, sb + j, c0:512], K[0:71, kb * 128:(kb + 1) * 128], Q[0:71, q0 + c0:q0 + 512], start=True, stop=True)
                    return r
                P.op("pe", em, reads=[B_K[bi], B_Q[bi]], writes=[c.psb[sb + j] for j in range(len(g))], name="S")

            state = {"blk": 0}

            def emit_exp_pv(g, gi):
                nonlocal pt_rr
                sb = (gi % 2) * GRP
                pi = pt_rr % 3
                pt_rr += 1
                pt = PT[pi]
                ng = len(g)
                P.op("act", lambda a, sb=sb, ng=ng, pt=pt: a.activation(out=pt[:, 0:ng, :], in_=c.ps[:, sb:sb + ng, :], func=AF.Exp),
                     reads=[c.psb[sb + j] for j in range(ng)], writes=[B_PT[pi]], name="exp")
                for j, (kb, c0) in enumerate(g):
                    if kb >= NKB_HALF + 4 * qb:
                        P.op("pool", lambda gp, j=j, c0=c0, pt=pt: gp.tensor_tensor(out=pt[:, j, c0:c0 + 128], in0=pt[:, j, c0:c0 + 128], in1=cm[:, :], op=ALU.mult),
                             reads=[B_PT[pi], B_cm], writes=[B_PT[pi]], name="cmask")
                b0 = state["blk"]

                def em(pe, g=g, pt=pt, b0=b0, ob=ob, nblk=nblk, V=V):
                    r = None
                    for j, (kb, c0) in enumerate(g):
                        r = pe.matmul(c.ps[0:65, ob, c0:512], V[:, kb, 0:65], pt[:, j, c0:512], start=(b0 + j == 0), stop=(b0 + j == nblk - 1))
                    return r
                state["blk"] += ng
                P.op("pe", em, reads=[B_PT[pi], B_V[bi]], writes=[c.psb[ob]], name="PV")

            emit_S(groups[0], 0)
            for gi in range(len(groups)):
                if gi + 1 < len(groups):
                    emit_S(groups[gi + 1], gi + 1)
                emit_exp_pv(groups[gi], gi)
            def ev(v, ob=ob, oi=oi):
                v.tensor_copy(out=osb[oi][0:64, :], in_=c.ps[0:64, ob, :])
                return v.tensor_copy(out=rsb[oi][64:65, :], in_=c.ps[64:65, ob, :])
            P.op("dve", ev, reads=[c.psb[ob]], writes=[B_osb[oi]], name="o_ev")
            P.dma("pool", lambda e, h=h, q0=q0, oi=oi: [e.dma_start(out=c.oT[h * 64:(h + 1) * 64, q0:q0 + 512], in_=osb[oi][0:64, :]),
                                                      e.dma_start(out=c.rsum[h:h + 1, q0:q0 + 512], in_=rsb[oi][64:65, :])],
                  ds_o[oi], 2, reads=[B_osb[oi]], writes=[c.B_oT, c.B_rsum], name="o_st")


def phase_e(P, c, ntiles=NT, nexp=NE):
    nc = P.nc
    P.sb_off = c.sb_phase_base
    ring = Ring(P, 4, name="ringe")
    c.ring = ring
    c.xT = P.sb("xTe", [128, KC, T], F32)
    oTt = P.sb("oTt", [128, KC, T], BF16)
    rbc = P.sb("rbc", [128, KC, T], F32)
    ytok = P.sb("ytok", [128, 4, D], F32, off=P.sb_off - 16384)
    hf = P.sb("hf", [128, KC, T], F32)
    c.hT = P.sb("hTe", [128, KC, T], BF16)
    c.sq = P.sb("sqe", [128, KC, T], BF16)
    Gs = P.sb("Gs", [128, NE, T], F32)
    c.aT = P.sb("aTe", [128, FC, T], BF16)
    c.th = [P.sb(f"the{i}", [128, T], F32) for i in range(3)]
    c.st = [P.sb(f"ste{i}", [128, T], F32) for i in range(6)]
    rw_s = P.sb("rw_s", [128, KC, NE], F32)
    rb_s = P.sb("rb_s", [128, 4, NE], F32)
    sel_s = P.sb("sel_s", [128, NE, 128], F32)
    lg = P.sb("lg", [128, 4, NE], F32)
    lg2 = P.sb("lg2", [128, 4, NE], F32)
    eq1 = P.sb("eq1", [128, 4, NE], F32)
    eq2 = P.sb("eq2", [128, 4, NE], F32)
    gt = P.sb("gt", [128, 4, NE], F32)
    m1 = P.sb("m1", [128, 4], F32)
    m2 = P.sb("m2", [128, 4], F32)
    p1 = P.sb("p1", [128, 4], F32)
    p2 = P.sb("p2", [128, 4], F32)
    gT_s = P.sb("gT_s", [128, T], F32)
    gT_p = P.sb("gT_p", [8, T], F32)
    print("SBUF used phase E:", P.sb_off)
    c.B_xT = [Buf(f"xTe{k}") for k in range(KC)]
    c.B_hT, c.B_sq = Buf("hTe"), Buf("sqe")
    c.B_aT = [Buf(f"aTe{k}") for k in range(FC)]
    c.B_th = [Buf(f"the{i}") for i in range(3)]
    c.B_st = [Buf(f"ste{i}") for i in range(6)]
    B_oTt, B_rbc, B_rt = Buf("oTt"), Buf("rbc"), Buf("rt")
    B_hf = [Buf("hf")] * KC
    B_G = [Buf(f"G{e}") for e in range(NE)]
    B_gT, B_gTp = Buf("gT"), Buf("gTp")
    B_rc = Buf("rconst")
    ds_in = [P.new_dsem(f"ein{i}") for i in range(3)]
    ds_m = [P.new_dsem(f"em{i}") for i in range(3)]
    ds_h = [P.new_dsem(f"eh{i}") for i in range(3)]
    ds_out = P.new_dsem("eout")
    B_out = Buf("out_d")
    B_x2d = [Buf(f"x2d{t}") for t in range(ntiles)]
    B_hd = [Buf(f"hd{t}") for t in range(ntiles)]
    B_gd = [Buf(f"gd{t}") for t in range(ntiles)]
    x2T_d = nc.dram_tensor("x2T_d", [128, KC, TOK], F32)
    hT_d = nc.dram_tensor("hT_d", [128, KC, TOK], BF16)
    gT_d = nc.dram_tensor("gT_d", [8, TOK], F32)
    cp = Ctx()
    cp.__dict__.update(c.__dict__)
    cp.xT = hf
    cp.B_xT = B_hf
    P.dma("sp", lambda e: [e.dma_start(out=rw_s[:, :, :], in_=c.rw[:, :, :]), e.dma_start(out=rb_s[:, :, :], in_=c.rb4[:, :, :]),
                           e.dma_start(out=sel_s[:, :, :], in_=c.sel[:, :, :])], P.new_dsem("rconst"), 3, writes=[B_rc], name="rconst_ld")
    P.op("dve", lambda v: v.memset(gT_s[:, :], 0.0), writes=[B_gT], name="gT_zero")
    X = mybir.AxisListType.X

    def prologue(t):
        t0 = t * T
        P.dma("sp", lambda e: [e.dma_start(out=hf[:, :, :], in_=c.x1T[:, :, t0:t0 + T])], ds_in[0], 1, reads=c.B_x1T, writes=B_hf, name="x1_ld")
        P.dma("sp", lambda e: [e.dma_start(out=oTt[:, :, :], in_=c.oT[:, t0:t0 + T].rearrange("(cc p) t -> p cc t", p=128))], ds_in[1], 1,
              reads=[c.B_oT], writes=[B_oTt], name="oT_ld")

        def ldr(e):
            r = []
            for hh in range(2):
                base = c.rsum[hh:hh + 1, t0:t0 + T]
                src = bass.AP(tensor=base.tensor, offset=base.offset, ap=[[0, 64], [2 * TOK, 8], [1, T]])
                r.append(e.dma_start(out=rbc[hh * 64:(hh + 1) * 64, :, :], in_=src))
            return r
        P.dma("sp", ldr, ds_in[2], 2, reads=[c.B_rsum], writes=[B_rbc], name="rsum_ld")
        P.op("dve", lambda v: v.reciprocal(out=rbc[:, :, :], in_=rbc[:, :, :]), reads=[B_rbc], writes=[B_rbc], name="recip")
        P.op("pool", lambda g: g.tensor_tensor(out=oTt[:, :, :], in0=oTt[:, :, :], in1=rbc[:, :, :], op=ALU.mult), reads=[B_rbc, B_oTt], writes=[B_oTt], name="o_norm")
        yield
        s_ = ring.load(lambda i: [(ring.v8[i][:, :, :], w8(c.w_o_b, 0, 1024))], c.B_w["att"], name="w_o")
        for dc in range(KC):
            b = psbank(c)
            mm_group(P, c, b, [(ring.v8[s_][:, k, dc * 128:(dc + 1) * 128], oTt[:, k, :]) for k in range(KC)], [ring.bufs[s_], B_oTt], T, name="w_o")
            P.op("dve", lambda v, dc=dc, b=b: v.tensor_tensor(out=hf[:, dc, :], in0=c.ps[:, b, :], in1=hf[:, dc, :], op=ALU.add),
                 reads=[c.psb[b], B_hf[dc]], writes=[B_hf[dc]], name="res_att")
        if c.debug == "x2":
            P.dma("act", lambda e: [e.dma_start(out=c.dbg_x2T[:, :, t0:t0 + T], in_=hf[:, :, :])], ds_out, 1, reads=B_hf, writes=[B_out], name="x2_st")
        P.dma("act", lambda e: [e.dma_start(out=x2T_d[:, :, t0:t0 + T], in_=hf[:, :, :])], ds_h[0], 1, reads=B_hf, writes=[B_x2d[t]], name="x2d_st")
        yield
        ri = rms_stats(P, cp, T, B_hf)
        apply_norm(P, cp, T, ri, V_FFN1, rbc, B_rbc, name="hf")
        P.op("act", lambda a: a.copy(out=oTt[:, :, :], in_=rbc[:, :, :]), reads=[B_rbc], writes=[B_oTt], name="hT_cast")
        P.dma("act", lambda e: [e.dma_start(out=hT_d[:, :, t0:t0 + T], in_=oTt[:, :, :])], ds_h[1], 1, reads=[B_oTt], writes=[B_hd[t]], name="hd_st")
        yield
        b = psbank(c)

        def emr(pe, b=b):
            r = None
            for tc in range(4):
                for k in range(KC):
                    r = pe.matmul(c.ps[:, b, tc * 8:(tc + 1) * 8], rbc[:, k, tc * 128:(tc + 1) * 128], rw_s[:, k, :], start=(k == 0), stop=(k == KC - 1))
            return r
        P.op("pe", emr, reads=[B_rbc, B_rc], writes=[c.psb[b]], name="router_mm")
        B_s = {k: Buf("rt_" + k) for k in ("lg", "m1", "eq1", "lg2", "m2", "eq2", "p2", "p1", "gt")}
        P.op("dve", lambda v, b=b: v.tensor_tensor(out=lg[:, :, :], in0=c.ps[:, b, 0:32].rearrange("p (a e) -> p a e", e=NE), in1=rb_s[:, :, :], op=ALU.add),
             reads=[c.psb[b], B_rc, B_rt], writes=[B_s["lg"]], name="lg")
        P.op("dve", lambda v: v.tensor_reduce(out=m1[:, :], in_=lg[:, :, :], axis=X, op=ALU.max), reads=[B_s["lg"]], writes=[B_s["m1"]], name="m1")
        P.op("dve", lambda v: v.tensor_tensor(out=eq1[:, :, :], in0=lg[:, :, :], in1=bcast_last(m1[:, :], NE), op=ALU.is_equal),
             reads=[B_s["lg"], B_s["m1"]], writes=[B_s["eq1"]], name="eq1")
        P.op("dve", lambda v: v.scalar_tensor_tensor(out=lg2[:, :, :], in0=eq1[:, :, :], scalar=-1e30, in1=lg[:, :, :], op0=ALU.mult, op1=ALU.add),
             reads=[B_s["eq1"], B_s["lg"]], writes=[B_s["lg2"]], name="lg2")
        P.op("dve", lambda v: v.tensor_reduce(out=m2[:, :], in_=lg2[:, :, :], axis=X, op=ALU.max), reads=[B_s["lg2"]], writes=[B_s["m2"]], name="m2")
        P.op("dve", lambda v: v.tensor_tensor(out=eq2[:, :, :], in0=lg2[:, :, :], in1=bcast_last(m2[:, :], NE), op=ALU.is_equal),
             reads=[B_s["lg2"], B_s["m2"]], writes=[B_s["eq2"]], name="eq2")
        P.op("dve", lambda v: v.tensor_tensor(out=p2[:, :], in0=m2[:, :], in1=m1[:, :], op=ALU.subtract), reads=[B_s["m1"], B_s["m2"]], writes=[B_s["p2"]], name="d21")
        P.op("act", lambda a: a.activation(out=p2[:, :], in_=p2[:, :], func=AF.Tanh, scale=0.5), reads=[B_s["p2"]], writes=[B_s["p2"]], name="tanh_r")
        P.op("dve", lambda v: v.tensor_scalar(out=p2[:, :], in0=p2[:, :], scalar1=0.5, scalar2=0.5, op0=ALU.mult, op1=ALU.add), reads=[B_s["p2"]], writes=[B_s["p2"]], name="p2")
        P.op("dve", lambda v: v.tensor_scalar(out=p1[:, :], in0=p2[:, :], scalar1=-1.0, scalar2=1.0, op0=ALU.mult, op1=ALU.add), reads=[B_s["p2"]], writes=[B_s["p1"]], name="p1")
        P.op("dve", lambda v: v.tensor_tensor(out=gt[:, :, :], in0=eq1[:, :, :], in1=bcast_last(p1[:, :], NE), op=ALU.mult), reads=[B_s["eq1"], B_s["p1"]], writes=[B_s["gt"]], name="g1")
        P.op("dve", lambda v: v.tensor_tensor(out=eq2[:, :, :], in0=eq2[:, :, :], in1=bcast_last(p2[:, :], NE), op=ALU.mult), reads=[B_s["eq2"], B_s["p2"]], writes=[B_s["eq2"]], name="g2")
        P.op("dve", lambda v: v.tensor_tensor(out=gt[:, :, :], in0=gt[:, :, :], in1=eq2[:, :, :], op=ALU.add), reads=[B_s["gt"], B_s["eq2"]], writes=[B_s["gt"], B_rt], name="gates")
        yield
        b = psbank(c)

        def emgt(pe, b=b):
            r = None
            for tc in range(4):
                r = pe.transpose(out=c.ps[0:8, b, tc * 128:(tc + 1) * 128], in_=gt[:, tc, :], identity=c.ident_f[:, :])
            return r
        P.op("pe", emgt, reads=[B_rt, c.B_const], writes=[c.psb[b]], name="gT")
        P.op("act", lambda a, b=b: a.copy(out=gT_p[0:8, :], in_=c.ps[0:8, b, :]), reads=[c.psb[b]], writes=[B_gTp], name="gT_ev")
        P.dma("act", lambda e: [e.dma_start(out=gT_d[:, t0:t0 + T], in_=gT_p[0:8, :])], ds_h[2], 1, reads=[B_gTp], writes=[B_gd[t]], name="gd_st")
        yield

    def run_all(gen):
        for _ in gen:
            pass

    run_all(prologue(0))
    for t in range(ntiles):
        t0 = t * T
        nxt = prologue(t + 1) if t + 1 < ntiles else None
        P.dma("sp", lambda e, t0=t0: [e.dma_start(out=c.xT[:, :, :], in_=x2T_d[:, :, t0:t0 + T])], ds_m[0], 1, reads=[B_x2d[t]], writes=c.B_xT, name="x2_ld")
        P.dma("sp", lambda e, t0=t0: [e.dma_start(out=c.hT[:, :, :], in_=hT_d[:, :, t0:t0 + T])], ds_m[1], 1, reads=[B_hd[t]], writes=[c.B_hT], name="h_ld")
        P.dma("sp", lambda e, t0=t0: [e.dma_start(out=gT_s[0:8, :], in_=gT_d[:, t0:t0 + T])], ds_m[2], 1, reads=[B_gd[t]], writes=[B_gT], name="g_ld")
        for e in range(NE):
            b = psbank(c)
            P.op("pe", lambda pe, b=b, e=e: pe.matmul(c.ps[:, b, :], sel_s[:, e, :], gT_s[:, :], start=True, stop=True),
                 reads=[B_gT, B_rc], writes=[c.psb[b]], name="G_bc")
            if e % 2 == 0:
                P.op("act", lambda a, b=b, e=e: a.copy(out=Gs[:, e, :], in_=c.ps[:, b, :]), reads=[c.psb[b]], writes=[B_G[e]], name="G_ev")
            else:
                P.op("dve", lambda v, b=b, e=e: v.tensor_copy(out=Gs[:, e, :], in_=c.ps[:, b, :]), reads=[c.psb[b]], writes=[B_G[e]], name="G_ev")
        if c.debug == "x2":
            if nxt is not None:
                run_all(nxt)
            continue
        for e in range(nexp):
            ffn_stage(P, c, c.moe_g_b[e], c.moe_u_b[e], c.moe_d_b[e], c.B_w[f"e{e}"], c.hT, c.B_hT, gate=(Gs[:, e, :], B_G[e]))
            if nxt is not None and 1 <= e <= 5:
                next(nxt)
        if nxt is not None and nexp < 6:
            run_all(nxt)
        if c.debug == "x3":
            P.dma("act", lambda e, t0=t0: [e.dma_start(out=c.dbg_x2T[:, :, t0:t0 + T], in_=c.xT[:, :, :])], ds_out, 1, reads=c.B_xT, writes=[B_out], name="x3_st")
            continue
        ri = rms_stats(P, c, T, c.B_xT)
        B_y = B_hf[0]
        apply_norm(P, c, T, ri, V_FIN, hf, B_y, name="yT")
        for tc in range(4):
            for kh in range(2):
                b = psbank(c)

                def emt(pe, tc=tc, kh=kh, b=b):
                    r = None
                    for kk in range(4):
                        k = kh * 4 + kk
                        r = pe.transpose(out=c.ps[:, b, kk * 128:(kk + 1) * 128], in_=hf[:, k, tc * 128:(tc + 1) * 128], identity=c.ident_f[:, :])
                    return r
                P.op("pe", emt, reads=[B_y, c.B_const], writes=[c.psb[b]], name="y_xpose")
                if kh == 0:
                    P.op("act", lambda a, tc=tc, kh=kh, b=b: a.copy(out=ytok[:, tc, kh * 512:(kh + 1) * 512], in_=c.ps[:, b, :]), reads=[c.psb[b]], writes=[B_rbc], name="y_ev")
                else:
                    P.op("dve", lambda v, tc=tc, kh=kh, b=b: v.tensor_copy(out=ytok[:, tc, kh * 512:(kh + 1) * 512], in_=c.ps[:, b, :]), reads=[c.psb[b]], writes=[B_rbc], name="y_ev")
        P.dma("act", lambda e, t0=t0: [e.dma_start(out=c.out[t0:t0 + T, :].rearrange("(tc p) d -> p tc d", p=128), in_=ytok[:, :, :])], ds_out, 1,
              reads=[B_rbc], writes=[B_out], name="out_st")


def host_prep(inp):
    f32 = np.float32
    x = np.asarray(inp["x"], f32)

    def pv(v):
        return np.ascontiguousarray(np.asarray(v, f32).reshape(8, 128).T)
    b_in = np.asarray(inp["conv_b_in"], f32)[0]
    vecs = np.concatenate([
        pv(inp["mix_norm"][0]), pv(inp["ffn_norm"][0]), pv(b_in[:1024]), pv(b_in[1024:]),
        pv(inp["conv_b_dw"][0]), pv(inp["conv_ln_g"][0]), pv(inp["conv_ln_b"][0]), pv(inp["conv_b_out"][0]),
        pv(inp["kv_norm"]), pv(inp["mix_norm"][1]), pv(inp["ffn_norm"][1]), pv(inp["final_norm"])], axis=1)
    wdw = np.ascontiguousarray(np.asarray(inp["conv_w_dw"], f32)[0].reshape(31, 8, 128).transpose(2, 1, 0))
    bf = np.asarray(inp["b_f"], f32).reshape(16, 1)
    rw = np.ascontiguousarray(np.asarray(inp["router_w"], f32)[0].reshape(8, 128, 8).transpose(1, 0, 2))
    rb4 = np.ascontiguousarray(np.broadcast_to(np.asarray(inp["router_b"], f32)[0][None, None, :], (128, 4, 8)))
    sel = np.zeros((128, 8, 128), f32)
    for e in range(8):
        sel[e, e, :] = 1.0
    ident = np.eye(128, dtype=f32)
    cmask = (np.arange(128)[:, None] <= np.arange(128)[None, :]).astype(ml_dtypes.bfloat16)
    common = dict(vecs=vecs, wdw=wdw, bf=bf, rw=rw, rb4=rb4, sel=sel, ident=ident, cmask=cmask,
                  conv_w_in=np.asarray(inp["conv_w_in"], f32)[0], conv_w_out=np.asarray(inp["conv_w_out"], f32)[0],
                  ffn_w_gate=np.asarray(inp["ffn_w_gate"], f32)[0], ffn_w_up=np.asarray(inp["ffn_w_up"], f32)[0],
                  ffn_w_down=np.asarray(inp["ffn_w_down"], f32)[0], w_kvf=np.asarray(inp["w_kvf"], f32),
                  w_q=np.asarray(inp["w_q"], f32)[0], w_o=np.asarray(inp["w_o"], f32)[0],
                  moe_w_gate=np.asarray(inp["moe_w_gate"], f32)[0], moe_w_up=np.asarray(inp["moe_w_up"], f32)[0],
                  moe_w_down=np.asarray(inp["moe_w_down"], f32)[0])
    maps = []
    for core in range(8):
        b, hf = core // 2, core % 2
        m = dict(common)
        m["x"] = np.ascontiguousarray(x[b, hf * 4096:(hf + 1) * 4096])
        if hf == 0:
            m["xhalo"] = np.zeros((32, 1024), f32)
            m["hmask"] = np.zeros((128, 1), f32)
            m["flagrow"] = np.full((1, 4096), -30000.0, dtype=ml_dtypes.bfloat16)
        else:
            m["xhalo"] = np.ascontiguousarray(x[b, 4096 - 32:4096])
            m["hmask"] = np.ones((128, 1), f32)
            m["flagrow"] = np.zeros((1, 4096), dtype=ml_dtypes.bfloat16)
        maps.append(m)
    return maps


def build(debug=None, ntiles=NT, ne_decl=NE, stop_after=None):
    nc = bass.Bass("TRN2", target_bir_lowering=False)
    c = Ctx()
    declare_io(nc, c, debug, ne_decl)
    P = Prog(nc)
    setup_common(P, c)
    convert_weights(P, c)
    c.sb_phase_base = P.sb_off
    alloc_phase_a(P, c)
    print("SBUF used after phase A alloc:", P.sb_off)
    phase_a(P, c, ntiles=ntiles, debug=debug)
    if debug not in ("xa", "a"):
        P.barrier()
        phase_b(P, c)
        if stop_after != "b":
            phase_c(P, c)
        P.barrier()
        if stop_after in ("b", "c"):
            dk = nc.dram_tensor("dbg_kaug", [H, 7, TOK], BF16, kind="ExternalOutput")
            dq = nc.dram_tensor("dbg_qaug", [H, 7, TOK], BF16, kind="ExternalOutput")
            dgi = nc.dram_tensor("dbg_gin", [112, TOK], BF16, kind="ExternalOutput")
            ds = P.new_dsem("dbg")
            P.dma("sp", lambda e: [e.dma_start(out=dk[:, :, :], in_=c.kaug_own[:, :, :]),
                                   e.dma_start(out=dq[:, :, :], in_=c.qaug[:, :, :]), e.dma_start(out=dgi[:, :], in_=c.ga[:, :])], ds, 3, name="dbg_out")
        else:
            phase_d(P, c)
            P.barrier()
            if stop_after == "d":
                do = nc.dram_tensor("dbg_oT", [D, TOK], BF16, kind="ExternalOutput")
                dr = nc.dram_tensor("dbg_rsum", [H, TOK], F32, kind="ExternalOutput")
                ds = P.new_dsem("dbg")
                P.dma("sp", lambda e: [e.dma_start(out=do[:, :], in_=c.oT[:, :]), e.dma_start(out=dr[:, :], in_=c.rsum[:, :])], ds, 2, name="dbg_out")
            else:
                ce = Ctx()
                ce.__dict__.update(c.__dict__)
                phase_e(P, ce, nexp=ne_decl)
    if debug in ("xa", "a"):
        c.dbg_x1T = nc.dram_tensor("dbg_x1T", [128, KC, TOK], F32, kind="ExternalOutput")
        c.dbg_gin = nc.dram_tensor("dbg_gin", [2160, TOK], BF16, kind="ExternalOutput")
        c.dbg_qT = nc.dram_tensor("dbg_qT", [D, TOK], BF16, kind="ExternalOutput")
        c.dbg_flog = nc.dram_tensor("dbg_flog", [16, TOK], F32, kind="ExternalOutput")
        ds = P.new_dsem("dbg")
        P.dma("sp", lambda e: [e.dma_start(out=c.dbg_x1T[:, :, :], in_=c.x1T[:, :, :]),
                               e.dma_start(out=c.dbg_gin[:, :], in_=c.gin[:, :]),
                               e.dma_start(out=c.dbg_qT[:, :], in_=c.qT[:, :]),
                               e.dma_start(out=c.dbg_flog[:, :], in_=c.flog[:, :])], ds, 4,
              reads=c.B_x1T + [c.B_gin, c.B_qT, c.B_flog], name="dbg_out")
    stats = P.emit_all()
    print("ops per engine (n, waits):", stats)
    return nc


def kernel(**inputs):
    maps = host_prep(inputs)
    nc = build(debug=None)
    res = run_bass_kernel_spmd(nc, maps, core_ids=list(range(8)))
    out = np.empty((4, 8192, 1024), np.float32)
    for core in range(8):
        out[core // 2, (core % 2) * 4096:(core % 2 + 1) * 4096] = res.results[core]["out"]
    return out
```

```python
import numpy as np
import ml_dtypes
import concourse.bass as bass
import concourse.mybir as mybir
from concourse.bass_utils import run_bass_kernel_spmd


F32 = mybir.dt.float32
BF16 = mybir.dt.bfloat16
AF = mybir.ActivationFunctionType
ALU = mybir.AluOpType

ENGS = ("pe", "act", "dve", "pool", "sp")


class Buf:
    __slots__ = ("name", "last_w", "readers", "dsem")

    def __init__(self, name):
        self.name = name
        self.last_w = None
        self.readers = []
        self.dsem = None


class Op:
    __slots__ = ("eng", "emit", "deps", "is_dma", "needs_inc", "semval", "dsem", "dval", "npieces", "idx", "name", "inc", "hoist")

    def __init__(self, eng, emit, is_dma=False, name=""):
        self.eng = eng
        self.emit = emit
        self.deps = []
        self.is_dma = is_dma
        self.needs_inc = False
        self.semval = None
        self.dsem = None
        self.dval = None
        self.npieces = 0
        self.name = name
        self.hoist = False


class DSem:
    __slots__ = ("sem", "total", "last_op")

    def __init__(self, sem):
        self.sem = sem
        self.total = 0
        self.last_op = None


class Prog:
    def __init__(self, nc):
        self.nc = nc
        self.ops = {e: [] for e in ENGS}
        self.all_ops = []
        self.esem = {}
        self.dsems = []
        self.pending_barrier = {e: [] for e in ENGS}
        self.sb_off = 16512
        self.sb_end = 229344
        self._n = 0

    def sb(self, name, shape, dtype, off=None):
        nbytes = int(np.prod(shape[1:])) * (2 if dtype == BF16 else 4)
        if off is None:
            off = (self.sb_off + 63) // 64 * 64
            self.sb_off = off + nbytes
            assert self.sb_off <= self.sb_end, f"SBUF overflow at {name}: {self.sb_off}"
        self._n += 1
        return self.nc.alloc_sbuf_tensor_at(f"{name}_{self._n}", list(shape), dtype, offset=off)

    def new_dsem(self, name):
        d = DSem(self.nc.alloc_semaphore(f"d_{name}_{len(self.dsems)}"))
        self.dsems.append(d)
        return d

    def _add(self, op, reads, writes):
        deps = []
        for b in reads:
            if b.last_w is not None:
                deps.append(b.last_w)
        for b in writes:
            if b.last_w is not None:
                deps.append(b.last_w)
            deps.extend(b.readers)
        deps.extend(self.pending_barrier[op.eng])
        self.pending_barrier[op.eng] = []
        seen = set()
        for d in deps:
            if d is op or id(d) in seen:
                continue
            seen.add(id(d))
            op.deps.append(d)
            if not d.is_dma:
                d.needs_inc = True
        for b in reads:
            b.readers.append(op)
        for b in writes:
            b.last_w = op
            b.readers = []
        op.idx = len(self.all_ops)
        self.ops[op.eng].append(op)
        self.all_ops.append(op)
        return op

    def op(self, eng, emit, reads=(), writes=(), name=""):
        return self._add(Op(eng, emit, False, name), list(reads), list(writes))

    def dma(self, q, emit, dsem, npieces, reads=(), writes=(), name="", inc=16):
        o = Op(q, emit, True, name)
        o.dsem = dsem
        o.npieces = npieces
        o.inc = inc
        if dsem.last_op is not None:
            o.deps.append(dsem.last_op)
        dsem.total += inc * npieces
        o.dval = dsem.total
        dsem.last_op = o
        return self._add(o, list(reads), list(writes))

    def barrier(self):
        lasts = []
        for e in ENGS:
            if self.ops[e]:
                for o in reversed(self.ops[e]):
                    if not o.is_dma:
                        lasts.append(o)
                        break
        for d in self.dsems:
            if d.last_op is not None:
                lasts.append(d.last_op)
        for e in ENGS:
            self.pending_barrier[e] = list(lasts)

    def emit_all(self, final_waits_eng="sp"):
        nc = self.nc
        for e in ("pe", "act", "dve", "pool"):
            self.esem[e] = nc.alloc_semaphore(f"e_{e}")
        for e in ENGS:
            cnt = 0
            for o in self.ops[e]:
                if o.is_dma:
                    continue
                if o.needs_inc:
                    cnt += 1
                    o.semval = cnt
        peidx = {id(o): i for i, o in enumerate(self.ops["pe"])}
        maxpe = {}
        last = {e: -1 for e in ENGS}
        for o in self.all_ops:
            m = last[o.eng]
            if o.eng == "pe":
                m = max(m, peidx[id(o)])
            for d in o.deps:
                m = max(m, maxpe[id(d)])
            maxpe[id(o)] = m
            last[o.eng] = m
        stats = {}
        with nc.Block() as block:
            def wait_list(e):
                waited = {}
                out = []
                for o in self.ops[e]:
                    ws = []
                    for d in o.deps:
                        if d.is_dma:
                            key, sem, val = id(d.dsem), d.dsem.sem, d.dval
                        else:
                            if d.eng == e and e == "pe":
                                continue
                            key, sem, val = d.eng, self.esem[d.eng], d.semval
                        if waited.get(key, 0) >= val:
                            continue
                        waited[key] = val
                        ws.append((sem, val, maxpe[id(d)]))
                    out.append(ws)
                return out, waited

            def run(e):
                def body(eng):
                    W, waited = wait_list(e)
                    ops = self.ops[e]
                    if e == "pe":
                        for j in range(1, len(ops)):
                            if not ops[j].hoist:
                                continue
                            keep = []
                            for w in W[j]:
                                if w[2] <= j - 2:
                                    W[j - 1].append(w)
                                else:
                                    keep.append(w)
                            W[j] = keep
                    nw = 0
                    for o, ws in zip(ops, W):
                        for (sem, val, _) in ws:
                            eng.wait_ge(sem, val)
                            nw += 1
                        r = o.emit(eng)
                        if o.is_dma:
                            assert len(r) == o.npieces, (o.name, len(r), o.npieces)
                            for ins in r:
                                ins.then_inc(o.dsem.sem, o.inc)
                        elif o.needs_inc:
                            r.then_inc(self.esem[e], 1)
                    if e == final_waits_eng:
                        for d in self.dsems:
                            if d.total and waited.get(id(d), 0) < d.total:
                                eng.wait_ge(d.sem, d.total)
                    stats[e] = (len(ops), nw)
                return body
            block.tensor(run("pe"))
            block.scalar(run("act"))
            block.vector(run("dve"))
            block.gpsimd(run("pool"))
            block.sync(run("sp"))
        return stats


D = 1024
KC = 8
T = 512
TOK = 4096
NT = TOK // T
FF = 2816
FC = 22
H = 16
CW = 31
NE = 8
EPS = 1e-6

V_MIX0, V_FFN0, V_BA, V_BG, V_BDW, V_LNG, V_LNB, V_BOUT, V_KVN, V_MIX1, V_FFN1, V_FIN = [8 * i for i in range(12)]
NV = 96


class Ctx:
    pass


def declare_io(nc, c, debug, ne_decl=NE):
    c.ne_decl = ne_decl
    def din(name, shape, dt=F32):
        return nc.dram_tensor(name, list(shape), dt, kind="ExternalInput")

    c.x = din("x", [TOK, D])
    c.xhalo = din("xhalo", [32, D])
    c.vecs = din("vecs", [128, NV])
    c.wdw = din("wdw", [128, KC, CW])
    c.bf = din("bf", [16, 1])
    c.hmask = din("hmask", [128, 1])
    c.flagrow = din("flagrow", [1, TOK], BF16)
    c.rw = din("rw", [128, KC, NE])
    c.rb4 = din("rb4", [128, 4, NE])
    c.sel = din("sel", [128, NE, 128])
    c.ident = din("ident", [128, 128])
    c.cmask = din("cmask", [128, 128], BF16)
    c.w_in = din("conv_w_in", [D, 2 * D])
    c.w_out = din("conv_w_out", [D, D])
    c.ffn_g = din("ffn_w_gate", [D, FF])
    c.ffn_u = din("ffn_w_up", [D, FF])
    c.ffn_d = din("ffn_w_down", [FF, D])
    c.w_kvf = din("w_kvf", [D, 2 * D + H])
    c.w_q = din("w_q", [D, D])
    c.w_o = din("w_o", [D, D])
    c.moe_g = din("moe_w_gate", [ne_decl, D, FF])
    c.moe_u = din("moe_w_up", [ne_decl, D, FF])
    c.moe_d = din("moe_w_down", [ne_decl, FF, D])

    def scr(name, shape, dt=BF16):
        return nc.dram_tensor(name, list(shape), dt)

    c.w_in_b = scr("w_in_b", [D, 2 * D])
    c.w_out_b = scr("w_out_b", [D, D])
    c.ffn_g_b = scr("ffn_g_b", [D, FF])
    c.ffn_u_b = scr("ffn_u_b", [D, FF])
    c.ffn_d_b = scr("ffn_d_b", [FF, D])
    c.w_kvf_b = scr("w_kvf_b", [D, 2 * D + H])
    c.w_q_b = scr("w_q_b", [D, D])
    c.w_o_b = scr("w_o_b", [D, D])
    c.moe_g_b = scr("moe_g_b", [ne_decl, D, FF])
    c.moe_u_b = scr("moe_u_b", [ne_decl, D, FF])
    c.moe_d_b = scr("moe_d_b", [ne_decl, FF, D])
    c.x1T = scr("x1T", [128, KC, TOK], F32)
    c.gk = [scr(f"gk{t}", [D, T]) for t in range(NT)]
    c.gko = [scr(f"gko{t}", [2 * D, T]) for t in range(NT)]
    c.gv = [scr(f"gv{t}", [T, D]) for t in range(NT)]
    c.gvo = [scr(f"gvo{t}", [2 * T, D]) for t in range(NT)]
    c.ga = scr("ga", [112, TOK])
    c.gao = scr("gao", [224, TOK])
    c.qT = scr("qT", [D, TOK])
    c.kaug_own = scr("kaug_own", [H, 7, TOK])
    c.qaug = scr("qaug", [H, 7, TOK])
    c.oT = scr("oT", [D, TOK])
    c.flog = scr("flog_d", [16, TOK], F32)
    c.rsum = scr("rsum", [H, TOK], F32)
    c.out = nc.dram_tensor("out", [TOK, D], F32, kind="ExternalOutput")
    c.debug = debug
    if debug in ("x2", "x3", "g"):
        c.dbg_x2T = nc.dram_tensor("dbg_x2T", [128, KC, TOK], F32, kind="ExternalOutput")


def setup_common(P, c):
    nc = P.nc
    c.ps = nc.alloc_psum_tensor("ps", [128, 8, 512], F32)
    c.psb = [Buf(f"psb{i}") for i in range(8)]
    c.ps_rr = 0
    c.ident_f = P.sb("ident_f", [128, 128], F32)
    c.ident_b = P.sb("ident_b", [128, 128], BF16)
    c.ones_b = P.sb("ones_b", [128, 128], BF16)
    c.vec = P.sb("vec", [128, NV], F32)
    c.vech = P.sb("vech", [128, 16], F32)
    c.wdw_s = P.sb("wdw_s", [128, KC, CW], F32)
    c.bf_s = P.sb("bf_s", [16, 1], F32)
    c.hmask_s = P.sb("hmask_s", [128, 1], F32)
    c.eps_t = P.sb("eps_t", [128, 1], F32)
    c.B_const = Buf("const")
    ds = P.new_dsem("const")
    c.ds_const = ds

    def ld(eng):
        r = []
        r.append(eng.dma_start(out=c.ident_f[:, :], in_=c.ident[:, :]))
        r.append(eng.dma_start(out=c.vec[:, :], in_=c.vecs[:, :]))
        r.append(eng.dma_start(out=c.wdw_s[:, :, :], in_=c.wdw[:, :, :]))
        r.append(eng.dma_start(out=c.bf_s[:, :], in_=c.bf[:, :]))
        r.append(eng.dma_start(out=c.hmask_s[:, :], in_=c.hmask[:, :]))
        return r
    P.dma("sp", ld, ds, 5, writes=[c.B_const], name="const_ld")
    c.B_const2 = Buf("const2")
    c.sb_phase_base = None

    def mk(eng):
        eng.tensor_copy(out=c.ident_b[:, :], in_=c.ident_f[:, :])
        eng.memset(c.ones_b[:, :], 1.0 / 1024.0)
        eng.memset(c.eps_t[:, :], EPS)
        return eng.tensor_scalar(out=c.vech[:, :], in0=c.vec[:, V_BA:V_BA + 16], scalar1=0.5, scalar2=None, op0=ALU.mult)
    P.op("dve", mk, reads=[c.B_const], writes=[c.B_const2], name="const_mk")


def psbank(c):
    i = c.ps_rr
    c.ps_rr = (c.ps_rr + 1) % 8
    return i


def convert_weights(P, c):
    c.B_w = {}

    def grp(name, pairs):
        ds = P.new_dsem("cv_" + name)
        b = Buf("wb_" + name)
        c.B_w[name] = b

        def em(eng, pairs=pairs):
            return [eng.dma_start(out=o, in_=i) for (o, i) in pairs]
        P.dma("pool", em, ds, len(pairs), writes=[b], name="cv_" + name)

    grp("in", [(c.w_in_b[:, :], c.w_in[:, :]), (c.w_out_b[:, :], c.w_out[:, :])])
    grp("ffn", [(c.ffn_g_b[:, :], c.ffn_g[:, :]), (c.ffn_u_b[:, :], c.ffn_u[:, :]),
                (c.ffn_d_b[0:1024, :], c.ffn_d[0:1024, :]), (c.ffn_d_b[1024:2048, :], c.ffn_d[1024:2048, :]),
                (c.ffn_d_b[2048:FF, :], c.ffn_d[2048:FF, :])])
    grp("att", [(c.w_kvf_b[:, :], c.w_kvf[:, :]), (c.w_q_b[:, :], c.w_q[:, :]), (c.w_o_b[:, :], c.w_o[:, :])])


def convert_expert(P, c, e):
    ds = P.new_dsem(f"cv_e{e}")
    b = Buf(f"wb_e{e}")
    c.B_w[f"e{e}"] = b
    pairs = [(c.moe_g_b[e, :, :], c.moe_g[e, :, :]), (c.moe_u_b[e, :, :], c.moe_u[e, :, :]),
             (c.moe_d_b[e, 0:1024, :], c.moe_d[e, 0:1024, :]), (c.moe_d_b[e, 1024:2048, :], c.moe_d[e, 1024:2048, :]),
             (c.moe_d_b[e, 2048:FF, :], c.moe_d[e, 2048:FF, :])]

    def em(eng, pairs=pairs):
        return [eng.dma_start(out=o, in_=i) for (o, i) in pairs]
    P.dma("pool", em, ds, len(pairs), writes=[b], name=f"cv_e{e}")


class Ring:
    def __init__(self, P, n, name="ring"):
        self.P = P
        self.n = n
        self.off = []
        self.bufs = [Buf(f"{name}{i}") for i in range(n)]
        self.ds = [P.new_dsem(f"{name}{i}") for i in range(n)]
        self.v8 = []
        self.vgu = []
        for i in range(n):
            t = P.sb(f"{name}{i}", [128, 8, 1024], BF16)
            off = P.sb_off - 16384
            self.v8.append(t)
            self.vgu.append(P.sb(f"{name}gu{i}", [128, 2, 8, 512], BF16, off=off))
        self.rr = 0

    def load(self, pieces_fn, wbuf, name="wl", q="sp"):
        i = self.rr
        self.rr = (self.rr + 1) % self.n
        pieces = pieces_fn(i)

        def em(eng, pieces=pieces):
            return [eng.dma_start(out=o, in_=s) for (o, s) in pieces]
        self.P.dma(q, em, self.ds[i], len(pieces), reads=([wbuf] if wbuf is not None else []), writes=[self.bufs[i]], name=name)
        return i


def w8(src, c0, ncols):
    return src[:, c0:c0 + ncols].rearrange("(kc p) c -> p kc c", p=128)


def alloc_phase_a(P, c):
    c.ring = Ring(P, 4)
    c.x_tok = P.sb("x_tok", [128, 4, D], F32)
    off = P.sb_off - 16384
    c.v = P.sb("v", [128, KC, T], F32, off=off)
    c.B_r1 = Buf("r1")
    c.B_v = [Buf(f"v{k}") for k in range(KC)]
    c.xT = P.sb("xT", [128, KC, T], F32)
    c.B_xT = [Buf(f"xT{k}") for k in range(KC)]
    c.sq = P.sb("sq", [128, KC, T], BF16)
    c.B_sq = Buf("sq")
    c.hT = P.sb("hT", [128, KC, T], BF16)
    c.B_hT = Buf("hT")
    c.ap_ = P.sb("aprime", [128, KC, T], BF16)
    c.B_ap = [Buf(f"ap{k}") for k in range(KC)]
    c.uT = P.sb("uT", [128, KC, 32 + T], BF16)
    c.B_uT = [Buf(f"uT{k}") for k in range(KC)]
    c.sT = P.sb("sT", [128, KC, T], BF16)
    c.B_sT = Buf("sT")
    c.aT = P.sb("aT", [128, FC, T], BF16)
    c.B_aT = [Buf(f"aT{k}") for k in range(FC)]
    offa = P.sb_off - FC * T * 2
    c.kst = P.sb("kst", [128, KC, T], BF16, off=offa)
    c.vst = P.sb("vst", [128, 4, D], BF16, off=offa + 8192)
    c.diag = P.sb("diag", [128, 2, CW, 128], BF16)
    c.B_diag = [Buf("diag0"), Buf("diag1")]
    c.th = [P.sb(f"th{i}", [128, T], F32) for i in range(3)]
    c.B_th = [Buf(f"th{i}") for i in range(3)]
    c.th_rr = 0
    c.st = [P.sb(f"st{i}", [128, T], F32) for i in range(6)]
    c.B_st = [Buf(f"st{i}") for i in range(6)]
    c.B_flog = Buf("flog")
    c.wf = P.sb("wf", [128, KC, 16], BF16)
    c.B_wf = Buf("wf")
    c.ds_x = P.new_dsem("x")
    c.ds_st = [P.new_dsem(f"store{i}") for i in range(4)]
    c.B_x1T = [Buf(f"x1T_{t}") for t in range(NT)]
    c.B_gk = [Buf(f"gk{t}") for t in range(NT)]
    c.B_gv = [Buf(f"gv{t}") for t in range(NT)]
    c.B_gko = [Buf(f"gko{t}") for t in range(NT)]
    c.B_gvo = [Buf(f"gvo{t}") for t in range(NT)]
    c.B_ga = Buf("ga")
    c.B_gao = Buf("gao")
    c.B_qT = Buf("qTd")


def mm_group(P, c, bank, pairs, reads, ncols, nrows=128, name="mm", col0=0):
    ps = c.ps
    n = len(pairs)

    def em(pe, pairs=pairs):
        r = None
        for i, (l, rr) in enumerate(pairs):
            r = pe.matmul(ps[0:nrows, bank, col0:col0 + ncols], l, rr, start=(i == 0), stop=(i == n - 1))
        return r
    o = P.op("pe", em, reads=reads, writes=[c.psb[bank]], name=name)
    o.hoist = True
    return o


def rms_stats(P, c, ncols, src_bufs):
    xT, sq = c.xT, c.sq
    P.op("act", lambda a: a.activation(out=sq[:, :, 0:ncols], in_=xT[:, :, 0:ncols], func=AF.Square),
         reads=src_bufs, writes=[c.B_sq], name="sq")
    b = psbank(c)
    mm_group(P, c, b, [(c.ones_b[:, :], sq[:, k, 0:ncols]) for k in range(KC)], [c.B_sq, c.B_const2], ncols, name="ms")
    i0, i1 = 0, 1
    t0, t1 = c.st[i0], c.st[i1]
    P.op("act", lambda a: a.activation(out=t0[:, 0:ncols], in_=c.ps[:, b, 0:ncols], func=AF.Ln, bias=c.eps_t[:, 0:1], scale=1.0),
         reads=[c.psb[b], c.B_const2], writes=[c.B_st[i0]], name="ms_ln")
    P.op("act", lambda a: a.activation(out=t1[:, 0:ncols], in_=t0[:, 0:ncols], func=AF.Exp, scale=-0.5),
         reads=[c.B_st[i0]], writes=[c.B_st[i1]], name="rstd")
    return i1


def apply_norm(P, c, ncols, rstd_i, gcol, dst, dstbuf, name="hn"):
    xT = c.xT
    rs = c.st[rstd_i]
    for k in range(KC):
        P.op("dve", lambda v, k=k: v.scalar_tensor_tensor(out=dst[:, k, 0:ncols], in0=xT[:, k, 0:ncols], scalar=c.vec[:, gcol + k:gcol + k + 1],
                                                         in1=rs[:, 0:ncols], op0=ALU.mult, op1=ALU.mult),
             reads=[c.B_xT[k], c.B_st[rstd_i], c.B_const], writes=[dstbuf], name=name)


def load_x_and_transpose(P, c, src_ap, ntok):
    nch = (ntok + 127) // 128
    if ntok >= 128:
        def em(eng):
            return [eng.dma_start(out=c.x_tok[:, 0:nch, :], in_=src_ap.rearrange("(c p) d -> p c d", p=128))]
    else:
        def em(eng):
            return [eng.dma_start(out=c.x_tok[0:ntok, 0, :], in_=src_ap)]
    P.dma("sp", em, c.ds_x, 1, writes=[c.B_r1] + c.B_v, name="x_ld")
    for k in range(KC):
        b = psbank(c)

        def emt(pe, k=k, b=b):
            r = None
            for tc in range(nch):
                n = min(128, ntok - tc * 128)
                r = pe.transpose(out=c.ps[:, b, tc * 128:tc * 128 + n], in_=c.x_tok[0:n, tc, k * 128:(k + 1) * 128], identity=c.ident_f[0:n, 0:n])
            return r
        P.op("pe", emt, reads=[c.B_r1, c.B_const], writes=[c.psb[b]], name="xpose")
        if k % 2 == 0:
            P.op("act", lambda a, k=k, b=b: a.copy(out=c.xT[:, k, 0:ntok], in_=c.ps[:, b, 0:ntok]), reads=[c.psb[b]], writes=[c.B_xT[k]], name="xT_ev")
        else:
            P.op("dve", lambda v, k=k, b=b: v.tensor_copy(out=c.xT[:, k, 0:ntok], in_=c.ps[:, b, 0:ntok]), reads=[c.psb[b]], writes=[c.B_xT[k]], name="xT_ev")


def glu_stage(P, c, ncols, ucol0, mask_halo=False, direct=False):
    ring = c.ring
    if direct:
        sa = ring.load(lambda i: [(ring.v8[i][:, :, :], w8(c.w_in, 0, 1024))], None, name="w_in_a_direct", q="pool")
        sg = ring.load(lambda i: [(ring.v8[i][:, :, :], w8(c.w_in, 1024, 1024))], None, name="w_in_g_direct", q="pool")
    else:
        sa = ring.load(lambda i: [(ring.v8[i][:, :, :], w8(c.w_in_b, 0, 1024))], c.B_w["in"], name="w_in_a")
        sg = ring.load(lambda i: [(ring.v8[i][:, :, :], w8(c.w_in_b, 1024, 1024))], c.B_w["in"], name="w_in_g")
    for oc in range(KC):
        b = psbank(c)
        mm_group(P, c, b, [(ring.v8[sa][:, k, oc * 128:(oc + 1) * 128], c.hT[:, k, 0:ncols]) for k in range(KC)],
                 [ring.bufs[sa], c.B_hT], ncols, name="w_in_a")
        P.op("act", lambda a, oc=oc, b=b: a.activation(out=c.ap_[:, oc, 0:ncols], in_=c.ps[:, b, 0:ncols], func=AF.Identity,
                                                       bias=c.vech[:, oc:oc + 1], scale=0.5),
             reads=[c.psb[b], c.B_const2], writes=[c.B_ap[oc]], name="aprime")
    for oc in range(KC):
        b = psbank(c)
        mm_group(P, c, b, [(ring.v8[sg][:, k, oc * 128:(oc + 1) * 128], c.hT[:, k, 0:ncols]) for k in range(KC)],
                 [ring.bufs[sg], c.B_hT], ncols, name="w_in_g")
        ti = c.th_rr
        c.th_rr = (c.th_rr + 1) % 3
        th = c.th[ti]
        P.op("act", lambda a, oc=oc, b=b, th=th: a.activation(out=th[:, 0:ncols], in_=c.ps[:, b, 0:ncols], func=AF.Tanh,
                                                              bias=c.vech[:, 8 + oc:9 + oc], scale=0.5),
             reads=[c.psb[b], c.B_const2], writes=[c.B_th[ti]], name="tanh")
        if not mask_halo:
            P.op("dve", lambda v, oc=oc, th=th: v.scalar_tensor_tensor(out=c.uT[:, oc, ucol0:ucol0 + ncols], in0=th[:, 0:ncols], scalar=1.0,
                                                                      in1=c.ap_[:, oc, 0:ncols], op0=ALU.add, op1=ALU.mult),
                 reads=[c.B_th[ti], c.B_ap[oc]], writes=[c.B_uT[oc]], name="glu")
        else:
            P.op("dve", lambda v, oc=oc, th=th: v.scalar_tensor_tensor(out=th[:, 0:ncols], in0=th[:, 0:ncols], scalar=1.0, in1=c.ap_[:, oc, 0:ncols], op0=ALU.add, op1=ALU.mult),
                 reads=[c.B_th[ti], c.B_ap[oc]], writes=[c.B_th[ti]], name="glu_h1")
            P.op("dve", lambda v, oc=oc, th=th: v.tensor_scalar(out=c.uT[:, oc, ucol0:ucol0 + ncols], in0=th[:, 0:ncols], scalar1=c.hmask_s[:, 0:1], scalar2=None, op0=ALU.mult),
                 reads=[c.B_th[ti], c.B_const], writes=[c.B_uT[oc]], name="glu_h2")


def conv_ln_stage(P, c):
    for k in range(KC):
        di = k % 2
        dg = c.diag

        def emd(g, k=k, di=di):
            r = None
            for j in range(CW):
                r = g.tensor_scalar(out=dg[:, di, j, :], in0=c.ident_b[:, :], scalar1=c.wdw_s[:, k, j:j + 1], scalar2=1.0, op0=ALU.mult, op1=ALU.mult)
            return r
        P.op("pool" if k % 2 == 0 else "dve", emd, reads=[c.B_const, c.B_const2], writes=[c.B_diag[di]], name="diag")
        b = psbank(c)
        mm_group(P, c, b, [(dg[:, di, j, :], c.uT[:, k, 2 + j:2 + j + T]) for j in range(CW)], [c.B_diag[di], c.B_uT[k]], T, name="conv")
        P.op("act", lambda a, k=k, b=b: a.activation(out=c.v[:, k, :], in_=c.ps[:, b, :], func=AF.Identity, bias=c.vec[:, V_BDW + k:V_BDW + k + 1], scale=1.0),
             reads=[c.psb[b], c.B_const], writes=[c.B_v[k]], name="v_ev")
        P.op("act", lambda a, k=k, b=b: a.activation(out=c.sq[:, k, :], in_=c.ps[:, b, :], func=AF.Square, bias=c.vec[:, V_BDW + k:V_BDW + k + 1], scale=1.0),
             reads=[c.psb[b], c.B_const], writes=[c.B_sq], name="v_sq")
        P.op("pool", lambda g, k=k: g.tensor_copy(out=c.ap_[:, k, :], in_=c.v[:, k, :]), reads=[c.B_v[k]], writes=[c.B_ap[k]], name="vb")
    bm = psbank(c)
    mm_group(P, c, bm, [(c.ones_b[:, :], c.ap_[:, k, :]) for k in range(KC)], c.B_ap + [c.B_const2], T, name="ln_mean")
    be = psbank(c)
    mm_group(P, c, be, [(c.ones_b[:, :], c.sq[:, k, :]) for k in range(KC)], [c.B_sq, c.B_const2], T, name="ln_ex2")
    mean, m2, var, rstd, Bt = c.st[2], c.st[3], c.st[0], c.st[1], c.st[4]
    P.op("act", lambda a: a.copy(out=mean[:, :], in_=c.ps[:, bm, :]), reads=[c.psb[bm]], writes=[c.B_st[2]], name="mean")
    P.op("dve", lambda v: v.tensor_tensor(out=m2[:, :], in0=mean[:, :], in1=mean[:, :], op=ALU.mult), reads=[c.B_st[2]], writes=[c.B_st[3]], name="m2")
    P.op("dve", lambda v: v.scalar_tensor_tensor(out=var[:, :], in0=c.ps[:, be, :], scalar=EPS, in1=m2[:, :], op0=ALU.add, op1=ALU.subtract),
         reads=[c.psb[be], c.B_st[3]], writes=[c.B_st[0]], name="var")
    P.op("act", lambda a: a.activation(out=var[:, :], in_=var[:, :], func=AF.Ln), reads=[c.B_st[0]], writes=[c.B_st[0]], name="ln_ln")
    P.op("act", lambda a: a.activation(out=rstd[:, :], in_=var[:, :], func=AF.Exp, scale=-0.5), reads=[c.B_st[0]], writes=[c.B_st[1]], name="ln_rstd")
    P.op("dve", lambda v: v.scalar_tensor_tensor(out=Bt[:, :], in0=mean[:, :], scalar=-1.0, in1=rstd[:, :], op0=ALU.mult, op1=ALU.mult),
         reads=[c.B_st[2], c.B_st[1]], writes=[c.B_st[4]], name="ln_B")
    for k in range(KC):
        ti = c.th_rr
        c.th_rr = (c.th_rr + 1) % 3
        th = c.th[ti]
        P.op("pool", lambda g, k=k, th=th: g.tensor_tensor(out=th[:, :], in0=c.v[:, k, :], in1=rstd[:, :], op=ALU.mult),
             reads=[c.B_v[k], c.B_st[1]], writes=[c.B_th[ti]], name="ln_t1")
        P.op("dve", lambda v, th=th: v.tensor_tensor(out=th[:, :], in0=th[:, :], in1=Bt[:, :], op=ALU.add),
             reads=[c.B_th[ti], c.B_st[4]], writes=[c.B_th[ti]], name="ln_t2")
        P.op("act", lambda a, k=k, th=th: a.activation(out=c.sT[:, k, :], in_=th[:, :], func=AF.Silu, bias=c.vec[:, V_LNB + k:V_LNB + k + 1],
                                                       scale=c.vec[:, V_LNG + k:V_LNG + k + 1]),
             reads=[c.B_th[ti], c.B_const], writes=[c.B_sT], name="ln_silu")


def halo_shift(P, c):
    for k in range(KC):
        P.op("pool", lambda g, k=k: g.tensor_copy(out=c.uT[:, k, 2:32], in_=c.uT[:, k, 2 + T:32 + T]), reads=[c.B_uT[k]], writes=[c.B_uT[k]], name="halo")


def wout_stage(P, c):
    ring = c.ring
    s = ring.load(lambda i: [(ring.v8[i][:, :, :], w8(c.w_out_b, 0, 1024))], c.B_w["in"], name="w_out")
    for dc in range(KC):
        b = psbank(c)
        mm_group(P, c, b, [(ring.v8[s][:, k, dc * 128:(dc + 1) * 128], c.sT[:, k, :]) for k in range(KC)], [ring.bufs[s], c.B_sT], T, name="w_out")
        P.op("dve", lambda v, dc=dc, b=b: v.scalar_tensor_tensor(out=c.xT[:, dc, :], in0=c.ps[:, b, :], scalar=c.vec[:, V_BOUT + dc:V_BOUT + dc + 1],
                                                                in1=c.xT[:, dc, :], op0=ALU.add, op1=ALU.add),
             reads=[c.psb[b], c.B_xT[dc], c.B_const], writes=[c.B_xT[dc]], name="res1")


FGROUPS = [(0, 4), (4, 4), (8, 4), (12, 4), (16, 4), (20, 2)]
DHALF = [[(0, 8), (8, 4)], [(12, 8), (20, 2)]]


def ffn_stage(P, c, g_b, u_b, d_b, wbuf, hsrc, hbuf, gate=None):
    ring = c.ring
    for (f0, nf) in FGROUPS:
        s = ring.load(lambda i, f0=f0, nf=nf: [(ring.vgu[i][:, 0, :, 0:nf * 128], w8(g_b, f0 * 128, nf * 128)),
                                              (ring.vgu[i][:, 1, :, 0:nf * 128], w8(u_b, f0 * 128, nf * 128))], wbuf, name="w_gu")
        for j in range(nf):
            fc = f0 + j
            bg = psbank(c)
            mm_group(P, c, bg, [(ring.vgu[s][:, 0, k, j * 128:(j + 1) * 128], hsrc[:, k, :]) for k in range(KC)], [ring.bufs[s], hbuf], T, name="ffn_g")
            bu = psbank(c)
            mm_group(P, c, bu, [(ring.vgu[s][:, 1, k, j * 128:(j + 1) * 128], hsrc[:, k, :]) for k in range(KC)], [ring.bufs[s], hbuf], T, name="ffn_u")
            ti = c.th_rr
            c.th_rr = (c.th_rr + 1) % 3
            th = c.th[ti]
            P.op("act", lambda a, bg=bg, th=th: a.activation(out=th[:, :], in_=c.ps[:, bg, :], func=AF.Silu), reads=[c.psb[bg]], writes=[c.B_th[ti]], name="silu")
            P.op("dve", lambda v, bu=bu, th=th, fc=fc: v.tensor_tensor(out=c.aT[:, fc, :], in0=th[:, :], in1=c.ps[:, bu, :], op=ALU.mult),
                 reads=[c.B_th[ti], c.psb[bu]], writes=[c.B_aT[fc]], name="a_mul")
    for half in range(2):
        slots = []
        for (f0, nf) in DHALF[half]:
            s = ring.load(lambda i, f0=f0, nf=nf: [(ring.v8[i][:, 0:nf, :], d_b[f0 * 128:(f0 + nf) * 128, :].rearrange("(fc p) c -> p fc c", p=128))], wbuf, name="w_d")
            slots.append((s, f0, nf))
        for dc in range(KC):
            b = psbank(c)
            pairs = []
            reads = []
            for (s, f0, nf) in slots:
                reads.append(ring.bufs[s])
                for j in range(nf):
                    pairs.append((ring.v8[s][:, j, dc * 128:(dc + 1) * 128], c.aT[:, f0 + j, :]))
                    reads.append(c.B_aT[f0 + j])
            mm_group(P, c, b, pairs, reads, T, name="ffn_d")
            if gate is None:
                P.op("dve", lambda v, dc=dc, b=b: v.tensor_tensor(out=c.xT[:, dc, :], in0=c.ps[:, b, :], in1=c.xT[:, dc, :], op=ALU.add),
                     reads=[c.psb[b], c.B_xT[dc]], writes=[c.B_xT[dc]], name="res2")
            else:
                gt, gbuf = gate
                ti = c.th_rr
                c.th_rr = (c.th_rr + 1) % 3
                th = c.th[ti]
                P.op("dve", lambda v, b=b, th=th: v.tensor_tensor(out=th[:, :], in0=c.ps[:, b, :], in1=gt, op=ALU.mult),
                     reads=[c.psb[b], gbuf], writes=[c.B_th[ti]], name="gmul")
                P.op("pool", lambda g, dc=dc, th=th: g.tensor_tensor(out=c.xT[:, dc, :], in0=th[:, :], in1=c.xT[:, dc, :], op=ALU.add),
                     reads=[c.B_th[ti], c.B_xT[dc]], writes=[c.B_xT[dc]], name="res_moe")


def allgather(P, c, src, dst, bsrc, bdst):
    ds = P.new_dsem("cc")

    def em(g):
        return [g.collective_compute("AllGather", ALU.bypass, replica_groups=[[0, 1], [2, 3], [4, 5], [6, 7]],
                                     ins=[src.ap()], outs=[dst.ap()])]
    P.dma("pool", em, ds, 1, reads=[bsrc], writes=[bdst], name="allgather", inc=1)


def proj_stage(P, c, t):
    ring = c.ring
    t0 = t * T
    ri = rms_stats(P, c, T, c.B_xT)
    apply_norm(P, c, T, ri, V_KVN, c.hT, c.B_hT, name="hn_kv")
    apply_norm(P, c, T, ri, V_MIX1, c.sT, c.B_sT, name="hn_q")
    s = ring.load(lambda i: [(ring.v8[i][:, :, :], w8(c.w_kvf_b, 0, 1024))], c.B_w["att"], name="w_k")
    B_kst = Buf("kst")
    for cc in range(KC):
        b = psbank(c)
        mm_group(P, c, b, [(ring.v8[s][:, k, cc * 128:(cc + 1) * 128], c.hT[:, k, :]) for k in range(KC)], [ring.bufs[s], c.B_hT], T, name="k_mm")
        P.op("act", lambda a, cc=cc, b=b: a.copy(out=c.kst[:, cc, :], in_=c.ps[:, b, :]), reads=[c.psb[b]], writes=[c.B_aT[cc]], name="k_ev")
    P.dma("act", lambda e: [e.dma_start(out=c.gk[t][:, :].rearrange("(cc p) t -> p cc t", p=128), in_=c.kst[:, :, :])],
          c.ds_st[0], 1, reads=c.B_aT[0:8], writes=[c.B_gk[t]], name="k_st")
    allgather(P, c, c.gk[t], c.gko[t], c.B_gk[t], c.B_gko[t])
    s = ring.load(lambda i: [(ring.v8[i][:, :, :], w8(c.w_kvf_b, 1024, 1024))], c.B_w["att"], name="w_v")
    for tc in range(4):
        for hf in range(2):
            b = psbank(c)
            mm_group(P, c, b, [(c.hT[:, k, tc * 128:(tc + 1) * 128], ring.v8[s][:, k, hf * 512:(hf + 1) * 512]) for k in range(KC)],
                     [ring.bufs[s], c.B_hT], 512, name="v_mm")
            P.op("dve", lambda v, tc=tc, hf=hf, b=b: v.tensor_copy(out=c.vst[:, tc, hf * 512:(hf + 1) * 512], in_=c.ps[:, b, :]),
                 reads=[c.psb[b]], writes=[c.B_aT[8 + tc * 2 + hf]], name="v_ev")
    P.dma("act", lambda e: [e.dma_start(out=c.gv[t][:, :].rearrange("(tc p) d -> p tc d", p=128), in_=c.vst[:, :, :])],
          c.ds_st[1], 1, reads=c.B_aT[8:16], writes=[c.B_gv[t]], name="v_st")
    allgather(P, c, c.gv[t], c.gvo[t], c.B_gv[t], c.B_gvo[t])
    b = psbank(c)
    mm_group(P, c, b, [(c.wf[:, k, :], c.hT[:, k, :]) for k in range(KC)], [c.B_wf, c.B_hT], T, nrows=16, name="f_mm")
    P.op("act", lambda a, b=b: a.activation(out=c.st[5][0:16, :], in_=c.ps[0:16, b, :], func=AF.Identity, bias=c.bf_s[:, 0:1], scale=1.0),
         reads=[c.psb[b], c.B_const], writes=[c.B_st[5]], name="f_ev")
    P.dma("act", lambda e: [e.dma_start(out=c.flog[:, t0:t0 + T], in_=c.st[5][0:16, :])], c.ds_st[3], 1, reads=[c.B_st[5]], writes=[c.B_flog], name="f_st")
    s = ring.load(lambda i: [(ring.v8[i][:, :, :], w8(c.w_q_b, 0, 1024))], c.B_w["att"], name="w_q")
    for cc in range(KC):
        b = psbank(c)
        mm_group(P, c, b, [(ring.v8[s][:, k, cc * 128:(cc + 1) * 128], c.sT[:, k, :]) for k in range(KC)], [ring.bufs[s], c.B_sT], T, name="q_mm")
        P.op("act", lambda a, cc=cc, b=b: a.activation(out=c.kst[:, cc, :], in_=c.ps[:, b, :], func=AF.Copy, scale=0.125),
             reads=[c.psb[b]], writes=[c.B_aT[cc]], name="q_ev")
    P.dma("act", lambda e: [e.dma_start(out=c.qT[:, t0:t0 + T].rearrange("(cc p) t -> p cc t", p=128), in_=c.kst[:, :, :])],
          c.ds_st[2], 1, reads=c.B_aT[0:8], writes=[c.B_qT], name="q_st")


def phase_a(P, c, ntiles=NT, debug=None):
    load_x_and_transpose(P, c, c.xhalo[:, :], 32)
    ri = rms_stats(P, c, 32, c.B_xT)
    apply_norm(P, c, 32, ri, V_MIX0, c.hT, c.B_hT)
    glu_stage(P, c, 32, 0, mask_halo=True, direct=True)
    for t in range(ntiles):
        t0 = t * T
        load_x_and_transpose(P, c, c.x[t0:t0 + T, :], T)
        ri = rms_stats(P, c, T, c.B_xT)
        apply_norm(P, c, T, ri, V_MIX0, c.hT, c.B_hT)
        glu_stage(P, c, T, 32, direct=(t == 0))
        if t == 0:
            convert_weights(P, c)
        conv_ln_stage(P, c)
        halo_shift(P, c)
        wout_stage(P, c)
        if debug == "xa":
            P.dma("act", lambda e, t0=t0: [e.dma_start(out=c.x1T[:, :, t0:t0 + T], in_=c.xT[:, :, :])], c.ds_st[3], 1, reads=c.B_xT, writes=[c.B_x1T[t]], name="xa_st")
            continue
        ri = rms_stats(P, c, T, c.B_xT)
        apply_norm(P, c, T, ri, V_FFN0, c.hT, c.B_hT)
        ffn_stage(P, c, c.ffn_g_b, c.ffn_u_b, c.ffn_d_b, c.B_w["ffn"], c.hT, c.B_hT)
        P.dma("act", lambda e, t0=t0: [e.dma_start(out=c.x1T[:, :, t0:t0 + T], in_=c.xT[:, :, :])], c.ds_st[3], 1, reads=c.B_xT, writes=[c.B_x1T[t]], name="x1_st")
        if t == 0:
            P.dma("sp", lambda e: [e.dma_start(out=c.wf[:, :, :], in_=w8(c.w_kvf_b, 2048, 16))], P.new_dsem("wf"), 1, reads=[c.B_w["att"]], writes=[c.B_wf], name="wf_ld")
        proj_stage(P, c, t)
        if t < c.ne_decl:
            convert_expert(P, c, t)


NKB_HALF = 32
GRP = 3


def bcast_last(ap2d, n):
    a = [list(x) for x in ap2d.ap]
    return bass.AP(tensor=ap2d.tensor, offset=ap2d.offset, ap=a + [[0, n]])


def phase_b(P, c):
    base = c.sb_phase_base
    P.sb_off = base
    fl = P.sb("fl", [16, TOK], F32)
    ones = P.sb("ones16", [16, TOK], F32)
    C = P.sb("C", [16, TOK], F32)
    e1 = P.sb("e1", [16, TOK], F32)
    hs = [P.sb(f"h{i}", [16, TOK], BF16) for i in range(3)]
    rs_ = [P.sb(f"r{i}", [16, TOK], BF16) for i in range(3)]
    ns = [P.sb(f"n{i}", [16, TOK], BF16) for i in range(3)]
    oneb = P.sb("oneb", [16, TOK], BF16)
    zerob = P.sb("zerob", [16, TOK], BF16)
    B = {k: Buf("pb_" + k) for k in ("fl", "ones", "C", "e1", "h", "r", "n", "cb")}
    ds = P.new_dsem("pb")
    P.dma("sp", lambda e: [e.dma_start(out=fl[:, :], in_=c.flog[:, :])], ds, 1, reads=[c.B_flog], writes=[B["fl"]], name="fl_ld")

    def mk(v):
        v.memset(ones[:, :], 1.0)
        v.memset(oneb[:, :], 1.0)
        return v.memset(zerob[:, :], 0.0)
    P.op("dve", mk, writes=[B["ones"], B["cb"]], name="pb_const")
    P.op("act", lambda a: a.activation(out=fl[:, :], in_=fl[:, :], func=AF.Exp, scale=-1.0), reads=[B["fl"]], writes=[B["fl"]], name="exp_f")
    P.op("act", lambda a: a.activation(out=fl[:, :], in_=fl[:, :], func=AF.Ln, bias=1.0, scale=1.0), reads=[B["fl"]], writes=[B["fl"]], name="ln_f")
    P.op("dve", lambda v: v.tensor_tensor_scan(out=C[:, :], data0=ones[:, :], data1=fl[:, :], initial=0.0, op0=ALU.mult, op1=ALU.add),
         reads=[B["fl"], B["ones"]], writes=[B["C"]], name="scan")

    def split(src, outs, bsrc, bout, nm):
        bs = [Buf(nm + str(i)) for i in range(3)]
        P.op("dve", lambda v: v.tensor_copy(out=outs[0][:, :], in_=src[:, :]), reads=[bsrc], writes=[bs[0]], name=nm)
        P.op("dve", lambda v: v.tensor_tensor(out=e1[:, :], in0=src[:, :], in1=outs[0][:, :], op=ALU.subtract), reads=[bsrc, bs[0]], writes=[B["e1"]], name=nm)
        P.op("dve", lambda v: v.tensor_copy(out=outs[1][:, :], in_=e1[:, :]), reads=[B["e1"]], writes=[bs[1]], name=nm)
        P.op("dve", lambda v: v.tensor_tensor(out=e1[:, :], in0=e1[:, :], in1=outs[1][:, :], op=ALU.subtract), reads=[B["e1"], bs[1]], writes=[B["e1"]], name=nm)
        P.op("dve", lambda v: v.tensor_copy(out=outs[2][:, :], in_=e1[:, :]), reads=[B["e1"]] + bs[0:2], writes=[bout], name=nm)
    split(C, hs, B["C"], B["h"], "split_h")

    def neg(v):
        r = None
        for i in range(3):
            r = v.tensor_scalar(out=ns[i][:, :], in0=hs[i][:, :], scalar1=-1.0, scalar2=None, op0=ALU.mult)
        return r
    P.op("dve", neg, reads=[B["h"]], writes=[B["n"]], name="neg_h")
    P.op("dve", lambda v: v.tensor_scalar(out=fl[:, :], in0=C[:, :], scalar1=C[:, TOK - 1:TOK], scalar2=None, op0=ALU.subtract),
         reads=[B["C"], B["fl"]], writes=[B["fl"]], name="R")
    split(fl, rs_, B["fl"], B["r"], "split_r")
    c.B_kaug_own = Buf("kaug_own")
    c.B_qaug = Buf("qaug_d")
    gin_aug = c.ga[:, :].rearrange("(h r) t -> h r t", r=7)

    def st(e):
        r = []
        for i in range(3):
            r.append(e.dma_start(out=c.kaug_own[:, i, :], in_=oneb[:, :]))
            r.append(e.dma_start(out=c.kaug_own[:, 3 + i, :], in_=hs[i][:, :]))
            r.append(e.dma_start(out=gin_aug[:, i, :], in_=oneb[:, :]))
            r.append(e.dma_start(out=gin_aug[:, 3 + i, :], in_=rs_[i][:, :]))
            r.append(e.dma_start(out=c.qaug[:, i, :], in_=ns[i][:, :]))
            r.append(e.dma_start(out=c.qaug[:, 3 + i, :], in_=oneb[:, :]))
        r.append(e.dma_start(out=c.kaug_own[:, 6, :], in_=zerob[:, :]))
        r.append(e.dma_start(out=gin_aug[:, 6, :], in_=zerob[:, :]))
        r.append(e.dma_start(out=c.qaug[:, 6, :], in_=oneb[:, :]))
        return r
    P.dma("sp", st, ds, 21, reads=[B["h"], B["r"], B["n"], B["cb"]], writes=[c.B_kaug_own, c.B_ga, c.B_qaug], name="aug_st")


def phase_c(P, c):
    allgather(P, c, c.ga, c.gao, c.B_ga, c.B_gao)


def phase_d(P, c, nheads=H):
    nc = P.nc
    P.sb_off = c.sb_phase_base
    Kt = [P.sb(f"Kt{i}", [128, 2 * TOK], BF16) for i in range(2)]
    Vt = [P.sb(f"Vt{i}", [128, 2 * NKB_HALF, 65], BF16) for i in range(2)]
    Qt = [P.sb(f"Qt{i}", [128, TOK], BF16) for i in range(2)]
    PT = [P.sb(f"PT{i}", [128, GRP, 512], BF16) for i in range(3)]
    osb = [P.sb(f"osb{i}", [128, 512], BF16) for i in range(2)]
    rsb = [P.sb(f"rsb{i}", [128, 512], F32) for i in range(2)]
    cm = P.sb("cm", [128, 128], BF16)
    B_K = [Buf(f"Kt{i}") for i in range(2)]
    B_V = [Buf(f"Vt{i}") for i in range(2)]
    B_Q = [Buf(f"Qt{i}") for i in range(2)]
    B_PT = [Buf(f"PT{i}") for i in range(3)]
    B_osb = [Buf(f"osb{i}") for i in range(2)]
    B_cm = Buf("cm")
    ds_k = [P.new_dsem(f"k{i}") for i in range(2)]
    ds_o = [P.new_dsem(f"o{i}") for i in range(2)]
    ds_m = P.new_dsem("cm")
    c.B_oT = Buf("oT_d")
    c.B_rsum = Buf("rsum_d")
    P.dma("sp", lambda e: [e.dma_start(out=cm[:, :], in_=c.cmask[:, :])], ds_m, 1, writes=[B_cm], name="cm_ld")

    def ones_col(v):
        v.memset(Vt[0][:, :, 64:65], 1.0)
        return v.memset(Vt[1][:, :, 64:65], 1.0)
    P.op("dve", ones_col, writes=B_V, name="ones_col")

    pt_rr = 0
    o_rr = 0
    def load_head(h):
        bi = h % 2
        K, V, Q = Kt[bi], Vt[bi], Qt[bi]

        def ldk(e, h=h, K=K, V=V, Q=Q):
            r = []
            for t in range(NT):
                r.append(e.dma_start(out=K[0:64, t * T:(t + 1) * T], in_=c.gko[t][h * 64:(h + 1) * 64, :]))
                r.append(e.dma_start(out=K[0:64, TOK + t * T:TOK + (t + 1) * T], in_=c.gk[t][h * 64:(h + 1) * 64, :]))
                r.append(e.dma_start(out=V[:, 4 * t:4 * t + 4, 0:64], in_=c.gvo[t][0:T, h * 64:(h + 1) * 64].rearrange("(kb p) d -> p kb d", p=128)))
                r.append(e.dma_start(out=V[:, NKB_HALF + 4 * t:NKB_HALF + 4 * t + 4, 0:64], in_=c.gv[t][:, h * 64:(h + 1) * 64].rearrange("(kb p) d -> p kb d", p=128)))
            r.append(e.dma_start(out=K[64:70, 0:TOK], in_=c.gao[h * 7:h * 7 + 6, :]))
            r.append(e.dma_start(out=K[70:71, 0:TOK], in_=c.flagrow[:, :]))
            r.append(e.dma_start(out=K[64:71, TOK:2 * TOK], in_=c.kaug_own[h, :, :]))
            r.append(e.dma_start(out=Q[0:64, :], in_=c.qT[h * 64:(h + 1) * 64, :]))
            r.append(e.dma_start(out=Q[64:71, :], in_=c.qaug[h, :, :]))
            return r
        P.dma("sp", ldk, ds_k[bi], 4 * NT + 5, reads=c.B_gko + c.B_gk + c.B_gvo + c.B_gv + [c.B_gao, c.B_kaug_own, c.B_qT, c.B_qaug], writes=[B_K[bi], B_V[bi], B_Q[bi]], name="kvq_ld")

    load_head(0)
    for h in range(nheads):
        bi = h % 2
        K, V, Q = Kt[bi], Vt[bi], Qt[bi]
        if h + 1 < nheads:
            load_head(h + 1)

        for qb in range(NT):
            q0 = qb * 512
            blocks = [(kb, 0) for kb in range(NKB_HALF)] + [(NKB_HALF + j, 0) for j in range(4 * qb)]
            blocks += [(NKB_HALF + 4 * qb + i, 128 * i) for i in range(4)]
            groups = [blocks[i:i + GRP] for i in range(0, len(blocks), GRP)]
            ob = 6 + (o_rr % 2)
            oi = o_rr % 2
            o_rr += 1
            nblk = len(blocks)

            def emit_S(g, gi):
                sb = (gi % 2) * GRP

                def em(pe, g=g, sb=sb, K=K, Q=Q, q0=q0):
                    r = None
                    for j, (kb, c0) in enumerate(g):
                        r = pe.matmul(c.ps[:, sb + j, c0:512], K[0:71, kb * 128:(kb + 1) * 128], Q[0:71, q0 + c0:q0 + 512], start=True, stop=True)
                    return r
                P.op("pe", em, reads=[B_K[bi], B_Q[bi]], writes=[c.psb[sb + j] for j in range(len(g))], name="S")

            state = {"blk": 0}

            def emit_exp_pv(g, gi):
                nonlocal pt_rr
                sb = (gi % 2) * GRP
                pi = pt_rr % 3
                pt_rr += 1
                pt = PT[pi]
                ng = len(g)
                P.op("act", lambda a, sb=sb, ng=ng, pt=pt: a.activation(out=pt[:, 0:ng, :], in_=c.ps[:, sb:sb + ng, :], func=AF.Exp),
                     reads=[c.psb[sb + j] for j in range(ng)], writes=[B_PT[pi]], name="exp")
                for j, (kb, c0) in enumerate(g):
                    if kb >= NKB_HALF + 4 * qb:
                        P.op("pool", lambda gp, j=j, c0=c0, pt=pt: gp.tensor_tensor(out=pt[:, j, c0:c0 + 128], in0=pt[:, j, c0:c0 + 128], in1=cm[:, :], op=ALU.mult),
                             reads=[B_PT[pi], B_cm], writes=[B_PT[pi]], name="cmask")
                b0 = state["blk"]

                def em(pe, g=g, pt=pt, b0=b0, ob=ob, nblk=nblk, V=V):
                    r = None
                    for j, (kb, c0) in enumerate(g):
                        r = pe.matmul(c.ps[0:65, ob, c0:512], V[:, kb, 0:65], pt[:, j, c0:512], start=(b0 + j == 0), stop=(b0 + j == nblk - 1))
                    return r
                state["blk"] += ng
                P.op("pe", em, reads=[B_PT[pi], B_V[bi]], writes=[c.psb[ob]], name="PV")

            emit_S(groups[0], 0)
            for gi in range(len(groups)):
                if gi + 1 < len(groups):
                    emit_S(groups[gi + 1], gi + 1)
                emit_exp_pv(groups[gi], gi)
            def ev(v, ob=ob, oi=oi):
                v.tensor_copy(out=osb[oi][0:64, :], in_=c.ps[0:64, ob, :])
                return v.tensor_copy(out=rsb[oi][64:65, :], in_=c.ps[64:65, ob, :])
            P.op("dve", ev, reads=[c.psb[ob]], writes=[B_osb[oi]], name="o_ev")
            P.dma("pool", lambda e, h=h, q0=q0, oi=oi: [e.dma_start(out=c.oT[h * 64:(h + 1) * 64, q0:q0 + 512], in_=osb[oi][0:64, :]),
                                                      e.dma_start(out=c.rsum[h:h + 1, q0:q0 + 512], in_=rsb[oi][64:65, :])],
                  ds_o[oi], 2, reads=[B_osb[oi]], writes=[c.B_oT, c.B_rsum], name="o_st")


def phase_e(P, c, ntiles=NT, nexp=NE):
    nc = P.nc
    P.sb_off = c.sb_phase_base
    ring = Ring(P, 4, name="ringe")
    c.ring = ring
    c.xT = P.sb("xTe", [128, KC, T], F32)
    oTt = P.sb("oTt", [128, KC, T], BF16)
    rbc = P.sb("rbc", [128, KC, T], F32)
    ytok = P.sb("ytok", [128, 4, D], F32, off=P.sb_off - 16384)
    hf = P.sb("hf", [128, KC, T], F32)
    c.hT = P.sb("hTe", [128, KC, T], BF16)
    c.sq = P.sb("sqe", [128, KC, T], BF16)
    Gs = P.sb("Gs", [128, NE, T], F32)
    c.aT = P.sb("aTe", [128, FC, T], BF16)
    c.th = [P.sb(f"the{i}", [128, T], F32) for i in range(3)]
    c.st = [P.sb(f"ste{i}", [128, T], F32) for i in range(6)]
    rw_s = P.sb("rw_s", [128, KC, NE], F32)
    rb_s = P.sb("rb_s", [128, 4, NE], F32)
    sel_s = P.sb("sel_s", [128, NE, 128], F32)
    lg = P.sb("lg", [128, 4, NE], F32)
    lg2 = P.sb("lg2", [128, 4, NE], F32)
    eq1 = P.sb("eq1", [128, 4, NE], F32)
    eq2 = P.sb("eq2", [128, 4, NE], F32)
    gt = P.sb("gt", [128, 4, NE], F32)
    m1 = P.sb("m1", [128, 4], F32)
    m2 = P.sb("m2", [128, 4], F32)
    p1 = P.sb("p1", [128, 4], F32)
    p2 = P.sb("p2", [128, 4], F32)
    gT_s = P.sb("gT_s", [128, T], F32)
    gT_p = P.sb("gT_p", [8, T], F32)
    print("SBUF used phase E:", P.sb_off)
    c.B_xT = [Buf(f"xTe{k}") for k in range(KC)]
    c.B_hT, c.B_sq = Buf("hTe"), Buf("sqe")
    c.B_aT = [Buf(f"aTe{k}") for k in range(FC)]
    c.B_th = [Buf(f"the{i}") for i in range(3)]
    c.B_st = [Buf(f"ste{i}") for i in range(6)]
    B_oTt, B_rbc, B_rt = Buf("oTt"), Buf("rbc"), Buf("rt")
    B_hf = [Buf("hf")] * KC
    B_G = [Buf(f"G{e}") for e in range(NE)]
    B_gT, B_gTp = Buf("gT"), Buf("gTp")
    B_rc = Buf("rconst")
    ds_in = [P.new_dsem(f"ein{i}") for i in range(3)]
    ds_m = [P.new_dsem(f"em{i}") for i in range(3)]
    ds_h = [P.new_dsem(f"eh{i}") for i in range(3)]
    ds_out = P.new_dsem("eout")
    B_out = Buf("out_d")
    B_x2d = [Buf(f"x2d{t}") for t in range(ntiles)]
    B_hd = [Buf(f"hd{t}") for t in range(ntiles)]
    B_gd = [Buf(f"gd{t}") for t in range(ntiles)]
    x2T_d = nc.dram_tensor("x2T_d", [128, KC, TOK], F32)
    hT_d = nc.dram_tensor("hT_d", [128, KC, TOK], BF16)
    gT_d = nc.dram_tensor("gT_d", [8, TOK], F32)
    cp = Ctx()
    cp.__dict__.update(c.__dict__)
    cp.xT = hf
    cp.B_xT = B_hf
    P.dma("sp", lambda e: [e.dma_start(out=rw_s[:, :, :], in_=c.rw[:, :, :]), e.dma_start(out=rb_s[:, :, :], in_=c.rb4[:, :, :]),
                           e.dma_start(out=sel_s[:, :, :], in_=c.sel[:, :, :])], P.new_dsem("rconst"), 3, writes=[B_rc], name="rconst_ld")
    P.op("dve", lambda v: v.memset(gT_s[:, :], 0.0), writes=[B_gT], name="gT_zero")
    X = mybir.AxisListType.X

    def prologue(t):
        t0 = t * T
        P.dma("sp", lambda e: [e.dma_start(out=hf[:, :, :], in_=c.x1T[:, :, t0:t0 + T])], ds_in[0], 1, reads=c.B_x1T, writes=B_hf, name="x1_ld")
        P.dma("sp", lambda e: [e.dma_start(out=oTt[:, :, :], in_=c.oT[:, t0:t0 + T].rearrange("(cc p) t -> p cc t", p=128))], ds_in[1], 1,
              reads=[c.B_oT], writes=[B_oTt], name="oT_ld")

        def ldr(e):
            r = []
            for hh in range(2):
                base = c.rsum[hh:hh + 1, t0:t0 + T]
                src = bass.AP(tensor=base.tensor, offset=base.offset, ap=[[0, 64], [2 * TOK, 8], [1, T]])
                r.append(e.dma_start(out=rbc[hh * 64:(hh + 1) * 64, :, :], in_=src))
            return r
        P.dma("sp", ldr, ds_in[2], 2, reads=[c.B_rsum], writes=[B_rbc], name="rsum_ld")
        P.op("dve", lambda v: v.reciprocal(out=rbc[:, :, :], in_=rbc[:, :, :]), reads=[B_rbc], writes=[B_rbc], name="recip")
        P.op("pool", lambda g: g.tensor_tensor(out=oTt[:, :, :], in0=oTt[:, :, :], in1=rbc[:, :, :], op=ALU.mult), reads=[B_rbc, B_oTt], writes=[B_oTt], name="o_norm")
        yield
        s_ = ring.load(lambda i: [(ring.v8[i][:, :, :], w8(c.w_o_b, 0, 1024))], c.B_w["att"], name="w_o")
        for dc in range(KC):
            b = psbank(c)
            mm_group(P, c, b, [(ring.v8[s_][:, k, dc * 128:(dc + 1) * 128], oTt[:, k, :]) for k in range(KC)], [ring.bufs[s_], B_oTt], T, name="w_o")
            P.op("dve", lambda v, dc=dc, b=b: v.tensor_tensor(out=hf[:, dc, :], in0=c.ps[:, b, :], in1=hf[:, dc, :], op=ALU.add),
                 reads=[c.psb[b], B_hf[dc]], writes=[B_hf[dc]], name="res_att")
        if c.debug == "x2":
            P.dma("act", lambda e: [e.dma_start(out=c.dbg_x2T[:, :, t0:t0 + T], in_=hf[:, :, :])], ds_out, 1, reads=B_hf, writes=[B_out], name="x2_st")
        P.dma("act", lambda e: [e.dma_start(out=x2T_d[:, :, t0:t0 + T], in_=hf[:, :, :])], ds_h[0], 1, reads=B_hf, writes=[B_x2d[t]], name="x2d_st")
        yield
        ri = rms_stats(P, cp, T, B_hf)
        apply_norm(P, cp, T, ri, V_FFN1, rbc, B_rbc, name="hf")
        P.op("act", lambda a: a.copy(out=oTt[:, :, :], in_=rbc[:, :, :]), reads=[B_rbc], writes=[B_oTt], name="hT_cast")
        P.dma("act", lambda e: [e.dma_start(out=hT_d[:, :, t0:t0 + T], in_=oTt[:, :, :])], ds_h[1], 1, reads=[B_oTt], writes=[B_hd[t]], name="hd_st")
        yield
        b = psbank(c)

        def emr(pe, b=b):
            r = None
            for tc in range(4):
                for k in range(KC):
                    r = pe.matmul(c.ps[:, b, tc * 8:(tc + 1) * 8], rbc[:, k, tc * 128:(tc + 1) * 128], rw_s[:, k, :], start=(k == 0), stop=(k == KC - 1))
            return r
        P.op("pe", emr, reads=[B_rbc, B_rc], writes=[c.psb[b]], name="router_mm")
        B_s = {k: Buf("rt_" + k) for k in ("lg", "m1", "eq1", "lg2", "m2", "eq2", "p2", "p1", "gt")}
        P.op("dve", lambda v, b=b: v.tensor_tensor(out=lg[:, :, :], in0=c.ps[:, b, 0:32].rearrange("p (a e) -> p a e", e=NE), in1=rb_s[:, :, :], op=ALU.add),
             reads=[c.psb[b], B_rc, B_rt], writes=[B_s["lg"]], name="lg")
        P.op("dve", lambda v: v.tensor_reduce(out=m1[:, :], in_=lg[:, :, :], axis=X, op=ALU.max), reads=[B_s["lg"]], writes=[B_s["m1"]], name="m1")
        P.op("dve", lambda v: v.tensor_tensor(out=eq1[:, :, :], in0=lg[:, :, :], in1=bcast_last(m1[:, :], NE), op=ALU.is_equal),
             reads=[B_s["lg"], B_s["m1"]], writes=[B_s["eq1"]], name="eq1")
        P.op("dve", lambda v: v.scalar_tensor_tensor(out=lg2[:, :, :], in0=eq1[:, :, :], scalar=-1e30, in1=lg[:, :, :], op0=ALU.mult, op1=ALU.add),
             reads=[B_s["eq1"], B_s["lg"]], writes=[B_s["lg2"]], name="lg2")
        P.op("dve", lambda v: v.tensor_reduce(out=m2[:, :], in_=lg2[:, :, :], axis=X, op=ALU.max), reads=[B_s["lg2"]], writes=[B_s["m2"]], name="m2")
        P.op("dve", lambda v: v.tensor_tensor(out=eq2[:, :, :], in0=lg2[:, :, :], in1=bcast_last(m2[:, :], NE), op=ALU.is_equal),
             reads=[B_s["lg2"], B_s["m2"]], writes=[B_s["eq2"]], name="eq2")
        P.op("dve", lambda v: v.tensor_tensor(out=p2[:, :], in0=m2[:, :], in1=m1[:, :], op=ALU.subtract), reads=[B_s["m1"], B_s["m2"]], writes=[B_s["p2"]], name="d21")
        P.op("act", lambda a: a.activation(out=p2[:, :], in_=p2[:, :], func=AF.Tanh, scale=0.5), reads=[B_s["p2"]], writes=[B_s["p2"]], name="tanh_r")
        P.op("dve", lambda v: v.tensor_scalar(out=p2[:, :], in0=p2[:, :], scalar1=0.5, scalar2=0.5, op0=ALU.mult, op1=ALU.add), reads=[B_s["p2"]], writes=[B_s["p2"]], name="p2")
        P.op("dve", lambda v: v.tensor_scalar(out=p1[:, :], in0=p2[:, :], scalar1=-1.0, scalar2=1.0, op0=ALU.mult, op1=ALU.add), reads=[B_s["p2"]], writes=[B_s["p1"]], name="p1")
        P.op("dve", lambda v: v.tensor_tensor(out=gt[:, :, :], in0=eq1[:, :, :], in1=bcast_last(p1[:, :], NE), op=ALU.mult), reads=[B_s["eq1"], B_s["p1"]], writes=[B_s["gt"]], name="g1")
        P.op("dve", lambda v: v.tensor_tensor(out=eq2[:, :, :], in0=eq2[:, :, :], in1=bcast_last(p2[:, :], NE), op=ALU.mult), reads=[B_s["eq2"], B_s["p2"]], writes=[B_s["eq2"]], name="g2")
        P.op("dve", lambda v: v.tensor_tensor(out=gt[:, :, :], in0=gt[:, :, :], in1=eq2[:, :, :], op=ALU.add), reads=[B_s["gt"], B_s["eq2"]], writes=[B_s["gt"], B_rt], name="gates")
        yield
        b = psbank(c)

        def emgt(pe, b=b):
            r = None
            for tc in range(4):
                r = pe.transpose(out=c.ps[0:8, b, tc * 128:(tc + 1) * 128], in_=gt[:, tc, :], identity=c.ident_f[:, :])
            return r
        P.op("pe", emgt, reads=[B_rt, c.B_const], writes=[c.psb[b]], name="gT")
        P.op("act", lambda a, b=b: a.copy(out=gT_p[0:8, :], in_=c.ps[0:8, b, :]), reads=[c.psb[b]], writes=[B_gTp], name="gT_ev")
        P.dma("act", lambda e: [e.dma_start(out=gT_d[:, t0:t0 + T], in_=gT_p[0:8, :])], ds_h[2], 1, reads=[B_gTp], writes=[B_gd[t]], name="gd_st")
        yield

    def run_all(gen):
        for _ in gen:
            pass

    run_all(prologue(0))
    for t in range(ntiles):
        t0 = t * T
        nxt = prologue(t + 1) if t + 1 < ntiles else None
        P.dma("sp", lambda e, t0=t0: [e.dma_start(out=c.xT[:, :, :], in_=x2T_d[:, :, t0:t0 + T])], ds_m[0], 1, reads=[B_x2d[t]], writes=c.B_xT, name="x2_ld")
        P.dma("sp", lambda e, t0=t0: [e.dma_start(out=c.hT[:, :, :], in_=hT_d[:, :, t0:t0 + T])], ds_m[1], 1, reads=[B_hd[t]], writes=[c.B_hT], name="h_ld")
        P.dma("sp", lambda e, t0=t0: [e.dma_start(out=gT_s[0:8, :], in_=gT_d[:, t0:t0 + T])], ds_m[2], 1, reads=[B_gd[t]], writes=[B_gT], name="g_ld")
        for e in range(NE):
            b = psbank(c)
            P.op("pe", lambda pe, b=b, e=e: pe.matmul(c.ps[:, b, :], sel_s[:, e, :], gT_s[:, :], start=True, stop=True),
                 reads=[B_gT, B_rc], writes=[c.psb[b]], name="G_bc")
            if e % 2 == 0:
                P.op("act", lambda a, b=b, e=e: a.copy(out=Gs[:, e, :], in_=c.ps[:, b, :]), reads=[c.psb[b]], writes=[B_G[e]], name="G_ev")
            else:
                P.op("dve", lambda v, b=b, e=e: v.tensor_copy(out=Gs[:, e, :], in_=c.ps[:, b, :]), reads=[c.psb[b]], writes=[B_G[e]], name="G_ev")
        if c.debug == "x2":
            if nxt is not None:
                run_all(nxt)
            continue
        for e in range(nexp):
            ffn_stage(P, c, c.moe_g_b[e], c.moe_u_b[e], c.moe_d_b[e], c.B_w[f"e{e}"], c.hT, c.B_hT, gate=(Gs[:, e, :], B_G[e]))
            if nxt is not None and 1 <= e <= 5:
                next(nxt)
        if nxt is not None and nexp < 6:
            run_all(nxt)
        if c.debug == "x3":
            P.dma("act", lambda e, t0=t0: [e.dma_start(out=c.dbg_x2T[:, :, t0:t0 + T], in_=c.xT[:, :, :])], ds_out, 1, reads=c.B_xT, writes=[B_out], name="x3_st")
            continue
        ri = rms_stats(P, c, T, c.B_xT)
        B_y = B_hf[0]
        apply_norm(P, c, T, ri, V_FIN, hf, B_y, name="yT")
        for tc in range(4):
            for kh in range(2):
                b = psbank(c)

                def emt(pe, tc=tc, kh=kh, b=b):
                    r = None
                    for kk in range(4):
                        k = kh * 4 + kk
                        r = pe.transpose(out=c.ps[:, b, kk * 128:(kk + 1) * 128], in_=hf[:, k, tc * 128:(tc + 1) * 128], identity=c.ident_f[:, :])
                    return r
                P.op("pe", emt, reads=[B_y, c.B_const], writes=[c.psb[b]], name="y_xpose")
                if kh == 0:
                    P.op("act", lambda a, tc=tc, kh=kh, b=b: a.copy(out=ytok[:, tc, kh * 512:(kh + 1) * 512], in_=c.ps[:, b, :]), reads=[c.psb[b]], writes=[B_rbc], name="y_ev")
                else:
                    P.op("dve", lambda v, tc=tc, kh=kh, b=b: v.tensor_copy(out=ytok[:, tc, kh * 512:(kh + 1) * 512], in_=c.ps[:, b, :]), reads=[c.psb[b]], writes=[B_rbc], name="y_ev")
        P.dma("act", lambda e, t0=t0: [e.dma_start(out=c.out[t0:t0 + T, :].rearrange("(tc p) d -> p tc d", p=128), in_=ytok[:, :, :])], ds_out, 1,
              reads=[B_rbc], writes=[B_out], name="out_st")


def host_prep(inp):
    f32 = np.float32
    x = np.asarray(inp["x"], f32)

    def pv(v):
        return np.ascontiguousarray(np.asarray(v, f32).reshape(8, 128).T)
    b_in = np.asarray(inp["conv_b_in"], f32)[0]
    vecs = np.concatenate([
        pv(inp["mix_norm"][0]), pv(inp["ffn_norm"][0]), pv(b_in[:1024]), pv(b_in[1024:]),
        pv(inp["conv_b_dw"][0]), pv(inp["conv_ln_g"][0]), pv(inp["conv_ln_b"][0]), pv(inp["conv_b_out"][0]),
        pv(inp["kv_norm"]), pv(inp["mix_norm"][1]), pv(inp["ffn_norm"][1]), pv(inp["final_norm"])], axis=1)
    wdw = np.ascontiguousarray(np.asarray(inp["conv_w_dw"], f32)[0].reshape(31, 8, 128).transpose(2, 1, 0))
    bf = np.asarray(inp["b_f"], f32).reshape(16, 1)
    rw = np.ascontiguousarray(np.asarray(inp["router_w"], f32)[0].reshape(8, 128, 8).transpose(1, 0, 2))
    rb4 = np.ascontiguousarray(np.broadcast_to(np.asarray(inp["router_b"], f32)[0][None, None, :], (128, 4, 8)))
    sel = np.zeros((128, 8, 128), f32)
    for e in range(8):
        sel[e, e, :] = 1.0
    ident = np.eye(128, dtype=f32)
    cmask = (np.arange(128)[:, None] <= np.arange(128)[None, :]).astype(ml_dtypes.bfloat16)
    common = dict(vecs=vecs, wdw=wdw, bf=bf, rw=rw, rb4=rb4, sel=sel, ident=ident, cmask=cmask,
                  conv_w_in=np.asarray(inp["conv_w_in"], f32)[0], conv_w_out=np.asarray(inp["conv_w_out"], f32)[0],
                  ffn_w_gate=np.asarray(inp["ffn_w_gate"], f32)[0], ffn_w_up=np.asarray(inp["ffn_w_up"], f32)[0],
                  ffn_w_down=np.asarray(inp["ffn_w_down"], f32)[0], w_kvf=np.asarray(inp["w_kvf"], f32),
                  w_q=np.asarray(inp["w_q"], f32)[0], w_o=np.asarray(inp["w_o"], f32)[0],
                  moe_w_gate=np.asarray(inp["moe_w_gate"], f32)[0], moe_w_up=np.asarray(inp["moe_w_up"], f32)[0],
                  moe_w_down=np.asarray(inp["moe_w_down"], f32)[0])
    maps = []
    for core in range(8):
        b, hf = core // 2, core % 2
        m = dict(common)
        m["x"] = np.ascontiguousarray(x[b, hf * 4096:(hf + 1) * 4096])
        if hf == 0:
            m["xhalo"] = np.zeros((32, 1024), f32)
            m["hmask"] = np.zeros((128, 1), f32)
            m["flagrow"] = np.full((1, 4096), -30000.0, dtype=ml_dtypes.bfloat16)
        else:
            m["xhalo"] = np.ascontiguousarray(x[b, 4096 - 32:4096])
            m["hmask"] = np.ones((128, 1), f32)
            m["flagrow"] = np.zeros((1, 4096), dtype=ml_dtypes.bfloat16)
        maps.append(m)
    return maps


def build(debug=None, ntiles=NT, ne_decl=NE, stop_after=None):
    nc = bass.Bass("TRN2", target_bir_lowering=False)
    c = Ctx()
    declare_io(nc, c, debug, ne_decl)
    P = Prog(nc)
    setup_common(P, c)
    c.sb_phase_base = P.sb_off
    alloc_phase_a(P, c)
    print("SBUF used after phase A alloc:", P.sb_off)
    phase_a(P, c, ntiles=ntiles, debug=debug)
    if debug not in ("xa", "a"):
        P.barrier()
        phase_b(P, c)
        if stop_after != "b":
            phase_c(P, c)
        P.barrier()
        if stop_after in ("b", "c"):
            dk = nc.dram_tensor("dbg_kaug", [H, 7, TOK], BF16, kind="ExternalOutput")
            dq = nc.dram_tensor("dbg_qaug", [H, 7, TOK], BF16, kind="ExternalOutput")
            dgi = nc.dram_tensor("dbg_gin", [112, TOK], BF16, kind="ExternalOutput")
            ds = P.new_dsem("dbg")
            P.dma("sp", lambda e: [e.dma_start(out=dk[:, :, :], in_=c.kaug_own[:, :, :]),
                                   e.dma_start(out=dq[:, :, :], in_=c.qaug[:, :, :]), e.dma_start(out=dgi[:, :], in_=c.ga[:, :])], ds, 3, name="dbg_out")
        else:
            phase_d(P, c)
            P.barrier()
            if stop_after == "d":
                do = nc.dram_tensor("dbg_oT", [D, TOK], BF16, kind="ExternalOutput")
                dr = nc.dram_tensor("dbg_rsum", [H, TOK], F32, kind="ExternalOutput")
                ds = P.new_dsem("dbg")
                P.dma("sp", lambda e: [e.dma_start(out=do[:, :], in_=c.oT[:, :]), e.dma_start(out=dr[:, :], in_=c.rsum[:, :])], ds, 2, name="dbg_out")
            else:
                ce = Ctx()
                ce.__dict__.update(c.__dict__)
                phase_e(P, ce, nexp=ne_decl)
    if debug in ("xa", "a"):
        c.dbg_x1T = nc.dram_tensor("dbg_x1T", [128, KC, TOK], F32, kind="ExternalOutput")
        c.dbg_gin = nc.dram_tensor("dbg_gin", [2160, TOK], BF16, kind="ExternalOutput")
        c.dbg_qT = nc.dram_tensor("dbg_qT", [D, TOK], BF16, kind="ExternalOutput")
        c.dbg_flog = nc.dram_tensor("dbg_flog", [16, TOK], F32, kind="ExternalOutput")
        ds = P.new_dsem("dbg")
        P.dma("sp", lambda e: [e.dma_start(out=c.dbg_x1T[:, :, :], in_=c.x1T[:, :, :]),
                               e.dma_start(out=c.dbg_gin[:, :], in_=c.gin[:, :]),
                               e.dma_start(out=c.dbg_qT[:, :], in_=c.qT[:, :]),
                               e.dma_start(out=c.dbg_flog[:, :], in_=c.flog[:, :])], ds, 4,
              reads=c.B_x1T + [c.B_gin, c.B_qT, c.B_flog], name="dbg_out")
    stats = P.emit_all()
    print("ops per engine (n, waits):", stats)
    return nc


def kernel(**inputs):
    maps = host_prep(inputs)
    nc = build(debug=None)
    res = run_bass_kernel_spmd(nc, maps, core_ids=list(range(8)))
    out = np.empty((4, 8192, 1024), np.float32)
    for core in range(8):
        out[core // 2, (core % 2) * 4096:(core % 2 + 1) * 4096] = res.results[core]["out"]
    return out
```

```python
import numpy as np
import ml_dtypes
import concourse.bass as bass
import concourse.mybir as mybir
from concourse.bass_utils import run_bass_kernel_spmd


F32 = mybir.dt.float32
BF16 = mybir.dt.bfloat16
AF = mybir.ActivationFunctionType
ALU = mybir.AluOpType

ENGS = ("pe", "act", "dve", "pool", "sp")


class Buf:
    __slots__ = ("name", "last_w", "readers", "dsem")

    def __init__(self, name):
        self.name = name
        self.last_w = None
        self.readers = []
        self.dsem = None


class Op:
    __slots__ = ("eng", "emit", "deps", "is_dma", "needs_inc", "semval", "dsem", "dval", "npieces", "idx", "name", "inc", "hoist")

    def __init__(self, eng, emit, is_dma=False, name=""):
        self.eng = eng
        self.emit = emit
        self.deps = []
        self.is_dma = is_dma
        self.needs_inc = False
        self.semval = None
        self.dsem = None
        self.dval = None
        self.npieces = 0
        self.name = name
        self.hoist = False


class DSem:
    __slots__ = ("sem", "total", "last_op", "nobarrier")

    def __init__(self, sem):
        self.sem = sem
        self.total = 0
        self.last_op = None
        self.nobarrier = False


class Prog:
    def __init__(self, nc):
        self.nc = nc
        self.ops = {e: [] for e in ENGS}
        self.all_ops = []
        self.esem = {}
        self.dsems = []
        self.pending_barrier = {e: [] for e in ENGS}
        self.sb_off = 16512
        self.sb_end = 229344
        self._n = 0

    def sb(self, name, shape, dtype, off=None):
        nbytes = int(np.prod(shape[1:])) * (2 if dtype == BF16 else 4)
        if off is None:
            off = (self.sb_off + 63) // 64 * 64
            self.sb_off = off + nbytes
            assert self.sb_off <= self.sb_end, f"SBUF overflow at {name}: {self.sb_off}"
        self._n += 1
        return self.nc.alloc_sbuf_tensor_at(f"{name}_{self._n}", list(shape), dtype, offset=off)

    def new_dsem(self, name, nobarrier=False):
        d = DSem(self.nc.alloc_semaphore(f"d_{name}_{len(self.dsems)}"))
        d.nobarrier = nobarrier
        self.dsems.append(d)
        return d

    def _add(self, op, reads, writes):
        deps = []
        for b in reads:
            if b.last_w is not None:
                deps.append(b.last_w)
        for b in writes:
            if b.last_w is not None:
                deps.append(b.last_w)
            deps.extend(b.readers)
        deps.extend(self.pending_barrier[op.eng])
        self.pending_barrier[op.eng] = []
        seen = set()
        for d in deps:
            if d is op or id(d) in seen:
                continue
            seen.add(id(d))
            op.deps.append(d)
            if not d.is_dma:
                d.needs_inc = True
        for b in reads:
            b.readers.append(op)
        for b in writes:
            b.last_w = op
            b.readers = []
        op.idx = len(self.all_ops)
        self.ops[op.eng].append(op)
        self.all_ops.append(op)
        return op

    def op(self, eng, emit, reads=(), writes=(), name=""):
        return self._add(Op(eng, emit, False, name), list(reads), list(writes))

    def dma(self, q, emit, dsem, npieces, reads=(), writes=(), name="", inc=16):
        o = Op(q, emit, True, name)
        o.dsem = dsem
        o.npieces = npieces
        o.inc = inc
        if dsem.last_op is not None:
            o.deps.append(dsem.last_op)
        dsem.total += inc * npieces
        o.dval = dsem.total
        dsem.last_op = o
        return self._add(o, list(reads), list(writes))

    def barrier(self):
        lasts = []
        for e in ENGS:
            if self.ops[e]:
                for o in reversed(self.ops[e]):
                    if not o.is_dma:
                        lasts.append(o)
                        break
        for d in self.dsems:
            if d.last_op is not None and not d.nobarrier:
                lasts.append(d.last_op)
        for e in ENGS:
            self.pending_barrier[e] = list(lasts)

    def emit_all(self, final_waits_eng="sp"):
        nc = self.nc
        for e in ("pe", "act", "dve", "pool"):
            self.esem[e] = nc.alloc_semaphore(f"e_{e}")
        for e in ENGS:
            cnt = 0
            for o in self.ops[e]:
                if o.is_dma:
                    continue
                if o.needs_inc:
                    cnt += 1
                    o.semval = cnt
        peidx = {id(o): i for i, o in enumerate(self.ops["pe"])}
        maxpe = {}
        last = {e: -1 for e in ENGS}
        for o in self.all_ops:
            m = last[o.eng]
            if o.eng == "pe":
                m = max(m, peidx[id(o)])
            for d in o.deps:
                m = max(m, maxpe[id(d)])
            maxpe[id(o)] = m
            last[o.eng] = m
        stats = {}
        with nc.Block() as block:
            def wait_list(e):
                waited = {}
                out = []
                for o in self.ops[e]:
                    ws = []
                    for d in o.deps:
                        if d.is_dma:
                            key, sem, val = id(d.dsem), d.dsem.sem, d.dval
                        else:
                            if d.eng == e and e == "pe":
                                continue
                            key, sem, val = d.eng, self.esem[d.eng], d.semval
                        if waited.get(key, 0) >= val:
                            continue
                        waited[key] = val
                        ws.append((sem, val, maxpe[id(d)]))
                    out.append(ws)
                return out, waited

            def run(e):
                def body(eng):
                    W, waited = wait_list(e)
                    ops = self.ops[e]
                    if e == "pe":
                        for j in range(1, len(ops)):
                            if not ops[j].hoist:
                                continue
                            keep = []
                            for w in W[j]:
                                if w[2] <= j - 2:
                                    W[j - 1].append(w)
                                else:
                                    keep.append(w)
                            W[j] = keep
                    nw = 0
                    for o, ws in zip(ops, W):
                        for (sem, val, _) in ws:
                            eng.wait_ge(sem, val)
                            nw += 1
                        r = o.emit(eng)
                        if o.is_dma:
                            assert len(r) == o.npieces, (o.name, len(r), o.npieces)
                            for ins in r:
                                ins.then_inc(o.dsem.sem, o.inc)
                        elif o.needs_inc:
                            r.then_inc(self.esem[e], 1)
                    if e == final_waits_eng:
                        for d in self.dsems:
                            if d.total and waited.get(id(d), 0) < d.total:
                                eng.wait_ge(d.sem, d.total)
                    stats[e] = (len(ops), nw)
                return body
            block.tensor(run("pe"))
            block.scalar(run("act"))
            block.vector(run("dve"))
            block.gpsimd(run("pool"))
            block.sync(run("sp"))
        return stats


D = 1024
KC = 8
T = 512
TOK = 4096
NT = TOK // T
FF = 2816
FC = 22
H = 16
CW = 31
NE = 8
EPS = 1e-6

V_MIX0, V_FFN0, V_BA, V_BG, V_BDW, V_LNG, V_LNB, V_BOUT, V_KVN, V_MIX1, V_FFN1, V_FIN = [8 * i for i in range(12)]
NV = 96


class Ctx:
    pass


def declare_io(nc, c, debug, ne_decl=NE):
    c.ne_decl = ne_decl
    def din(name, shape, dt=F32):
        return nc.dram_tensor(name, list(shape), dt, kind="ExternalInput")

    c.x = din("x", [TOK, D])
    c.xhalo = din("xhalo", [32, D])
    c.vecs = din("vecs", [128, NV])
    c.wdw = din("wdw", [128, KC, CW])
    c.bf = din("bf", [16, 1])
    c.hmask = din("hmask", [128, 1])
    c.flagrow = din("flagrow", [1, TOK], BF16)
    c.rw = din("rw", [128, KC, NE])
    c.rb4 = din("rb4", [128, 4, NE])
    c.sel = din("sel", [128, NE, 128])
    c.ident = din("ident", [128, 128])
    c.cmask = din("cmask", [128, 128], BF16)
    c.w_in = din("conv_w_in", [D, 2 * D])
    c.w_out = din("conv_w_out", [D, D])
    c.ffn_g = din("ffn_w_gate", [D, FF])
    c.ffn_u = din("ffn_w_up", [D, FF])
    c.ffn_d = din("ffn_w_down", [FF, D])
    c.w_kvf = din("w_kvf", [D, 2 * D + H])
    c.w_q = din("w_q", [D, D])
    c.w_o = din("w_o", [D, D])
    c.moe_g = din("moe_w_gate", [ne_decl, D, FF])
    c.moe_u = din("moe_w_up", [ne_decl, D, FF])
    c.moe_d = din("moe_w_down", [ne_decl, FF, D])

    def scr(name, shape, dt=BF16):
        return nc.dram_tensor(name, list(shape), dt)

    c.w_in_b = scr("w_in_b", [D, 2 * D])
    c.w_out_b = scr("w_out_b", [D, D])
    c.ffn_g_b = scr("ffn_g_b", [D, FF])
    c.ffn_u_b = scr("ffn_u_b", [D, FF])
    c.ffn_d_b = scr("ffn_d_b", [FF, D])
    c.w_kvf_b = scr("w_kvf_b", [D, 2 * D + H])
    c.w_q_b = scr("w_q_b", [D, D])
    c.w_o_b = scr("w_o_b", [D, D])
    c.moe_g_b = scr("moe_g_b", [ne_decl, D, FF])
    c.moe_u_b = scr("moe_u_b", [ne_decl, D, FF])
    c.moe_d_b = scr("moe_d_b", [ne_decl, FF, D])
    c.x1T = scr("x1T", [128, KC, TOK], F32)
    c.gk = [scr(f"gk{t}", [D, T]) for t in range(NT)]
    c.gko = [scr(f"gko{t}", [2 * D, T]) for t in range(NT)]
    c.gv = [scr(f"gv{t}", [T, D]) for t in range(NT)]
    c.gvo = [scr(f"gvo{t}", [2 * T, D]) for t in range(NT)]
    c.ga = scr("ga", [112, TOK])
    c.gao = scr("gao", [224, TOK])
    c.qT = scr("qT", [D, TOK])
    c.kaug_own = scr("kaug_own", [H, 7, TOK])
    c.qaug = scr("qaug", [H, 7, TOK])
    c.oT = scr("oT", [D, TOK])
    c.flog = scr("flog_d", [16, TOK], F32)
    c.rsum = scr("rsum", [H, TOK], F32)
    c.out = nc.dram_tensor("out", [TOK, D], F32, kind="ExternalOutput")
    c.debug = debug
    if debug in ("x2", "x3", "g"):
        c.dbg_x2T = nc.dram_tensor("dbg_x2T", [128, KC, TOK], F32, kind="ExternalOutput")


def setup_common(P, c):
    nc = P.nc
    c.ps = nc.alloc_psum_tensor("ps", [128, 8, 512], F32)
    c.psb = [Buf(f"psb{i}") for i in range(8)]
    c.ps_rr = 0
    c.ident_f = P.sb("ident_f", [128, 128], F32)
    c.ident_b = P.sb("ident_b", [128, 128], BF16)
    c.ones_b = P.sb("ones_b", [128, 128], BF16)
    c.vec = P.sb("vec", [128, NV], F32)
    c.vech = P.sb("vech", [128, 16], F32)
    c.wdw_s = P.sb("wdw_s", [128, KC, CW], F32)
    c.bf_s = P.sb("bf_s", [16, 1], F32)
    c.hmask_s = P.sb("hmask_s", [128, 1], F32)
    c.eps_t = P.sb("eps_t", [128, 1], F32)
    c.B_const = Buf("const")
    ds = P.new_dsem("const")
    c.ds_const = ds

    def ld(eng):
        r = []
        r.append(eng.dma_start(out=c.ident_f[:, :], in_=c.ident[:, :]))
        r.append(eng.dma_start(out=c.vec[:, :], in_=c.vecs[:, :]))
        r.append(eng.dma_start(out=c.wdw_s[:, :, :], in_=c.wdw[:, :, :]))
        r.append(eng.dma_start(out=c.bf_s[:, :], in_=c.bf[:, :]))
        r.append(eng.dma_start(out=c.hmask_s[:, :], in_=c.hmask[:, :]))
        return r
    P.dma("sp", ld, ds, 5, writes=[c.B_const], name="const_ld")
    c.B_const2 = Buf("const2")
    c.sb_phase_base = None

    def mk(eng):
        eng.tensor_copy(out=c.ident_b[:, :], in_=c.ident_f[:, :])
        eng.memset(c.ones_b[:, :], 1.0 / 1024.0)
        eng.memset(c.eps_t[:, :], EPS)
        return eng.tensor_scalar(out=c.vech[:, :], in0=c.vec[:, V_BA:V_BA + 16], scalar1=0.5, scalar2=None, op0=ALU.mult)
    P.op("dve", mk, reads=[c.B_const], writes=[c.B_const2], name="const_mk")


def psbank(c):
    i = c.ps_rr
    c.ps_rr = (c.ps_rr + 1) % 8
    return i


def convert_weights(P, c):
    c.B_w = {}

    def grp(name, pairs):
        ds = P.new_dsem("cv_" + name, nobarrier=True)
        b = Buf("wb_" + name)
        c.B_w[name] = b

        def em(eng, pairs=pairs):
            return [eng.dma_start(out=o, in_=i) for (o, i) in pairs]
        P.dma("pool", em, ds, len(pairs), writes=[b], name="cv_" + name)

    grp("in", [(c.w_in_b[:, :], c.w_in[:, :]), (c.w_out_b[:, :], c.w_out[:, :])])
    grp("ffn", [(c.ffn_g_b[:, :], c.ffn_g[:, :]), (c.ffn_u_b[:, :], c.ffn_u[:, :]),
                (c.ffn_d_b[0:1024, :], c.ffn_d[0:1024, :]), (c.ffn_d_b[1024:2048, :], c.ffn_d[1024:2048, :]),
                (c.ffn_d_b[2048:FF, :], c.ffn_d[2048:FF, :])])
    grp("att", [(c.w_kvf_b[:, :], c.w_kvf[:, :]), (c.w_q_b[:, :], c.w_q[:, :]), (c.w_o_b[:, :], c.w_o[:, :])])


def convert_expert(P, c, e):
    ds = P.new_dsem(f"cv_e{e}", nobarrier=True)
    b = Buf(f"wb_e{e}")
    c.B_w[f"e{e}"] = b
    pairs = [(c.moe_g_b[e, :, :], c.moe_g[e, :, :]), (c.moe_u_b[e, :, :], c.moe_u[e, :, :]),
             (c.moe_d_b[e, 0:1024, :], c.moe_d[e, 0:1024, :]), (c.moe_d_b[e, 1024:2048, :], c.moe_d[e, 1024:2048, :]),
             (c.moe_d_b[e, 2048:FF, :], c.moe_d[e, 2048:FF, :])]

    def em(eng, pairs=pairs):
        return [eng.dma_start(out=o, in_=i) for (o, i) in pairs]
    P.dma("pool", em, ds, len(pairs), writes=[b], name=f"cv_e{e}")


class Ring:
    def __init__(self, P, n, name="ring"):
        self.P = P
        self.n = n
        self.off = []
        self.bufs = [Buf(f"{name}{i}") for i in range(n)]
        self.ds = [P.new_dsem(f"{name}{i}") for i in range(n)]
        self.v8 = []
        self.vgu = []
        for i in range(n):
            t = P.sb(f"{name}{i}", [128, 8, 1024], BF16)
            off = P.sb_off - 16384
            self.v8.append(t)
            self.vgu.append(P.sb(f"{name}gu{i}", [128, 2, 8, 512], BF16, off=off))
        self.rr = 0

    def load(self, pieces_fn, wbuf, name="wl", q="sp"):
        i = self.rr
        self.rr = (self.rr + 1) % self.n
        pieces = pieces_fn(i)

        def em(eng, pieces=pieces):
            return [eng.dma_start(out=o, in_=s) for (o, s) in pieces]
        self.P.dma(q, em, self.ds[i], len(pieces), reads=([wbuf] if wbuf is not None else []), writes=[self.bufs[i]], name=name)
        return i


def w8(src, c0, ncols):
    return src[:, c0:c0 + ncols].rearrange("(kc p) c -> p kc c", p=128)


def alloc_phase_a(P, c):
    c.ring = Ring(P, 4)
    c.x_tok = P.sb("x_tok", [128, 4, D], F32)
    off = P.sb_off - 16384
    c.v = P.sb("v", [128, KC, T], F32, off=off)
    c.B_r1 = Buf("r1")
    c.B_v = [Buf(f"v{k}") for k in range(KC)]
    c.xT = P.sb("xT", [128, KC, T], F32)
    c.B_xT = [Buf(f"xT{k}") for k in range(KC)]
    c.sq = P.sb("sq", [128, KC, T], BF16)
    c.B_sq = Buf("sq")
    c.hT = P.sb("hT", [128, KC, T], BF16)
    c.B_hT = Buf("hT")
    c.ap_ = P.sb("aprime", [128, KC, T], BF16)
    c.B_ap = [Buf(f"ap{k}") for k in range(KC)]
    c.uT = P.sb("uT", [128, KC, 32 + T], BF16)
    c.B_uT = [Buf(f"uT{k}") for k in range(KC)]
    c.sT = P.sb("sT", [128, KC, T], BF16)
    c.B_sT = Buf("sT")
    c.aT = P.sb("aT", [128, FC, T], BF16)
    c.B_aT = [Buf(f"aT{k}") for k in range(FC)]
    offa = P.sb_off - FC * T * 2
    c.kst = P.sb("kst", [128, KC, T], BF16, off=offa)
    c.vst = P.sb("vst", [128, 4, D], BF16, off=offa + 8192)
    c.diag = P.sb("diag", [128, 2, CW, 128], BF16)
    c.B_diag = [Buf("diag0"), Buf("diag1")]
    c.th = [P.sb(f"th{i}", [128, T], F32) for i in range(3)]
    c.B_th = [Buf(f"th{i}") for i in range(3)]
    c.th_rr = 0
    c.st = [P.sb(f"st{i}", [128, T], F32) for i in range(6)]
    c.B_st = [Buf(f"st{i}") for i in range(6)]
    c.B_flog = Buf("flog")
    c.wf = P.sb("wf", [128, KC, 16], BF16)
    c.B_wf = Buf("wf")
    c.ds_x = P.new_dsem("x")
    c.ds_st = [P.new_dsem(f"store{i}") for i in range(4)]
    c.B_x1T = [Buf(f"x1T_{t}") for t in range(NT)]
    c.B_gk = [Buf(f"gk{t}") for t in range(NT)]
    c.B_gv = [Buf(f"gv{t}") for t in range(NT)]
    c.B_gko = [Buf(f"gko{t}") for t in range(NT)]
    c.B_gvo = [Buf(f"gvo{t}") for t in range(NT)]
    c.B_ga = Buf("ga")
    c.B_gao = Buf("gao")
    c.B_qT = Buf("qTd")


def mm_group(P, c, bank, pairs, reads, ncols, nrows=128, name="mm", col0=0):
    ps = c.ps
    n = len(pairs)

    def em(pe, pairs=pairs):
        r = None
        for i, (l, rr) in enumerate(pairs):
            r = pe.matmul(ps[0:nrows, bank, col0:col0 + ncols], l, rr, start=(i == 0), stop=(i == n - 1))
        return r
    o = P.op("pe", em, reads=reads, writes=[c.psb[bank]], name=name)
    o.hoist = True
    return o


def rms_stats(P, c, ncols, src_bufs):
    xT, sq = c.xT, c.sq
    P.op("act", lambda a: a.activation(out=sq[:, :, 0:ncols], in_=xT[:, :, 0:ncols], func=AF.Square),
         reads=src_bufs, writes=[c.B_sq], name="sq")
    b = psbank(c)
    mm_group(P, c, b, [(c.ones_b[:, :], sq[:, k, 0:ncols]) for k in range(KC)], [c.B_sq, c.B_const2], ncols, name="ms")
    i0, i1 = 0, 1
    t0, t1 = c.st[i0], c.st[i1]
    P.op("act", lambda a: a.activation(out=t0[:, 0:ncols], in_=c.ps[:, b, 0:ncols], func=AF.Ln, bias=c.eps_t[:, 0:1], scale=1.0),
         reads=[c.psb[b], c.B_const2], writes=[c.B_st[i0]], name="ms_ln")
    P.op("act", lambda a: a.activation(out=t1[:, 0:ncols], in_=t0[:, 0:ncols], func=AF.Exp, scale=-0.5),
         reads=[c.B_st[i0]], writes=[c.B_st[i1]], name="rstd")
    return i1


def apply_norm(P, c, ncols, rstd_i, gcol, dst, dstbuf, name="hn"):
    xT = c.xT
    rs = c.st[rstd_i]
    for k in range(KC):
        P.op("dve", lambda v, k=k: v.scalar_tensor_tensor(out=dst[:, k, 0:ncols], in0=xT[:, k, 0:ncols], scalar=c.vec[:, gcol + k:gcol + k + 1],
                                                         in1=rs[:, 0:ncols], op0=ALU.mult, op1=ALU.mult),
             reads=[c.B_xT[k], c.B_st[rstd_i], c.B_const], writes=[dstbuf], name=name)


def load_x_and_transpose(P, c, src_ap, ntok):
    nch = (ntok + 127) // 128
    if ntok >= 128:
        def em(eng):
            return [eng.dma_start(out=c.x_tok[:, 0:nch, :], in_=src_ap.rearrange("(c p) d -> p c d", p=128))]
    else:
        def em(eng):
            return [eng.dma_start(out=c.x_tok[0:ntok, 0, :], in_=src_ap)]
    P.dma("sp", em, c.ds_x, 1, writes=[c.B_r1] + c.B_v, name="x_ld")
    for k in range(KC):
        b = psbank(c)

        def emt(pe, k=k, b=b):
            r = None
            for tc in range(nch):
                n = min(128, ntok - tc * 128)
                r = pe.transpose(out=c.ps[:, b, tc * 128:tc * 128 + n], in_=c.x_tok[0:n, tc, k * 128:(k + 1) * 128], identity=c.ident_f[0:n, 0:n])
            return r
        P.op("pe", emt, reads=[c.B_r1, c.B_const], writes=[c.psb[b]], name="xpose")
        if k % 2 == 0:
            P.op("act", lambda a, k=k, b=b: a.copy(out=c.xT[:, k, 0:ntok], in_=c.ps[:, b, 0:ntok]), reads=[c.psb[b]], writes=[c.B_xT[k]], name="xT_ev")
        else:
            P.op("dve", lambda v, k=k, b=b: v.tensor_copy(out=c.xT[:, k, 0:ntok], in_=c.ps[:, b, 0:ntok]), reads=[c.psb[b]], writes=[c.B_xT[k]], name="xT_ev")


def glu_stage(P, c, ncols, ucol0, mask_halo=False, direct=False):
    ring = c.ring
    if direct:
        sa = ring.load(lambda i: [(ring.v8[i][:, :, :], w8(c.w_in, 0, 1024))], None, name="w_in_a_direct", q="pool")
        sg = ring.load(lambda i: [(ring.v8[i][:, :, :], w8(c.w_in, 1024, 1024))], None, name="w_in_g_direct", q="pool")
    else:
        sa = ring.load(lambda i: [(ring.v8[i][:, :, :], w8(c.w_in_b, 0, 1024))], c.B_w["in"], name="w_in_a")
        sg = ring.load(lambda i: [(ring.v8[i][:, :, :], w8(c.w_in_b, 1024, 1024))], c.B_w["in"], name="w_in_g")
    for oc in range(KC):
        b = psbank(c)
        mm_group(P, c, b, [(ring.v8[sa][:, k, oc * 128:(oc + 1) * 128], c.hT[:, k, 0:ncols]) for k in range(KC)],
                 [ring.bufs[sa], c.B_hT], ncols, name="w_in_a")
        P.op("act", lambda a, oc=oc, b=b: a.activation(out=c.ap_[:, oc, 0:ncols], in_=c.ps[:, b, 0:ncols], func=AF.Identity,
                                                       bias=c.vech[:, oc:oc + 1], scale=0.5),
             reads=[c.psb[b], c.B_const2], writes=[c.B_ap[oc]], name="aprime")
    for oc in range(KC):
        b = psbank(c)
        mm_group(P, c, b, [(ring.v8[sg][:, k, oc * 128:(oc + 1) * 128], c.hT[:, k, 0:ncols]) for k in range(KC)],
                 [ring.bufs[sg], c.B_hT], ncols, name="w_in_g")
        ti = c.th_rr
        c.th_rr = (c.th_rr + 1) % 3
        th = c.th[ti]
        P.op("act", lambda a, oc=oc, b=b, th=th: a.activation(out=th[:, 0:ncols], in_=c.ps[:, b, 0:ncols], func=AF.Tanh,
                                                              bias=c.vech[:, 8 + oc:9 + oc], scale=0.5),
             reads=[c.psb[b], c.B_const2], writes=[c.B_th[ti]], name="tanh")
        if not mask_halo:
            P.op("dve", lambda v, oc=oc, th=th: v.scalar_tensor_tensor(out=c.uT[:, oc, ucol0:ucol0 + ncols], in0=th[:, 0:ncols], scalar=1.0,
                                                                      in1=c.ap_[:, oc, 0:ncols], op0=ALU.add, op1=ALU.mult),
                 reads=[c.B_th[ti], c.B_ap[oc]], writes=[c.B_uT[oc]], name="glu")
        else:
            P.op("dve", lambda v, oc=oc, th=th: v.scalar_tensor_tensor(out=th[:, 0:ncols], in0=th[:, 0:ncols], scalar=1.0, in1=c.ap_[:, oc, 0:ncols], op0=ALU.add, op1=ALU.mult),
                 reads=[c.B_th[ti], c.B_ap[oc]], writes=[c.B_th[ti]], name="glu_h1")
            P.op("dve", lambda v, oc=oc, th=th: v.tensor_scalar(out=c.uT[:, oc, ucol0:ucol0 + ncols], in0=th[:, 0:ncols], scalar1=c.hmask_s[:, 0:1], scalar2=None, op0=ALU.mult),
                 reads=[c.B_th[ti], c.B_const], writes=[c.B_uT[oc]], name="glu_h2")


def conv_ln_stage(P, c):
    for k in range(KC):
        di = k % 2
        dg = c.diag

        def emd(g, k=k, di=di):
            r = None
            for j in range(CW):
                r = g.tensor_scalar(out=dg[:, di, j, :], in0=c.ident_b[:, :], scalar1=c.wdw_s[:, k, j:j + 1], scalar2=1.0, op0=ALU.mult, op1=ALU.mult)
            return r
        P.op("pool" if k % 2 == 0 else "dve", emd, reads=[c.B_const, c.B_const2], writes=[c.B_diag[di]], name="diag")
        b = psbank(c)
        mm_group(P, c, b, [(dg[:, di, j, :], c.uT[:, k, 2 + j:2 + j + T]) for j in range(CW)], [c.B_diag[di], c.B_uT[k]], T, name="conv")
        P.op("act", lambda a, k=k, b=b: a.activation(out=c.v[:, k, :], in_=c.ps[:, b, :], func=AF.Identity, bias=c.vec[:, V_BDW + k:V_BDW + k + 1], scale=1.0),
             reads=[c.psb[b], c.B_const], writes=[c.B_v[k]], name="v_ev")
        P.op("act", lambda a, k=k, b=b: a.activation(out=c.sq[:, k, :], in_=c.ps[:, b, :], func=AF.Square, bias=c.vec[:, V_BDW + k:V_BDW + k + 1], scale=1.0),
             reads=[c.psb[b], c.B_const], writes=[c.B_sq], name="v_sq")
        P.op("pool", lambda g, k=k: g.tensor_copy(out=c.ap_[:, k, :], in_=c.v[:, k, :]), reads=[c.B_v[k]], writes=[c.B_ap[k]], name="vb")
    bm = psbank(c)
    mm_group(P, c, bm, [(c.ones_b[:, :], c.ap_[:, k, :]) for k in range(KC)], c.B_ap + [c.B_const2], T, name="ln_mean")
    be = psbank(c)
    mm_group(P, c, be, [(c.ones_b[:, :], c.sq[:, k, :]) for k in range(KC)], [c.B_sq, c.B_const2], T, name="ln_ex2")
    mean, m2, var, rstd, Bt = c.st[2], c.st[3], c.st[0], c.st[1], c.st[4]
    P.op("act", lambda a: a.copy(out=mean[:, :], in_=c.ps[:, bm, :]), reads=[c.psb[bm]], writes=[c.B_st[2]], name="mean")
    P.op("dve", lambda v: v.tensor_tensor(out=m2[:, :], in0=mean[:, :], in1=mean[:, :], op=ALU.mult), reads=[c.B_st[2]], writes=[c.B_st[3]], name="m2")
    P.op("dve", lambda v: v.scalar_tensor_tensor(out=var[:, :], in0=c.ps[:, be, :], scalar=EPS, in1=m2[:, :], op0=ALU.add, op1=ALU.subtract),
         reads=[c.psb[be], c.B_st[3]], writes=[c.B_st[0]], name="var")
    P.op("act", lambda a: a.activation(out=var[:, :], in_=var[:, :], func=AF.Ln), reads=[c.B_st[0]], writes=[c.B_st[0]], name="ln_ln")
    P.op("act", lambda a: a.activation(out=rstd[:, :], in_=var[:, :], func=AF.Exp, scale=-0.5), reads=[c.B_st[0]], writes=[c.B_st[1]], name="ln_rstd")
    P.op("dve", lambda v: v.scalar_tensor_tensor(out=Bt[:, :], in0=mean[:, :], scalar=-1.0, in1=rstd[:, :], op0=ALU.mult, op1=ALU.mult),
         reads=[c.B_st[2], c.B_st[1]], writes=[c.B_st[4]], name="ln_B")
    for k in range(KC):
        ti = c.th_rr
        c.th_rr = (c.th_rr + 1) % 3
        th = c.th[ti]
        P.op("pool", lambda g, k=k, th=th: g.tensor_tensor(out=th[:, :], in0=c.v[:, k, :], in1=rstd[:, :], op=ALU.mult),
             reads=[c.B_v[k], c.B_st[1]], writes=[c.B_th[ti]], name="ln_t1")
        P.op("dve", lambda v, th=th: v.tensor_tensor(out=th[:, :], in0=th[:, :], in1=Bt[:, :], op=ALU.add),
             reads=[c.B_th[ti], c.B_st[4]], writes=[c.B_th[ti]], name="ln_t2")
        P.op("act", lambda a, k=k, th=th: a.activation(out=c.sT[:, k, :], in_=th[:, :], func=AF.Silu, bias=c.vec[:, V_LNB + k:V_LNB + k + 1],
                                                       scale=c.vec[:, V_LNG + k:V_LNG + k + 1]),
             reads=[c.B_th[ti], c.B_const], writes=[c.B_sT], name="ln_silu")


def halo_shift(P, c):
    for k in range(KC):
        P.op("pool", lambda g, k=k: g.tensor_copy(out=c.uT[:, k, 2:32], in_=c.uT[:, k, 2 + T:32 + T]), reads=[c.B_uT[k]], writes=[c.B_uT[k]], name="halo")


def wout_stage(P, c):
    ring = c.ring
    s = ring.load(lambda i: [(ring.v8[i][:, :, :], w8(c.w_out_b, 0, 1024))], c.B_w["in"], name="w_out")
    for dc in range(KC):
        b = psbank(c)
        mm_group(P, c, b, [(ring.v8[s][:, k, dc * 128:(dc + 1) * 128], c.sT[:, k, :]) for k in range(KC)], [ring.bufs[s], c.B_sT], T, name="w_out")
        P.op("dve", lambda v, dc=dc, b=b: v.scalar_tensor_tensor(out=c.xT[:, dc, :], in0=c.ps[:, b, :], scalar=c.vec[:, V_BOUT + dc:V_BOUT + dc + 1],
                                                                in1=c.xT[:, dc, :], op0=ALU.add, op1=ALU.add),
             reads=[c.psb[b], c.B_xT[dc], c.B_const], writes=[c.B_xT[dc]], name="res1")


FGROUPS = [(0, 4), (4, 4), (8, 4), (12, 4), (16, 4), (20, 2)]
DHALF = [[(0, 8), (8, 4)], [(12, 8), (20, 2)]]


def ffn_stage(P, c, g_b, u_b, d_b, wbuf, hsrc, hbuf, gate=None):
    ring = c.ring
    for (f0, nf) in FGROUPS:
        s = ring.load(lambda i, f0=f0, nf=nf: [(ring.vgu[i][:, 0, :, 0:nf * 128], w8(g_b, f0 * 128, nf * 128)),
                                              (ring.vgu[i][:, 1, :, 0:nf * 128], w8(u_b, f0 * 128, nf * 128))], wbuf, name="w_gu")
        for j in range(nf):
            fc = f0 + j
            bg = psbank(c)
            mm_group(P, c, bg, [(ring.vgu[s][:, 0, k, j * 128:(j + 1) * 128], hsrc[:, k, :]) for k in range(KC)], [ring.bufs[s], hbuf], T, name="ffn_g")
            bu = psbank(c)
            mm_group(P, c, bu, [(ring.vgu[s][:, 1, k, j * 128:(j + 1) * 128], hsrc[:, k, :]) for k in range(KC)], [ring.bufs[s], hbuf], T, name="ffn_u")
            ti = c.th_rr
            c.th_rr = (c.th_rr + 1) % 3
            th = c.th[ti]
            P.op("act", lambda a, bg=bg, th=th: a.activation(out=th[:, :], in_=c.ps[:, bg, :], func=AF.Silu), reads=[c.psb[bg]], writes=[c.B_th[ti]], name="silu")
            P.op("dve", lambda v, bu=bu, th=th, fc=fc: v.tensor_tensor(out=c.aT[:, fc, :], in0=th[:, :], in1=c.ps[:, bu, :], op=ALU.mult),
                 reads=[c.B_th[ti], c.psb[bu]], writes=[c.B_aT[fc]], name="a_mul")
    for half in range(2):
        slots = []
        for (f0, nf) in DHALF[half]:
            s = ring.load(lambda i, f0=f0, nf=nf: [(ring.v8[i][:, 0:nf, :], d_b[f0 * 128:(f0 + nf) * 128, :].rearrange("(fc p) c -> p fc c", p=128))], wbuf, name="w_d")
            slots.append((s, f0, nf))
        for dc in range(KC):
            b = psbank(c)
            pairs = []
            reads = []
            for (s, f0, nf) in slots:
                reads.append(ring.bufs[s])
                for j in range(nf):
                    pairs.append((ring.v8[s][:, j, dc * 128:(dc + 1) * 128], c.aT[:, f0 + j, :]))
                    reads.append(c.B_aT[f0 + j])
            mm_group(P, c, b, pairs, reads, T, name="ffn_d")
            if gate is None:
                P.op("dve", lambda v, dc=dc, b=b: v.tensor_tensor(out=c.xT[:, dc, :], in0=c.ps[:, b, :], in1=c.xT[:, dc, :], op=ALU.add),
                     reads=[c.psb[b], c.B_xT[dc]], writes=[c.B_xT[dc]], name="res2")
            else:
                gt, gbuf = gate
                ti = c.th_rr
                c.th_rr = (c.th_rr + 1) % 3
                th = c.th[ti]
                P.op("dve", lambda v, b=b, th=th: v.tensor_tensor(out=th[:, :], in0=c.ps[:, b, :], in1=gt, op=ALU.mult),
                     reads=[c.psb[b], gbuf], writes=[c.B_th[ti]], name="gmul")
                P.op("pool", lambda g, dc=dc, th=th: g.tensor_tensor(out=c.xT[:, dc, :], in0=th[:, :], in1=c.xT[:, dc, :], op=ALU.add),
                     reads=[c.B_th[ti], c.B_xT[dc]], writes=[c.B_xT[dc]], name="res_moe")


def allgather(P, c, src, dst, bsrc, bdst):
    ds = P.new_dsem("cc")

    def em(g):
        return [g.collective_compute("AllGather", ALU.bypass, replica_groups=[[0, 1], [2, 3], [4, 5], [6, 7]],
                                     ins=[src.ap()], outs=[dst.ap()])]
    P.dma("pool", em, ds, 1, reads=[bsrc], writes=[bdst], name="allgather", inc=1)


def proj_stage(P, c, t):
    ring = c.ring
    t0 = t * T
    ri = rms_stats(P, c, T, c.B_xT)
    apply_norm(P, c, T, ri, V_KVN, c.hT, c.B_hT, name="hn_kv")
    apply_norm(P, c, T, ri, V_MIX1, c.sT, c.B_sT, name="hn_q")
    s = ring.load(lambda i: [(ring.v8[i][:, :, :], w8(c.w_kvf_b, 0, 1024))], c.B_w["att"], name="w_k")
    B_kst = Buf("kst")
    for cc in range(KC):
        b = psbank(c)
        mm_group(P, c, b, [(ring.v8[s][:, k, cc * 128:(cc + 1) * 128], c.hT[:, k, :]) for k in range(KC)], [ring.bufs[s], c.B_hT], T, name="k_mm")
        P.op("act", lambda a, cc=cc, b=b: a.copy(out=c.kst[:, cc, :], in_=c.ps[:, b, :]), reads=[c.psb[b]], writes=[c.B_aT[cc]], name="k_ev")
    P.dma("act", lambda e: [e.dma_start(out=c.gk[t][:, :].rearrange("(cc p) t -> p cc t", p=128), in_=c.kst[:, :, :])],
          c.ds_st[0], 1, reads=c.B_aT[0:8], writes=[c.B_gk[t]], name="k_st")
    allgather(P, c, c.gk[t], c.gko[t], c.B_gk[t], c.B_gko[t])
    s = ring.load(lambda i: [(ring.v8[i][:, :, :], w8(c.w_kvf_b, 1024, 1024))], c.B_w["att"], name="w_v")
    for tc in range(4):
        for hf in range(2):
            b = psbank(c)
            mm_group(P, c, b, [(c.hT[:, k, tc * 128:(tc + 1) * 128], ring.v8[s][:, k, hf * 512:(hf + 1) * 512]) for k in range(KC)],
                     [ring.bufs[s], c.B_hT], 512, name="v_mm")
            P.op("dve", lambda v, tc=tc, hf=hf, b=b: v.tensor_copy(out=c.vst[:, tc, hf * 512:(hf + 1) * 512], in_=c.ps[:, b, :]),
                 reads=[c.psb[b]], writes=[c.B_aT[8 + tc * 2 + hf]], name="v_ev")
    P.dma("act", lambda e: [e.dma_start(out=c.gv[t][:, :].rearrange("(tc p) d -> p tc d", p=128), in_=c.vst[:, :, :])],
          c.ds_st[1], 1, reads=c.B_aT[8:16], writes=[c.B_gv[t]], name="v_st")
    allgather(P, c, c.gv[t], c.gvo[t], c.B_gv[t], c.B_gvo[t])
    b = psbank(c)
    mm_group(P, c, b, [(c.wf[:, k, :], c.hT[:, k, :]) for k in range(KC)], [c.B_wf, c.B_hT], T, nrows=16, name="f_mm")
    P.op("act", lambda a, b=b: a.activation(out=c.st[5][0:16, :], in_=c.ps[0:16, b, :], func=AF.Identity, bias=c.bf_s[:, 0:1], scale=1.0),
         reads=[c.psb[b], c.B_const], writes=[c.B_st[5]], name="f_ev")
    P.dma("act", lambda e: [e.dma_start(out=c.flog[:, t0:t0 + T], in_=c.st[5][0:16, :])], c.ds_st[3], 1, reads=[c.B_st[5]], writes=[c.B_flog], name="f_st")
    s = ring.load(lambda i: [(ring.v8[i][:, :, :], w8(c.w_q_b, 0, 1024))], c.B_w["att"], name="w_q")
    for cc in range(KC):
        b = psbank(c)
        mm_group(P, c, b, [(ring.v8[s][:, k, cc * 128:(cc + 1) * 128], c.sT[:, k, :]) for k in range(KC)], [ring.bufs[s], c.B_sT], T, name="q_mm")
        P.op("act", lambda a, cc=cc, b=b: a.activation(out=c.kst[:, cc, :], in_=c.ps[:, b, :], func=AF.Copy, scale=0.125),
             reads=[c.psb[b]], writes=[c.B_aT[cc]], name="q_ev")
    P.dma("act", lambda e: [e.dma_start(out=c.qT[:, t0:t0 + T].rearrange("(cc p) t -> p cc t", p=128), in_=c.kst[:, :, :])],
          c.ds_st[2], 1, reads=c.B_aT[0:8], writes=[c.B_qT], name="q_st")


def phase_a(P, c, ntiles=NT, debug=None):
    load_x_and_transpose(P, c, c.xhalo[:, :], 32)
    ri = rms_stats(P, c, 32, c.B_xT)
    apply_norm(P, c, 32, ri, V_MIX0, c.hT, c.B_hT)
    glu_stage(P, c, 32, 0, mask_halo=True, direct=True)
    for t in range(ntiles):
        t0 = t * T
        load_x_and_transpose(P, c, c.x[t0:t0 + T, :], T)
        ri = rms_stats(P, c, T, c.B_xT)
        apply_norm(P, c, T, ri, V_MIX0, c.hT, c.B_hT)
        glu_stage(P, c, T, 32, direct=(t == 0))
        if t == 0:
            convert_weights(P, c)
        conv_ln_stage(P, c)
        halo_shift(P, c)
        wout_stage(P, c)
        if debug == "xa":
            P.dma("act", lambda e, t0=t0: [e.dma_start(out=c.x1T[:, :, t0:t0 + T], in_=c.xT[:, :, :])], c.ds_st[3], 1, reads=c.B_xT, writes=[c.B_x1T[t]], name="xa_st")
            continue
        ri = rms_stats(P, c, T, c.B_xT)
        apply_norm(P, c, T, ri, V_FFN0, c.hT, c.B_hT)
        ffn_stage(P, c, c.ffn_g_b, c.ffn_u_b, c.ffn_d_b, c.B_w["ffn"], c.hT, c.B_hT)
        P.dma("act", lambda e, t0=t0: [e.dma_start(out=c.x1T[:, :, t0:t0 + T], in_=c.xT[:, :, :])], c.ds_st[3], 1, reads=c.B_xT, writes=[c.B_x1T[t]], name="x1_st")
        if t == 0:
            P.dma("sp", lambda e: [e.dma_start(out=c.wf[:, :, :], in_=w8(c.w_kvf_b, 2048, 16))], P.new_dsem("wf"), 1, reads=[c.B_w["att"]], writes=[c.B_wf], name="wf_ld")
        proj_stage(P, c, t)
        if t < c.ne_decl:
            convert_expert(P, c, t)


NKB_HALF = 32
GRP = 3


def bcast_last(ap2d, n):
    a = [list(x) for x in ap2d.ap]
    return bass.AP(tensor=ap2d.tensor, offset=ap2d.offset, ap=a + [[0, n]])


def phase_b(P, c):
    base = c.sb_phase_base
    P.sb_off = base
    fl = P.sb("fl", [16, TOK], F32)
    ones = P.sb("ones16", [16, TOK], F32)
    C = P.sb("C", [16, TOK], F32)
    e1 = P.sb("e1", [16, TOK], F32)
    hs = [P.sb(f"h{i}", [16, TOK], BF16) for i in range(3)]
    rs_ = [P.sb(f"r{i}", [16, TOK], BF16) for i in range(3)]
    ns = [P.sb(f"n{i}", [16, TOK], BF16) for i in range(3)]
    oneb = P.sb("oneb", [16, TOK], BF16)
    zerob = P.sb("zerob", [16, TOK], BF16)
    B = {k: Buf("pb_" + k) for k in ("fl", "ones", "C", "e1", "h", "r", "n", "cb")}
    ds = P.new_dsem("pb")
    P.dma("sp", lambda e: [e.dma_start(out=fl[:, :], in_=c.flog[:, :])], ds, 1, reads=[c.B_flog], writes=[B["fl"]], name="fl_ld")

    def mk(v):
        v.memset(ones[:, :], 1.0)
        v.memset(oneb[:, :], 1.0)
        return v.memset(zerob[:, :], 0.0)
    P.op("dve", mk, writes=[B["ones"], B["cb"]], name="pb_const")
    P.op("act", lambda a: a.activation(out=fl[:, :], in_=fl[:, :], func=AF.Exp, scale=-1.0), reads=[B["fl"]], writes=[B["fl"]], name="exp_f")
    P.op("act", lambda a: a.activation(out=fl[:, :], in_=fl[:, :], func=AF.Ln, bias=1.0, scale=1.0), reads=[B["fl"]], writes=[B["fl"]], name="ln_f")
    P.op("dve", lambda v: v.tensor_tensor_scan(out=C[:, :], data0=ones[:, :], data1=fl[:, :], initial=0.0, op0=ALU.mult, op1=ALU.add),
         reads=[B["fl"], B["ones"]], writes=[B["C"]], name="scan")

    def split(src, outs, bsrc, bout, nm):
        bs = [Buf(nm + str(i)) for i in range(3)]
        P.op("dve", lambda v: v.tensor_copy(out=outs[0][:, :], in_=src[:, :]), reads=[bsrc], writes=[bs[0]], name=nm)
        P.op("dve", lambda v: v.tensor_tensor(out=e1[:, :], in0=src[:, :], in1=outs[0][:, :], op=ALU.subtract), reads=[bsrc, bs[0]], writes=[B["e1"]], name=nm)
        P.op("dve", lambda v: v.tensor_copy(out=outs[1][:, :], in_=e1[:, :]), reads=[B["e1"]], writes=[bs[1]], name=nm)
        P.op("dve", lambda v: v.tensor_tensor(out=e1[:, :], in0=e1[:, :], in1=outs[1][:, :], op=ALU.subtract), reads=[B["e1"], bs[1]], writes=[B["e1"]], name=nm)
        P.op("dve", lambda v: v.tensor_copy(out=outs[2][:, :], in_=e1[:, :]), reads=[B["e1"]] + bs[0:2], writes=[bout], name=nm)
    split(C, hs, B["C"], B["h"], "split_h")

    def neg(v):
        r = None
        for i in range(3):
            r = v.tensor_scalar(out=ns[i][:, :], in0=hs[i][:, :], scalar1=-1.0, scalar2=None, op0=ALU.mult)
        return r
    P.op("dve", neg, reads=[B["h"]], writes=[B["n"]], name="neg_h")
    P.op("dve", lambda v: v.tensor_scalar(out=fl[:, :], in0=C[:, :], scalar1=C[:, TOK - 1:TOK], scalar2=None, op0=ALU.subtract),
         reads=[B["C"], B["fl"]], writes=[B["fl"]], name="R")
    split(fl, rs_, B["fl"], B["r"], "split_r")
    c.B_kaug_own = Buf("kaug_own")
    c.B_qaug = Buf("qaug_d")
    gin_aug = c.ga[:, :].rearrange("(h r) t -> h r t", r=7)

    def st(e):
        r = []
        for i in range(3):
            r.append(e.dma_start(out=c.kaug_own[:, i, :], in_=oneb[:, :]))
            r.append(e.dma_start(out=c.kaug_own[:, 3 + i, :], in_=hs[i][:, :]))
            r.append(e.dma_start(out=gin_aug[:, i, :], in_=oneb[:, :]))
            r.append(e.dma_start(out=gin_aug[:, 3 + i, :], in_=rs_[i][:, :]))
            r.append(e.dma_start(out=c.qaug[:, i, :], in_=ns[i][:, :]))
            r.append(e.dma_start(out=c.qaug[:, 3 + i, :], in_=oneb[:, :]))
        r.append(e.dma_start(out=c.kaug_own[:, 6, :], in_=zerob[:, :]))
        r.append(e.dma_start(out=gin_aug[:, 6, :], in_=zerob[:, :]))
        r.append(e.dma_start(out=c.qaug[:, 6, :], in_=oneb[:, :]))
        return r
    P.dma("sp", st, ds, 21, reads=[B["h"], B["r"], B["n"], B["cb"]], writes=[c.B_kaug_own, c.B_ga, c.B_qaug], name="aug_st")


def phase_c(P, c):
    allgather(P, c, c.ga, c.gao, c.B_ga, c.B_gao)


def phase_d(P, c, nheads=H):
    nc = P.nc
    P.sb_off = c.sb_phase_base
    Kt = [P.sb(f"Kt{i}", [128, 2 * TOK], BF16) for i in range(2)]
    Vt = [P.sb(f"Vt{i}", [128, 2 * NKB_HALF, 65], BF16) for i in range(2)]
    Qt = [P.sb(f"Qt{i}", [128, TOK], BF16) for i in range(2)]
    PT = [P.sb(f"PT{i}", [128, GRP, 512], BF16) for i in range(3)]
    osb = [P.sb(f"osb{i}", [128, 512], BF16) for i in range(2)]
    rsb = [P.sb(f"rsb{i}", [128, 512], F32) for i in range(2)]
    cm = P.sb("cm", [128, 128], BF16)
    B_K = [Buf(f"Kt{i}") for i in range(2)]
    B_V = [Buf(f"Vt{i}") for i in range(2)]
    B_Q = [Buf(f"Qt{i}") for i in range(2)]
    B_PT = [Buf(f"PT{i}") for i in range(3)]
    B_osb = [Buf(f"osb{i}") for i in range(2)]
    B_cm = Buf("cm")
    ds_k = [P.new_dsem(f"k{i}") for i in range(2)]
    ds_o = [P.new_dsem(f"o{i}") for i in range(2)]
    ds_m = P.new_dsem("cm")
    c.B_oT = Buf("oT_d")
    c.B_rsum = Buf("rsum_d")
    P.dma("sp", lambda e: [e.dma_start(out=cm[:, :], in_=c.cmask[:, :])], ds_m, 1, writes=[B_cm], name="cm_ld")

    def ones_col(v):
        v.memset(Vt[0][:, :, 64:65], 1.0)
        return v.memset(Vt[1][:, :, 64:65], 1.0)
    P.op("dve", ones_col, writes=B_V, name="ones_col")

    pt_rr = 0
    o_rr = 0
    def load_head(h):
        bi = h % 2
        K, V, Q = Kt[bi], Vt[bi], Qt[bi]

        def ldk(e, h=h, K=K, V=V, Q=Q):
            r = []
            for t in range(NT):
                r.append(e.dma_start(out=K[0:64, t * T:(t + 1) * T], in_=c.gko[t][h * 64:(h + 1) * 64, :]))
                r.append(e.dma_start(out=K[0:64, TOK + t * T:TOK + (t + 1) * T], in_=c.gk[t][h * 64:(h + 1) * 64, :]))
                r.append(e.dma_start(out=V[:, 4 * t:4 * t + 4, 0:64], in_=c.gvo[t][0:T, h * 64:(h + 1) * 64].rearrange("(kb p) d -> p kb d", p=128)))
                r.append(e.dma_start(out=V[:, NKB_HALF + 4 * t:NKB_HALF + 4 * t + 4, 0:64], in_=c.gv[t][:, h * 64:(h + 1) * 64].rearrange("(kb p) d -> p kb d", p=128)))
            r.append(e.dma_start(out=K[64:70, 0:TOK], in_=c.gao[h * 7:h * 7 + 6, :]))
            r.append(e.dma_start(out=K[70:71, 0:TOK], in_=c.flagrow[:, :]))
            r.append(e.dma_start(out=K[64:71, TOK:2 * TOK], in_=c.kaug_own[h, :, :]))
            r.append(e.dma_start(out=Q[0:64, :], in_=c.qT[h * 64:(h + 1) * 64, :]))
            r.append(e.dma_start(out=Q[64:71, :], in_=c.qaug[h, :, :]))
            return r
        P.dma("sp", ldk, ds_k[bi], 4 * NT + 5, reads=c.B_gko + c.B_gk + c.B_gvo + c.B_gv + [c.B_gao, c.B_kaug_own, c.B_qT, c.B_qaug], writes=[B_K[bi], B_V[bi], B_Q[bi]], name="kvq_ld")

    load_head(0)
    for h in range(nheads):
        bi = h % 2
        K, V, Q = Kt[bi], Vt[bi], Qt[bi]
        if h + 1 < nheads:
            load_head(h + 1)

        for qb in range(NT):
            q0 = qb * 512
            blocks = [(kb, 0) for kb in range(NKB_HALF)] + [(NKB_HALF + j, 0) for j in range(4 * qb)]
            blocks += [(NKB_HALF + 4 * qb + i, 128 * i) for i in range(4)]
            groups = [blocks[i:i + GRP] for i in range(0, len(blocks), GRP)]
            ob = 6 + (o_rr % 2)
            oi = o_rr % 2
            o_rr += 1
            nblk = len(blocks)

            def emit_S(g, gi):
                sb = (gi % 2) * GRP

                def em(pe, g=g, sb=sb, K=K, Q=Q, q0=q0):
                    r = None
                    for j, (kb, c0) in enumerate(g):
                        r = pe.matmul(c.ps[:, sb + j, c0:512], K[0:71, kb * 128:(kb + 1) * 128], Q[0:71, q0 + c0:q0 + 512], start=True, stop=True)
                    return r
                P.op("pe", em, reads=[B_K[bi], B_Q[bi]], writes=[c.psb[sb + j] for j in range(len(g))], name="S")

            state = {"blk": 0}

            def emit_exp_pv(g, gi):
                nonlocal pt_rr
                sb = (gi % 2) * GRP
                pi = pt_rr % 3
                pt_rr += 1
                pt = PT[pi]
                ng = len(g)
                P.op("act", lambda a, sb=sb, ng=ng, pt=pt: a.activation(out=pt[:, 0:ng, :], in_=c.ps[:, sb:sb + ng, :], func=AF.Exp),
                     reads=[c.psb[sb + j] for j in range(ng)], writes=[B_PT[pi]], name="exp")
                for j, (kb, c0) in enumerate(g):
                    if kb >= NKB_HALF + 4 * qb:
                        P.op("pool", lambda gp, j=j, c0=c0, pt=pt: gp.tensor_tensor(out=pt[:, j, c0:c0 + 128], in0=pt[:, j, c0:c0 + 128], in1=cm[:, :], op=ALU.mult),
                             reads=[B_PT[pi], B_cm], writes=[B_PT[pi]], name="cmask")
                b0 = state["blk"]

                def em(pe, g=g, pt=pt, b0=b0, ob=ob, nblk=nblk, V=V):
                    r = None
                    for j, (kb, c0) in enumerate(g):
                        r = pe.matmul(c.ps[0:65, ob, c0:512], V[:, kb, 0:65], pt[:, j, c0:512], start=(b0 + j == 0), stop=(b0 + j == nblk - 1))
                    return r
                state["blk"] += ng
                P.op("pe", em, reads=[B_PT[pi], B_V[bi]], writes=[c.psb[ob]], name="PV")

            emit_S(groups[0], 0)
            for gi in range(len(groups)):
                if gi + 1 < len(groups):
                    emit_S(groups[gi + 1], gi + 1)
                emit_exp_pv(groups[gi], gi)
            def ev(v, ob=ob, oi=oi):
                v.tensor_copy(out=osb[oi][0:64, :], in_=c.ps[0:64, ob, :])
                return v.tensor_copy(out=rsb[oi][64:65, :], in_=c.ps[64:65, ob, :])
            P.op("dve", ev, reads=[c.psb[ob]], writes=[B_osb[oi]], name="o_ev")
            P.dma("pool", lambda e, h=h, q0=q0, oi=oi: [e.dma_start(out=c.oT[h * 64:(h + 1) * 64, q0:q0 + 512], in_=osb[oi][0:64, :]),
                                                      e.dma_start(out=c.rsum[h:h + 1, q0:q0 + 512], in_=rsb[oi][64:65, :])],
                  ds_o[oi], 2, reads=[B_osb[oi]], writes=[c.B_oT, c.B_rsum], name="o_st")


def phase_e(P, c, ntiles=NT, nexp=NE):
    nc = P.nc
    P.sb_off = c.sb_phase_base
    ring = Ring(P, 4, name="ringe")
    c.ring = ring
    c.xT = P.sb("xTe", [128, KC, T], F32)
    oTt = P.sb("oTt", [128, KC, T], BF16)
    rbc = P.sb("rbc", [128, KC, T], F32)
    ytok = P.sb("ytok", [128, 4, D], F32, off=P.sb_off - 16384)
    hf = P.sb("hf", [128, KC, T], F32)
    c.hT = P.sb("hTe", [128, KC, T], BF16)
    c.sq = P.sb("sqe", [128, KC, T], BF16)
    Gs = P.sb("Gs", [128, NE, T], F32)
    c.aT = P.sb("aTe", [128, FC, T], BF16)
    c.th = [P.sb(f"the{i}", [128, T], F32) for i in range(3)]
    c.st = [P.sb(f"ste{i}", [128, T], F32) for i in range(6)]
    rw_s = P.sb("rw_s", [128, KC, NE], F32)
    rb_s = P.sb("rb_s", [128, 4, NE], F32)
    sel_s = P.sb("sel_s", [128, NE, 128], F32)
    lg = P.sb("lg", [128, 4, NE], F32)
    lg2 = P.sb("lg2", [128, 4, NE], F32)
    eq1 = P.sb("eq1", [128, 4, NE], F32)
    eq2 = P.sb("eq2", [128, 4, NE], F32)
    gt = P.sb("gt", [128, 4, NE], F32)
    m1 = P.sb("m1", [128, 4], F32)
    m2 = P.sb("m2", [128, 4], F32)
    p1 = P.sb("p1", [128, 4], F32)
    p2 = P.sb("p2", [128, 4], F32)
    gT_s = P.sb("gT_s", [128, T], F32)
    gT_p = P.sb("gT_p", [8, T], F32)
    print("SBUF used phase E:", P.sb_off)
    c.B_xT = [Buf(f"xTe{k}") for k in range(KC)]
    c.B_hT, c.B_sq = Buf("hTe"), Buf("sqe")
    c.B_aT = [Buf(f"aTe{k}") for k in range(FC)]
    c.B_th = [Buf(f"the{i}") for i in range(3)]
    c.B_st = [Buf(f"ste{i}") for i in range(6)]
    B_oTt, B_rbc, B_rt = Buf("oTt"), Buf("rbc"), Buf("rt")
    B_hf = [Buf("hf")] * KC
    B_G = [Buf(f"G{e}") for e in range(NE)]
    B_gT, B_gTp = Buf("gT"), Buf("gTp")
    B_rc = Buf("rconst")
    ds_in = [P.new_dsem(f"ein{i}") for i in range(3)]
    ds_m = [P.new_dsem(f"em{i}") for i in range(3)]
    ds_h = [P.new_dsem(f"eh{i}") for i in range(3)]
    ds_out = P.new_dsem("eout")
    B_out = Buf("out_d")
    B_x2d = [Buf(f"x2d{t}") for t in range(ntiles)]
    B_hd = [Buf(f"hd{t}") for t in range(ntiles)]
    B_gd = [Buf(f"gd{t}") for t in range(ntiles)]
    x2T_d = nc.dram_tensor("x2T_d", [128, KC, TOK], F32)
    hT_d = nc.dram_tensor("hT_d", [128, KC, TOK], BF16)
    gT_d = nc.dram_tensor("gT_d", [8, TOK], F32)
    cp = Ctx()
    cp.__dict__.update(c.__dict__)
    cp.xT = hf
    cp.B_xT = B_hf
    P.dma("sp", lambda e: [e.dma_start(out=rw_s[:, :, :], in_=c.rw[:, :, :]), e.dma_start(out=rb_s[:, :, :], in_=c.rb4[:, :, :]),
                           e.dma_start(out=sel_s[:, :, :], in_=c.sel[:, :, :])], P.new_dsem("rconst"), 3, writes=[B_rc], name="rconst_ld")
    P.op("dve", lambda v: v.memset(gT_s[:, :], 0.0), writes=[B_gT], name="gT_zero")
    X = mybir.AxisListType.X

    def prologue(t):
        t0 = t * T
        P.dma("sp", lambda e: [e.dma_start(out=hf[:, :, :], in_=c.x1T[:, :, t0:t0 + T])], ds_in[0], 1, reads=c.B_x1T, writes=B_hf, name="x1_ld")
        P.dma("sp", lambda e: [e.dma_start(out=oTt[:, :, :], in_=c.oT[:, t0:t0 + T].rearrange("(cc p) t -> p cc t", p=128))], ds_in[1], 1,
              reads=[c.B_oT], writes=[B_oTt], name="oT_ld")

        def ldr(e):
            r = []
            for hh in range(2):
                base = c.rsum[hh:hh + 1, t0:t0 + T]
                src = bass.AP(tensor=base.tensor, offset=base.offset, ap=[[0, 64], [2 * TOK, 8], [1, T]])
                r.append(e.dma_start(out=rbc[hh * 64:(hh + 1) * 64, :, :], in_=src))
            return r
        P.dma("sp", ldr, ds_in[2], 2, reads=[c.B_rsum], writes=[B_rbc], name="rsum_ld")
        P.op("dve", lambda v: v.reciprocal(out=rbc[:, :, :], in_=rbc[:, :, :]), reads=[B_rbc], writes=[B_rbc], name="recip")
        P.op("pool", lambda g: g.tensor_tensor(out=oTt[:, :, :], in0=oTt[:, :, :], in1=rbc[:, :, :], op=ALU.mult), reads=[B_rbc, B_oTt], writes=[B_oTt], name="o_norm")
        yield
        s_ = ring.load(lambda i: [(ring.v8[i][:, :, :], w8(c.w_o_b, 0, 1024))], c.B_w["att"], name="w_o")
        for dc in range(KC):
            b = psbank(c)
            mm_group(P, c, b, [(ring.v8[s_][:, k, dc * 128:(dc + 1) * 128], oTt[:, k, :]) for k in range(KC)], [ring.bufs[s_], B_oTt], T, name="w_o")
            P.op("dve", lambda v, dc=dc, b=b: v.tensor_tensor(out=hf[:, dc, :], in0=c.ps[:, b, :], in1=hf[:, dc, :], op=ALU.add),
                 reads=[c.psb[b], B_hf[dc]], writes=[B_hf[dc]], name="res_att")
        if c.debug == "x2":
            P.dma("act", lambda e: [e.dma_start(out=c.dbg_x2T[:, :, t0:t0 + T], in_=hf[:, :, :])], ds_out, 1, reads=B_hf, writes=[B_out], name="x2_st")
        P.dma("act", lambda e: [e.dma_start(out=x2T_d[:, :, t0:t0 + T], in_=hf[:, :, :])], ds_h[0], 1, reads=B_hf, writes=[B_x2d[t]], name="x2d_st")
        yield
        ri = rms_stats(P, cp, T, B_hf)
        apply_norm(P, cp, T, ri, V_FFN1, rbc, B_rbc, name="hf")
        P.op("act", lambda a: a.copy(out=oTt[:, :, :], in_=rbc[:, :, :]), reads=[B_rbc], writes=[B_oTt], name="hT_cast")
        P.dma("act", lambda e: [e.dma_start(out=hT_d[:, :, t0:t0 + T], in_=oTt[:, :, :])], ds_h[1], 1, reads=[B_oTt], writes=[B_hd[t]], name="hd_st")
        yield
        b = psbank(c)

        def emr(pe, b=b):
            r = None
            for tc in range(4):
                for k in range(KC):
                    r = pe.matmul(c.ps[:, b, tc * 8:(tc + 1) * 8], rbc[:, k, tc * 128:(tc + 1) * 128], rw_s[:, k, :], start=(k == 0), stop=(k == KC - 1))
            return r
        P.op("pe", emr, reads=[B_rbc, B_rc], writes=[c.psb[b]], name="router_mm")
        B_s = {k: Buf("rt_" + k) for k in ("lg", "m1", "eq1", "lg2", "m2", "eq2", "p2", "p1", "gt")}
        P.op("dve", lambda v, b=b: v.tensor_tensor(out=lg[:, :, :], in0=c.ps[:, b, 0:32].rearrange("p (a e) -> p a e", e=NE), in1=rb_s[:, :, :], op=ALU.add),
             reads=[c.psb[b], B_rc, B_rt], writes=[B_s["lg"]], name="lg")
        P.op("dve", lambda v: v.tensor_reduce(out=m1[:, :], in_=lg[:, :, :], axis=X, op=ALU.max), reads=[B_s["lg"]], writes=[B_s["m1"]], name="m1")
        P.op("dve", lambda v: v.tensor_tensor(out=eq1[:, :, :], in0=lg[:, :, :], in1=bcast_last(m1[:, :], NE), op=ALU.is_equal),
             reads=[B_s["lg"], B_s["m1"]], writes=[B_s["eq1"]], name="eq1")
        P.op("dve", lambda v: v.scalar_tensor_tensor(out=lg2[:, :, :], in0=eq1[:, :, :], scalar=-1e30, in1=lg[:, :, :], op0=ALU.mult, op1=ALU.add),
             reads=[B_s["eq1"], B_s["lg"]], writes=[B_s["lg2"]], name="lg2")
        P.op("dve", lambda v: v.tensor_reduce(out=m2[:, :], in_=lg2[:, :, :], axis=X, op=ALU.max), reads=[B_s["lg2"]], writes=[B_s["m2"]], name="m2")
        P.op("dve", lambda v: v.tensor_tensor(out=eq2[:, :, :], in0=lg2[:, :, :], in1=bcast_last(m2[:, :], NE), op=ALU.is_equal),
             reads=[B_s["lg2"], B_s["m2"]], writes=[B_s["eq2"]], name="eq2")
        P.op("dve", lambda v: v.tensor_tensor(out=p2[:, :], in0=m2[:, :], in1=m1[:, :], op=ALU.subtract), reads=[B_s["m1"], B_s["m2"]], writes=[B_s["p2"]], name="d21")
        P.op("act", lambda a: a.activation(out=p2[:, :], in_=p2[:, :], func=AF.Tanh, scale=0.5), reads=[B_s["p2"]], writes=[B_s["p2"]], name="tanh_r")
        P.op("dve", lambda v: v.tensor_scalar(out=p2[:, :], in0=p2[:, :], scalar1=0.5, scalar2=0.5, op0=ALU.mult, op1=ALU.add), reads=[B_s["p2"]], writes=[B_s["p2"]], name="p2")
        P.op("dve", lambda v: v.tensor_scalar(out=p1[:, :], in0=p2[:, :], scalar1=-1.0, scalar2=1.0, op0=ALU.mult, op1=ALU.add), reads=[B_s["p2"]], writes=[B_s["p1"]], name="p1")
        P.op("dve", lambda v: v.tensor_tensor(out=gt[:, :, :], in0=eq1[:, :, :], in1=bcast_last(p1[:, :], NE), op=ALU.mult), reads=[B_s["eq1"], B_s["p1"]], writes=[B_s["gt"]], name="g1")
        P.op("dve", lambda v: v.tensor_tensor(out=eq2[:, :, :], in0=eq2[:, :, :], in1=bcast_last(p2[:, :], NE), op=ALU.mult), reads=[B_s["eq2"], B_s["p2"]], writes=[B_s["eq2"]], name="g2")
        P.op("dve", lambda v: v.tensor_tensor(out=gt[:, :, :], in0=gt[:, :, :], in1=eq2[:, :, :], op=ALU.add), reads=[B_s["gt"], B_s["eq2"]], writes=[B_s["gt"], B_rt], name="gates")
        yield
        b = psbank(c)

        def emgt(pe, b=b):
            r = None
            for tc in range(4):
                r = pe.transpose(out=c.ps[0:8, b, tc * 128:(tc + 1) * 128], in_=gt[:, tc, :], identity=c.ident_f[:, :])
            return r
        P.op("pe", emgt, reads=[B_rt, c.B_const], writes=[c.psb[b]], name="gT")
        P.op("act", lambda a, b=b: a.copy(out=gT_p[0:8, :], in_=c.ps[0:8, b, :]), reads=[c.psb[b]], writes=[B_gTp], name="gT_ev")
        P.dma("act", lambda e: [e.dma_start(out=gT_d[:, t0:t0 + T], in_=gT_p[0:8, :])], ds_h[2], 1, reads=[B_gTp], writes=[B_gd[t]], name="gd_st")
        yield

    def run_all(gen):
        for _ in gen:
            pass

    run_all(prologue(0))
    for t in range(ntiles):
        t0 = t * T
        nxt = prologue(t + 1) if t + 1 < ntiles else None
        P.dma("sp", lambda e, t0=t0: [e.dma_start(out=c.xT[:, :, :], in_=x2T_d[:, :, t0:t0 + T])], ds_m[0], 1, reads=[B_x2d[t]], writes=c.B_xT, name="x2_ld")
        P.dma("sp", lambda e, t0=t0: [e.dma_start(out=c.hT[:, :, :], in_=hT_d[:, :, t0:t0 + T])], ds_m[1], 1, reads=[B_hd[t]], writes=[c.B_hT], name="h_ld")
        P.dma("sp", lambda e, t0=t0: [e.dma_start(out=gT_s[0:8, :], in_=gT_d[:, t0:t0 + T])], ds_m[2], 1, reads=[B_gd[t]], writes=[B_gT], name="g_ld")
        for e in range(NE):
            b = psbank(c)
            P.op("pe", lambda pe, b=b, e=e: pe.matmul(c.ps[:, b, :], sel_s[:, e, :], gT_s[:, :], start=True, stop=True),
                 reads=[B_gT, B_rc], writes=[c.psb[b]], name="G_bc")
            if e % 2 == 0:
                P.op("act", lambda a, b=b, e=e: a.copy(out=Gs[:, e, :], in_=c.ps[:, b, :]), reads=[c.psb[b]], writes=[B_G[e]], name="G_ev")
            else:
                P.op("dve", lambda v, b=b, e=e: v.tensor_copy(out=Gs[:, e, :], in_=c.ps[:, b, :]), reads=[c.psb[b]], writes=[B_G[e]], name="G_ev")
        if c.debug == "x2":
            if nxt is not None:
                run_all(nxt)
            continue
        for e in range(nexp):
            ffn_stage(P, c, c.moe_g_b[e], c.moe_u_b[e], c.moe_d_b[e], c.B_w[f"e{e}"], c.hT, c.B_hT, gate=(Gs[:, e, :], B_G[e]))
            if nxt is not None and 1 <= e <= 5:
                next(nxt)
        if nxt is not None and nexp < 6:
            run_all(nxt)
        if c.debug == "x3":
            P.dma("act", lambda e, t0=t0: [e.dma_start(out=c.dbg_x2T[:, :, t0:t0 + T], in_=c.xT[:, :, :])], ds_out, 1, reads=c.B_xT, writes=[B_out], name="x3_st")
            continue
        ri = rms_stats(P, c, T, c.B_xT)
        B_y = B_hf[0]
        apply_norm(P, c, T, ri, V_FIN, hf, B_y, name="yT")
        for tc in range(4):
            for kh in range(2):
                b = psbank(c)

                def emt(pe, tc=tc, kh=kh, b=b):
                    r = None
                    for kk in range(4):
                        k = kh * 4 + kk
                        r = pe.transpose(out=c.ps[:, b, kk * 128:(kk + 1) * 128], in_=hf[:, k, tc * 128:(tc + 1) * 128], identity=c.ident_f[:, :])
                    return r
                P.op("pe", emt, reads=[B_y, c.B_const], writes=[c.psb[b]], name="y_xpose")
                if kh == 0:
                    P.op("act", lambda a, tc=tc, kh=kh, b=b: a.copy(out=ytok[:, tc, kh * 512:(kh + 1) * 512], in_=c.ps[:, b, :]), reads=[c.psb[b]], writes=[B_rbc], name="y_ev")
                else:
                    P.op("dve", lambda v, tc=tc, kh=kh, b=b: v.tensor_copy(out=ytok[:, tc, kh * 512:(kh + 1) * 512], in_=c.ps[:, b, :]), reads=[c.psb[b]], writes=[B_rbc], name="y_ev")
        P.dma("act", lambda e, t0=t0: [e.dma_start(out=c.out[t0:t0 + T, :].rearrange("(tc p) d -> p tc d", p=128), in_=ytok[:, :, :])], ds_out, 1,
              reads=[B_rbc], writes=[B_out], name="out_st")


def host_prep(inp):
    f32 = np.float32
    x = np.asarray(inp["x"], f32)

    def pv(v):
        return np.ascontiguousarray(np.asarray(v, f32).reshape(8, 128).T)
    b_in = np.asarray(inp["conv_b_in"], f32)[0]
    vecs = np.concatenate([
        pv(inp["mix_norm"][0]), pv(inp["ffn_norm"][0]), pv(b_in[:1024]), pv(b_in[1024:]),
        pv(inp["conv_b_dw"][0]), pv(inp["conv_ln_g"][0]), pv(inp["conv_ln_b"][0]), pv(inp["conv_b_out"][0]),
        pv(inp["kv_norm"]), pv(inp["mix_norm"][1]), pv(inp["ffn_norm"][1]), pv(inp["final_norm"])], axis=1)
    wdw = np.ascontiguousarray(np.asarray(inp["conv_w_dw"], f32)[0].reshape(31, 8, 128).transpose(2, 1, 0))
    bf = np.asarray(inp["b_f"], f32).reshape(16, 1)
    rw = np.ascontiguousarray(np.asarray(inp["router_w"], f32)[0].reshape(8, 128, 8).transpose(1, 0, 2))
    rb4 = np.ascontiguousarray(np.broadcast_to(np.asarray(inp["router_b"], f32)[0][None, None, :], (128, 4, 8)))
    sel = np.zeros((128, 8, 128), f32)
    for e in range(8):
        sel[e, e, :] = 1.0
    ident = np.eye(128, dtype=f32)
    cmask = (np.arange(128)[:, None] <= np.arange(128)[None, :]).astype(ml_dtypes.bfloat16)
    common = dict(vecs=vecs, wdw=wdw, bf=bf, rw=rw, rb4=rb4, sel=sel, ident=ident, cmask=cmask,
                  conv_w_in=np.asarray(inp["conv_w_in"], f32)[0], conv_w_out=np.asarray(inp["conv_w_out"], f32)[0],
                  ffn_w_gate=np.asarray(inp["ffn_w_gate"], f32)[0], ffn_w_up=np.asarray(inp["ffn_w_up"], f32)[0],
                  ffn_w_down=np.asarray(inp["ffn_w_down"], f32)[0], w_kvf=np.asarray(inp["w_kvf"], f32),
                  w_q=np.asarray(inp["w_q"], f32)[0], w_o=np.asarray(inp["w_o"], f32)[0],
                  moe_w_gate=np.asarray(inp["moe_w_gate"], f32)[0], moe_w_up=np.asarray(inp["moe_w_up"], f32)[0],
                  moe_w_down=np.asarray(inp["moe_w_down"], f32)[0])
    maps = []
    for core in range(8):
        b, hf = core // 2, core % 2
        m = dict(common)
        m["x"] = np.ascontiguousarray(x[b, hf * 4096:(hf + 1) * 4096])
        if hf == 0:
            m["xhalo"] = np.zeros((32, 1024), f32)
            m["hmask"] = np.zeros((128, 1), f32)
            m["flagrow"] = np.full((1, 4096), -30000.0, dtype=ml_dtypes.bfloat16)
        else:
            m["xhalo"] = np.ascontiguousarray(x[b, 4096 - 32:4096])
            m["hmask"] = np.ones((128, 1), f32)
            m["flagrow"] = np.zeros((1, 4096), dtype=ml_dtypes.bfloat16)
        maps.append(m)
    return maps


def build(debug=None, ntiles=NT, ne_decl=NE, stop_after=None):
    nc = bass.Bass("TRN2", target_bir_lowering=False)
    c = Ctx()
    declare_io(nc, c, debug, ne_decl)
    P = Prog(nc)
    setup_common(P, c)
    c.sb_phase_base = P.sb_off
    alloc_phase_a(P, c)
    print("SBUF used after phase A alloc:", P.sb_off)
    phase_a(P, c, ntiles=ntiles, debug=debug)
    if debug not in ("xa", "a"):
        P.barrier()
        phase_b(P, c)
        if stop_after != "b":
            phase_c(P, c)
        P.barrier()
        if stop_after in ("b", "c"):
            dk = nc.dram_tensor("dbg_kaug", [H, 7, TOK], BF16, kind="ExternalOutput")
            dq = nc.dram_tensor("dbg_qaug", [H, 7, TOK], BF16, kind="ExternalOutput")
            dgi = nc.dram_tensor("dbg_gin", [112, TOK], BF16, kind="ExternalOutput")
            ds = P.new_dsem("dbg")
            P.dma("sp", lambda e: [e.dma_start(out=dk[:, :, :], in_=c.kaug_own[:, :, :]),
                                   e.dma_start(out=dq[:, :, :], in_=c.qaug[:, :, :]), e.dma_start(out=dgi[:, :], in_=c.ga[:, :])], ds, 3, name="dbg_out")
        else:
            phase_d(P, c)
            P.barrier()
            if stop_after == "d":
                do = nc.dram_tensor("dbg_oT", [D, TOK], BF16, kind="ExternalOutput")
                dr = nc.dram_tensor("dbg_rsum", [H, TOK], F32, kind="ExternalOutput")
                ds = P.new_dsem("dbg")
                P.dma("sp", lambda e: [e.dma_start(out=do[:, :], in_=c.oT[:, :]), e.dma_start(out=dr[:, :], in_=c.rsum[:, :])], ds, 2, name="dbg_out")
            else:
                ce = Ctx()
                ce.__dict__.update(c.__dict__)
                phase_e(P, ce, nexp=ne_decl)
    if debug in ("xa", "a"):
        c.dbg_x1T = nc.dram_tensor("dbg_x1T", [128, KC, TOK], F32, kind="ExternalOutput")
        c.dbg_gin = nc.dram_tensor("dbg_gin", [2160, TOK], BF16, kind="ExternalOutput")
        c.dbg_qT = nc.dram_tensor("dbg_qT", [D, TOK], BF16, kind="ExternalOutput")
        c.dbg_flog = nc.dram_tensor("dbg_flog", [16, TOK], F32, kind="ExternalOutput")
        ds = P.new_dsem("dbg")
        P.dma("sp", lambda e: [e.dma_start(out=c.dbg_x1T[:, :, :], in_=c.x1T[:, :, :]),
                               e.dma_start(out=c.dbg_gin[:, :], in_=c.gin[:, :]),
                               e.dma_start(out=c.dbg_qT[:, :], in_=c.qT[:, :]),
                               e.dma_start(out=c.dbg_flog[:, :], in_=c.flog[:, :])], ds, 4,
              reads=c.B_x1T + [c.B_gin, c.B_qT, c.B_flog], name="dbg_out")
    stats = P.emit_all()
    print("ops per engine (n, waits):", stats)
    return nc


def kernel(**inputs):
    maps = host_prep(inputs)
    nc = build(debug=None)
    res = run_bass_kernel_spmd(nc, maps, core_ids=list(range(8)))
    out = np.empty((4, 8192, 1024), np.float32)
    for core in range(8):
        out[core // 2, (core % 2) * 4096:(core % 2 + 1) * 4096] = res.results[core]["out"]
    return out
```
